# Optimizing a Trainium2 kernel written in Bass

```python
import jax
import jax.numpy as jnp
from jax import lax
import numpy as np

D_MODEL = 1024
BATCH = 2
SEQ = 8192
DEPTH = 4

N_HEADS = 4
HEAD_DIM = 64
BR_WIDTH = N_HEADS * HEAD_DIM
N_BRANCH = 5
N_MEM = 256
ROPE_THETA = 10000.0
EPS = 1e-6
Q_BLOCK = 128
DSA_TOPK_MAX = 256
IDX_HEADS = 4
IDX_DIM = 32
KV_LATENT = 128
NSA_KV_DIM = 64
CMP_LEN = 32
CMP_STRIDE = 16
CMP_HIDDEN = 128
SLC_BLOCK = 64
SLC_TOPN = 16
WINDOW = 512
FORCE_SCORE = 1e4

IN_LAYOUT = (
    ('dsa_q', BR_WIDTH), ('dsa_ckv', KV_LATENT), ('idx_q', IDX_HEADS * IDX_DIM), ('idx_k', IDX_DIM), ('idx_w', IDX_HEADS), ('dsa_z', BR_WIDTH),
    ('fox_q', BR_WIDTH), ('fox_k', BR_WIDTH), ('fox_v', BR_WIDTH), ('fox_f', N_HEADS), ('fox_z', BR_WIDTH),
    ('sb_q', BR_WIDTH), ('sb_k', BR_WIDTH), ('sb_v', BR_WIDTH), ('sb_z', BR_WIDTH),
    ('nsa_q', BR_WIDTH), ('nsa_kc', NSA_KV_DIM), ('nsa_vc', NSA_KV_DIM), ('nsa_ks', NSA_KV_DIM), ('nsa_vs', NSA_KV_DIM),
    ('nsa_kw', NSA_KV_DIM), ('nsa_vw', NSA_KV_DIM), ('nsa_g', 3 * N_HEADS), ('nsa_z', BR_WIDTH),
    ('mem_q', BR_WIDTH), ('mem_z', BR_WIDTH),
    ('merge', N_BRANCH * D_MODEL),
)
N_IN = sum(n for _, n in IN_LAYOUT)

kernel_name = 'gated_parallel_hybrid_trunk'


def _rmsnorm(x, g):
    xf = x.astype(jnp.float32)
    y = xf * lax.rsqrt(jnp.mean(xf * xf, axis=-1, keepdims=True) + EPS)
    return (y * g.astype(jnp.float32)).astype(x.dtype)


def _rope(x, pos):
    dh = x.shape[-1]
    inv = ROPE_THETA ** (-jnp.arange(0, dh, 2, dtype=jnp.float32) / dh)
    ang = pos.astype(jnp.float32)[..., None] * inv
    ang = ang.reshape(ang.shape[:2] + (1,) * (x.ndim - 3) + ang.shape[-1:])
    cos, sin = jnp.cos(ang), jnp.sin(ang)
    xf = x.astype(jnp.float32)
    x1, x2 = xf[..., : dh // 2], xf[..., dh // 2:]
    return jnp.concatenate([x1 * cos - x2 * sin, x2 * cos + x1 * sin], axis=-1).astype(x.dtype)


def _masked_softmax(logits, mask):
    l = jnp.where(mask, logits, -jnp.inf)
    m = jnp.max(l, axis=-1, keepdims=True)
    m = jnp.where(jnp.isfinite(m), m, 0.0)
    e = jnp.where(mask, jnp.exp(l - m), 0.0)
    return e / jnp.maximum(jnp.sum(e, axis=-1, keepdims=True), 1e-30)


def _to_blocks(a):
    b, s = a.shape[:2]
    return jnp.moveaxis(a.reshape((b, s // Q_BLOCK, Q_BLOCK) + a.shape[2:]), 1, 0)


def _from_blocks(a):
    nq, b, q = a.shape[:3]
    return jnp.moveaxis(a, 0, 1).reshape((b, nq * q) + a.shape[3:])


def _gather_rows(table, idx):
    return jax.vmap(lambda tb, ib: tb[ib])(table, idx)


def _split_columns(p):
    out, off = {}, 0
    for name, n in IN_LAYOUT:
        out[name] = p[..., off:off + n]
        off += n
    return out


def _dsa_attention(q, k, v, qi, ki, wi):
    B, S, H, Dh = q.shape
    k_sel = min(DSA_TOPK_MAX, S // 4)
    s_pos = jnp.arange(S)
    scale = Dh ** -0.5
    idx_scale = (IDX_DIM ** -0.5) * (IDX_HEADS ** -0.5)

    def block(args):
        qb, q_blk, qi_blk, wi_blk = args
        t = qb * Q_BLOCK + jnp.arange(Q_BLOCK)
        rel = jax.nn.relu(jnp.einsum('bqhd,bsd->bqhs', qi_blk, ki).astype(jnp.float32))
        score = jnp.einsum('bqhs,bqh->bqs', rel, wi_blk.astype(jnp.float32)) * idx_scale
        score = jnp.where(s_pos[None, None, :] <= t[None, :, None], score, -jnp.inf)
        _, sel = lax.top_k(score, k_sel)
        kg = _gather_rows(k, sel)
        vg = _gather_rows(v, sel)
        logits = jnp.einsum('bqhd,bqkhd->bhqk', q_blk, kg).astype(jnp.float32) * scale
        mask = (sel <= t[None, :, None])[:, None]
        p = _masked_softmax(logits, mask).astype(v.dtype)
        return jnp.einsum('bhqk,bqkhd->bqhd', p, vg)

    out = lax.map(block, (jnp.arange(S // Q_BLOCK), _to_blocks(q), _to_blocks(qi), _to_blocks(wi)))
    return _from_blocks(out)


def _fox_attention(q, k, v, log_f):
    B, S, H, Dh = q.shape
    s_pos = jnp.arange(S)
    scale = Dh ** -0.5
    cum = jnp.cumsum(log_f.astype(jnp.float32), axis=1)
    cum_s = jnp.moveaxis(cum, 1, 2)

    def block(args):
        qb, q_blk, c_blk = args
        t = qb * Q_BLOCK + jnp.arange(Q_BLOCK)
        logits = (jnp.einsum('bqhd,bshd->bhqs', q_blk, k).astype(jnp.float32) * scale
                  + jnp.moveaxis(c_blk, 1, 2)[..., None] - cum_s[:, :, None, :])
        mask = (s_pos[None, :] <= t[:, None])[None, None]
        p = _masked_softmax(logits, mask).astype(v.dtype)
        return jnp.einsum('bhqs,bshd->bqhd', p, v)

    out = lax.map(block, (jnp.arange(S // Q_BLOCK), _to_blocks(q), _to_blocks(cum)))
    return _from_blocks(out)


def _stick_breaking_attention(q, k, v):
    B, S, H, Dh = q.shape
    s_pos = jnp.arange(S)
    scale = Dh ** -0.5

    def block(args):
        qb, q_blk = args
        t = qb * Q_BLOCK + jnp.arange(Q_BLOCK)
        z = jnp.einsum('bqhd,bshd->bhqs', q_blk, k).astype(jnp.float32) * scale
        strict = (s_pos[None, :] < t[:, None])[None, None]
        log_keep = jnp.where(strict, jax.nn.log_sigmoid(-z), 0.0)
        later = lax.cumsum(log_keep, axis=3, reverse=True) - log_keep
        a = jnp.where(strict, jnp.exp(jax.nn.log_sigmoid(z) + later), 0.0).astype(v.dtype)
        return jnp.einsum('bhqs,bshd->bqhd', a, v)

    out = lax.map(block, (jnp.arange(S // Q_BLOCK), _to_blocks(q)))
    return _from_blocks(out)


def _nsa_attention(q, kc_tok, vc_tok, ks, vs, kw, vw, gates, pe_k, pe_v, wc1_k, wc2_k, wc1_v, wc2_v):
    B, S, H, Dh = q.shape
    scale = Dh ** -0.5
    n_cmp = (S - CMP_LEN) // CMP_STRIDE + 1
    n_blk = S // SLC_BLOCK
    n_sel = min(SLC_TOPN, n_blk)
    per_blk = SLC_BLOCK // CMP_STRIDE
    tok = (jnp.arange(n_cmp) * CMP_STRIDE)[:, None] + jnp.arange(CMP_LEN)[None, :]

    def compress(x_tok, pe, w1, w2):
        blocks = (x_tok[:, tok] + pe).reshape(B, n_cmp, CMP_LEN * x_tok.shape[-1])
        return jax.nn.silu(blocks @ w1) @ w2

    kc = compress(kc_tok, pe_k, wc1_k, wc2_k)
    vc = compress(vc_tok, pe_v, wc1_v, wc2_v)
    cmp_end = jnp.arange(n_cmp) * CMP_STRIDE + CMP_LEN - 1
    ks_blk = ks.reshape(B, n_blk, SLC_BLOCK, -1)
    vs_blk = vs.reshape(B, n_blk, SLC_BLOCK, -1)
    kw_pad = jnp.pad(kw, ((0, 0), (WINDOW, 0), (0, 0)))
    vw_pad = jnp.pad(vw, ((0, 0), (WINDOW, 0), (0, 0)))
    blk_ids = jnp.arange(n_blk)
    in_blk = jnp.arange(SLC_BLOCK)
    win_off = jnp.arange(WINDOW + Q_BLOCK) - WINDOW

    def block(args):
        qb, q_blk, g_blk = args
        t = qb * Q_BLOCK + jnp.arange(Q_BLOCK)
        lc = jnp.einsum('bqhd,bnd->bhqn', q_blk, kc).astype(jnp.float32) * scale
        pc = _masked_softmax(lc, (cmp_end[None, :] <= t[:, None])[None, None])
        o_cmp = jnp.einsum('bhqn,bnd->bqhd', pc.astype(vc.dtype), vc)
        imp = jnp.pad(jnp.sum(pc, axis=1), ((0, 0), (0, 0), (0, n_blk * per_blk - n_cmp)))
        imp = jnp.sum(imp.reshape(B, Q_BLOCK, n_blk, per_blk), axis=-1)
        cur = t // SLC_BLOCK
        forced = (blk_ids[None, :] == 0) | (blk_ids[None, :] == cur[:, None]) | (blk_ids[None, :] == cur[:, None] - 1)
        visible = blk_ids[None, :] <= cur[:, None]
        imp = jnp.where(forced[None], FORCE_SCORE, jnp.where(visible[None], imp, -1.0))
        _, sel = lax.top_k(imp, n_sel)
        ksg = _gather_rows(ks_blk, sel).reshape(B, Q_BLOCK, n_sel * SLC_BLOCK, -1)
        vsg = _gather_rows(vs_blk, sel).reshape(B, Q_BLOCK, n_sel * SLC_BLOCK, -1)
        pos = (sel[..., None] * SLC_BLOCK + in_blk).reshape(B, Q_BLOCK, n_sel * SLC_BLOCK)
        ls = jnp.einsum('bqhd,bqkd->bhqk', q_blk, ksg).astype(jnp.float32) * scale
        ps = _masked_softmax(ls, (pos <= t[None, :, None])[:, None])
        o_slc = jnp.einsum('bhqk,bqkd->bqhd', ps.astype(vs.dtype), vsg)
        kwb = lax.dynamic_slice_in_dim(kw_pad, qb * Q_BLOCK, WINDOW + Q_BLOCK, axis=1)
        vwb = lax.dynamic_slice_in_dim(vw_pad, qb * Q_BLOCK, WINDOW + Q_BLOCK, axis=1)
        s = qb * Q_BLOCK + win_off
        d = t[:, None] - s[None, :]
        mw = ((d >= 0) & (d < WINDOW) & (s[None, :] >= 0))[None, None]
        lw = jnp.einsum('bqhd,bkd->bhqk', q_blk, kwb).astype(jnp.float32) * scale
        pw = _masked_softmax(lw, mw)
        o_win = jnp.einsum('bhqk,bkd->bqhd', pw.astype(vw.dtype), vwb)
        return (g_blk[:, :, 0, :, None] * o_cmp + g_blk[:, :, 1, :, None] * o_slc
                + g_blk[:, :, 2, :, None] * o_win)

    out = lax.map(block, (jnp.arange(S // Q_BLOCK), _to_blocks(q), _to_blocks(gates)))
    return _from_blocks(out)


def _memory_attention(q, mem_k, mem_v):
    logits = jnp.einsum('bshd,bmhd->bhsm', q, mem_k).astype(jnp.float32) * HEAD_DIM ** -0.5
    p = jax.nn.softmax(logits, axis=-1).astype(mem_v.dtype)
    return jnp.einsum('bhsm,bmhd->bshd', p, mem_v)


def _layer(x, mem, positions, norm_g, w_in, kv_norm, w_uk, w_uv, fox_b, pe_k, pe_v,
           wc1_k, wc2_k, wc1_v, wc2_v, mem_norm, w_mem_kv, w_branch, w_out):
    B, S, _ = x.shape
    h = _rmsnorm(x, norm_g)
    p = _split_columns(h @ w_in)
    heads = lambda a: a.reshape(B, S, N_HEADS, HEAD_DIM)

    c_kv = _rmsnorm(p['dsa_ckv'], kv_norm)
    k_a = _rope(heads(c_kv @ w_uk), positions)
    v_a = heads(c_kv @ w_uv)
    q_a = _rope(heads(p['dsa_q']), positions)
    qi = _rope(p['idx_q'].reshape(B, S, IDX_HEADS, IDX_DIM), positions)
    ki = _rope(p['idx_k'], positions)
    y_a = _dsa_attention(q_a, k_a, v_a, qi, ki, p['idx_w'])

    log_f = jax.nn.log_sigmoid(p['fox_f'].astype(jnp.float32) + fox_b.astype(jnp.float32))
    y_b = _fox_attention(heads(p['fox_q']), heads(p['fox_k']), heads(p['fox_v']), log_f)

    y_c = _stick_breaking_attention(heads(p['sb_q']), heads(p['sb_k']), heads(p['sb_v']))

    q_d = _rope(heads(p['nsa_q']), positions)
    gates = jax.nn.sigmoid(p['nsa_g'].reshape(B, S, 3, N_HEADS))
    y_d = _nsa_attention(q_d, _rope(p['nsa_kc'], positions), p['nsa_vc'],
                         _rope(p['nsa_ks'], positions), p['nsa_vs'],
                         _rope(p['nsa_kw'], positions), p['nsa_vw'], gates,
                         pe_k, pe_v, wc1_k, wc2_k, wc1_v, wc2_v)

    mkv = _rmsnorm(mem, mem_norm) @ w_mem_kv
    mem_k = mkv[..., :BR_WIDTH].reshape(B, -1, N_HEADS, HEAD_DIM)
    mem_v = mkv[..., BR_WIDTH:].reshape(B, -1, N_HEADS, HEAD_DIM)
    y_e = _memory_attention(heads(p['mem_q']), mem_k, mem_v)

    ys = jnp.stack([
        y_a.reshape(B, S, BR_WIDTH) * jax.nn.silu(p['dsa_z']),
        y_b.reshape(B, S, BR_WIDTH) * jax.nn.silu(p['fox_z']),
        y_c.reshape(B, S, BR_WIDTH) * jax.nn.silu(p['sb_z']),
        y_d.reshape(B, S, BR_WIDTH) * jax.nn.silu(p['nsa_z']),
        y_e.reshape(B, S, BR_WIDTH) * jax.nn.silu(p['mem_z']),
    ], axis=2)
    merge = jax.nn.sigmoid(p['merge'].reshape(B, S, N_BRANCH, D_MODEL))
    merged = jnp.sum(merge * jnp.einsum('bsnc,ncd->bsnd', ys, w_branch), axis=2)
    return x + merged @ w_out


def setup_inputs(seed: int = 0) -> dict:
    key = jax.random.key(seed)
    ks = jax.random.split(key, 24)
    f32 = jnp.float32

    def nrm(k, shape, fan_in):
        return jax.random.normal(k, shape, f32) * fan_in ** -0.5

    def gain(k, shape):
        return 1.0 + 0.02 * jax.random.normal(k, shape, f32)

    return {
        'x': jax.random.normal(ks[0], (BATCH, SEQ, D_MODEL), f32),
        'mem': jax.random.normal(ks[1], (BATCH, N_MEM, D_MODEL), f32),
        'positions': jnp.broadcast_to(jnp.arange(SEQ, dtype=jnp.int32), (BATCH, SEQ)),
        'norm_g': gain(ks[2], (DEPTH, D_MODEL)),
        'w_in': nrm(ks[3], (DEPTH, D_MODEL, N_IN), D_MODEL),
        'kv_norm': gain(ks[4], (DEPTH, KV_LATENT)),
        'w_uk': nrm(ks[5], (DEPTH, KV_LATENT, BR_WIDTH), KV_LATENT),
        'w_uv': nrm(ks[6], (DEPTH, KV_LATENT, BR_WIDTH), KV_LATENT),
        'fox_bias': jnp.linspace(1.0, 4.0, N_HEADS, dtype=f32)[None, :] + 0.1 * jax.random.normal(ks[7], (DEPTH, N_HEADS), f32),
        'nsa_pe_k': 0.1 * jax.random.normal(ks[8], (DEPTH, CMP_LEN, NSA_KV_DIM), f32),
        'nsa_pe_v': 0.1 * jax.random.normal(ks[9], (DEPTH, CMP_LEN, NSA_KV_DIM), f32),
        'nsa_wc1_k': nrm(ks[10], (DEPTH, CMP_LEN * NSA_KV_DIM, CMP_HIDDEN), CMP_LEN * NSA_KV_DIM),
        'nsa_wc2_k': nrm(ks[11], (DEPTH, CMP_HIDDEN, NSA_KV_DIM), CMP_HIDDEN),
        'nsa_wc1_v': nrm(ks[12], (DEPTH, CMP_LEN * NSA_KV_DIM, CMP_HIDDEN), CMP_LEN * NSA_KV_DIM),
        'nsa_wc2_v': nrm(ks[13], (DEPTH, CMP_HIDDEN, NSA_KV_DIM), CMP_HIDDEN),
        'mem_norm': gain(ks[14], (DEPTH, D_MODEL)),
        'w_mem_kv': nrm(ks[15], (DEPTH, D_MODEL, 2 * BR_WIDTH), D_MODEL),
        'w_branch': nrm(ks[16], (DEPTH, N_BRANCH, BR_WIDTH, D_MODEL), BR_WIDTH),
        'w_out': nrm(ks[17], (DEPTH, D_MODEL, D_MODEL), D_MODEL),
        'final_norm': gain(ks[18], (D_MODEL,)),
    }


def reference(x, mem, positions, norm_g, w_in, kv_norm, w_uk, w_uv, fox_bias, nsa_pe_k, nsa_pe_v,
              nsa_wc1_k, nsa_wc2_k, nsa_wc1_v, nsa_wc2_v, mem_norm, w_mem_kv, w_branch, w_out, final_norm):
    for l in range(DEPTH):
        x = _layer(x, mem, positions, norm_g[l], w_in[l], kv_norm[l], w_uk[l], w_uv[l], fox_bias[l],
                   nsa_pe_k[l], nsa_pe_v[l], nsa_wc1_k[l], nsa_wc2_k[l], nsa_wc1_v[l], nsa_wc2_v[l],
                   mem_norm[l], w_mem_kv[l], w_branch[l], w_out[l])
    return _rmsnorm(x, final_norm)
```

```python
import math
from contextlib import ExitStack
import numpy as np
import ml_dtypes
import concourse.bass as bass
import concourse.mybir as mybir
from concourse.bass_utils import run_bass_kernel_spmd

F32 = mybir.dt.float32
BF16 = mybir.dt.bfloat16
I32 = mybir.dt.int32
U8 = mybir.dt.uint8
AF = mybir.ActivationFunctionType
ALU = mybir.AluOpType
AX = mybir.AxisListType
NPBF = ml_dtypes.bfloat16

D_MODEL = 1024
NCH = 8
EPS = 1e-6
PI = math.pi


class Prog:
    ENG = ("pe", "act", "dve", "pool", "sp")
    CH = 20000
    ND = 24

    def __init__(self, nc, stack):
        self.nc = nc
        self.stack = stack
        self.q = {e: [] for e in self.ENG}
        self.n = {e: 0 for e in self.ENG}
        self.esems = {e: [] for e in self.ENG}
        self.seen = {e: {} for e in self.ENG}
        self.lastw = {}
        self.readers = {}
        self.dsems = [stack.enter_context(nc.semaphore(f"dq{i}")) for i in range(self.ND)]
        self.dcount = [0] * self.ND
        self.dlast = [None] * self.ND
        self.dnext = 0
        self.latest = {}
        self.nsem = 0

    def _esem(self, eng, idx):
        c = (idx - 1) // self.CH
        while len(self.esems[eng]) <= c:
            self.esems[eng].append(self.stack.enter_context(self.nc.semaphore(f"s_{eng}{len(self.esems[eng])}")))
        return self.esems[eng][c], (idx - 1) % self.CH + 1

    def _deps(self, eng, reads, writes):
        toks = []
        for k in reads:
            t = self.lastw.get(k)
            if t is not None:
                toks.append(t)
        for k in writes:
            t = self.lastw.get(k)
            if t is not None:
                toks.append(t)
            for t in self.readers.get(k, {}).values():
                if t[2] != eng:
                    toks.append(t)
        return toks

    def _waits(self, eng, toks):
        need = {}
        for (sem, val, _e, sid) in toks:
            if self.seen[eng].get(sid, 0) < val:
                if sid not in need or need[sid][1] < val:
                    need[sid] = (sem, val)
        for sid, (sem, val) in need.items():
            self.seen[eng][sid] = val
        return list(need.values())

    def _commit(self, tok, reads, writes):
        for k in writes:
            self.lastw[k] = tok
            self.readers[k] = {}
        for k in reads:
            self.readers.setdefault(k, {})[tok[3]] = tok

    def op(self, eng, fn, reads=(), writes=()):
        waits = self._waits(eng, self._deps(eng, reads, writes))
        self.n[eng] += 1
        sem, val = self._esem(eng, self.n[eng])
        tok = (sem, val, eng, ("e", eng, (self.n[eng] - 1) // self.CH))
        self.latest[eng] = tok

        def emit(E, waits=waits, fn=fn, sem=sem):
            for (s, v) in waits:
                E.wait_ge(s, v)
            fn(E).then_inc(sem, 1)

        self.q[eng].append(emit)
        self._commit(tok, reads, writes)
        return tok

    def dma(self, eng, out, in_, reads=(), writes=(), **kw):
        d = self.dnext % self.ND
        self.dnext += 1
        toks = self._deps(None, reads, writes)
        if self.dlast[d] is not None:
            toks.append(self.dlast[d])
        waits = self._waits(eng, toks)
        self.dcount[d] += 16
        sem = self.dsems[d]
        tok = (sem, self.dcount[d], "dma", ("d", d))
        self.dlast[d] = tok

        def emit(E, waits=waits, sem=sem, out=out, in_=in_, kw=kw):
            for (s, v) in waits:
                E.wait_ge(s, v)
            E.dma_start(out=out, in_=in_, **kw).then_inc(sem, 16)

        self.q[eng].append(emit)
        self._commit(tok, reads, writes)
        return tok

    def coll(self, ins, outs, groups, reads=(), writes=()):
        if not hasattr(self, "ccsem"):
            self.ccsem = self.stack.enter_context(self.nc.semaphore("ccsem"))
            self.ccn = 0
            self.cclast = None
        toks = self._deps(None, reads, writes)
        if self.cclast is not None:
            toks.append(self.cclast)
        waits = self._waits("pool", toks)
        self.ccn += 1
        sem = self.ccsem
        tok = (sem, self.ccn, "cc", ("cc",))
        self.cclast = tok
        self.latest["cc"] = tok
        ins = [a.opt() for a in ins]
        outs = [a.opt() for a in outs]

        def emit(E, waits=waits, sem=sem):
            for (s_, v) in waits:
                E.wait_ge(s_, v)
            E.collective_compute("AllGather", ALU.bypass, replica_groups=groups, ins=ins, outs=outs).then_inc(sem)

        self.q["pool"].append(emit)
        self._commit(tok, reads, writes)
        return tok

    def barrier(self, engines=None):
        toks = list(self.latest.values()) + [t for t in self.dlast if t is not None]
        for eng in (engines or self.ENG):
            waits = self._waits(eng, toks)
            if waits:
                def emit(E, waits=waits):
                    for (s, v) in waits:
                        E.wait_ge(s, v)
                self.q[eng].append(emit)
        self.lastw = {}
        self.readers = {}

    def emit(self):
        nc = self.nc
        with nc.Block() as blk:
            for eng, reg in (("sp", blk.sync), ("pe", blk.tensor), ("act", blk.scalar),
                             ("dve", blk.vector), ("pool", blk.gpsimd)):
                fns = self.q[eng]
                if fns:
                    reg(lambda E, fns=fns: [f(E) for f in fns])
        self.q = {e: [] for e in self.ENG}


class Pool2:
    CNT = [0]

    def __init__(self, nc, stack):
        self.nc = nc
        self.stack = stack

    def sb(self, name, shape, dt):
        Pool2.CNT[0] += 1
        return self.stack.enter_context(self.nc.sbuf_tensor(f"{name}_{Pool2.CNT[0]}", list(shape), dt))

    def ps(self, name, shape, dt=F32):
        Pool2.CNT[0] += 1
        return self.stack.enter_context(self.nc.psum_tensor(f"{name}_{Pool2.CNT[0]}", list(shape), dt))


A_FM_GROUPS = [("ckv", 128), ("idxk", 32), ("idxk_p", 32), ("foxk0", 128), ("foxk1", 128),
               ("sbk0", 128), ("sbk1", 128), ("kcks", 128), ("kcks_p", 128), ("kw", 64), ("kw_p", 64), ("vc", 64)]
A_NFM = sum(n for _, n in A_FM_GROUPS)
A_NTM = 512 + 132
KFM_ROWS = {"dsak": 0, "foxk": 256, "sbk": 512, "kcks": 768, "kw": 896, "vc": 960, "idxk": 1024}
KFM_N = 1152


def build_A(T, ctx=None):
    nc = ctx["nc"] if ctx else bass.Bass("TRN2", target_bir_lowering=False)
    NT = T // 512
    ov = ctx["ov"] if ctx else {}
    dr = lambda name, shape, dt, kind: ov[name] if name in ov else nc.dram_tensor(name, list(shape), dt, kind=kind).ap()
    xT = dr("xT", [1024, T], F32, "ExternalInput")
    pos = dr("pos", [1, T], I32, "ExternalInput")
    gcol = dr("gcol", [128, 8], F32, "ExternalInput")
    wfm = dr("wfm", [1024, A_NFM], F32, "ExternalInput")
    wtm = dr("wtm", [1024, A_NTM], F32, "ExternalInput")
    kvg = dr("kvg", [128, 1], F32, "ExternalInput")
    wuk2 = dr("wuk2", [128, 512], F32, "ExternalInput")
    wuv = dr("wuv", [128, 256], F32, "ExternalInput")
    foxb = dr("foxb", [128, 4], F32, "ExternalInput")
    rc = dr("rc", [128, 8], F32, "ExternalInput")
    hT_o = dr("hT", [1024, T], BF16, "ExternalOutput")
    kfm_o = dr("kfm", [KFM_N, T], BF16, "ExternalOutput")
    v3_o = dr("v3", [3, T, 260], BF16, "ExternalOutput")
    v2_o = dr("v2", [2, T, 65], BF16, "ExternalOutput")
    lf_o = dr("lf", [T, 4], F32, "ExternalOutput")
    tab_o = dr("tab", [4, 128, T], F32, "ExternalOutput")

    with ExitStack() as stack:
        P = ctx["P"] if ctx else Prog(nc, stack)
        M = Pool2(nc, stack)
        ones = M.sb("ones", [128, 128], BF16)
        g_sb = M.sb("g", [128, 8], F32)
        kvg_sb = M.sb("kvg", [128, 1], F32)
        rc_sb = M.sb("rc", [128, 8], F32)
        foxb_sb = M.sb("foxb", [128, 4], F32)
        nfoxb = M.sb("nfoxb", [128, 4], F32)
        wfm_sb = M.sb("wfm", [128, 8, A_NFM], BF16)
        wtm_sb = M.sb("wtm", [128, 8, A_NTM], BF16)
        wuk_sb = M.sb("wuk", [128, 512], BF16)
        wuv_sb = M.sb("wuv", [128, 256], BF16)
        stg = M.sb("stg", [128, 8, 512], F32)
        P.op("pool", lambda E: E.memset(ones[:], 1.0), writes=["ones"])
        P.dma("sp", g_sb[:], gcol, writes=["g"])
        P.dma("sp", kvg_sb[:], kvg, writes=["kvg"])
        P.dma("sp", rc_sb[:], rc, writes=["rc"])
        P.dma("sp", foxb_sb[:], foxb, writes=["foxb"])
        P.op("dve", lambda E: E.tensor_scalar(nfoxb[:], foxb_sb[:], -1.0, None, ALU.mult), reads=["foxb"], writes=["nfoxb"])
        def load_w(dst, src, ncols, key):
            c0 = 0
            while c0 < ncols:
                n = min(512, ncols - c0)
                P.dma("sp", stg[:, :, :n], src[:, c0:c0 + n].rearrange("(c p) n -> p c n", p=128), writes=["stg"])
                P.op("dve", lambda E, c0=c0, n=n: E.tensor_copy(dst[:, :, c0:c0 + n], stg[:, :, :n]), reads=["stg"], writes=[key])
                c0 += n
        load_w(wfm_sb, wfm, A_NFM, "wfm")
        load_w(wtm_sb, wtm, A_NTM, "wtm")
        P.dma("sp", stg[:, 0, :], wuk2, writes=["stg"])
        P.op("dve", lambda E: E.tensor_copy(wuk_sb[:], stg[:, 0, :]), reads=["stg"], writes=["wuk"])
        P.dma("sp", stg[:, 0, :256], wuv, writes=["stg"])
        P.op("dve", lambda E: E.tensor_copy(wuv_sb[:], stg[:, 0, :256]), reads=["stg"], writes=["wuv"])

        x_sb = M.sb("x", [128, 8, 512], F32)
        sq = M.sb("sq", [128, 8, 512], BF16)
        rstd = M.sb("rstd", [128, 512], F32)
        h_sb = M.sb("h", [128, 8, 512], BF16)
        posi = M.sb("posi", [128, 512], I32)
        posf = M.sb("posf", [128, 512], F32)
        ang = M.sb("ang", [128, 512], F32)
        tabs = M.sb("tabs", [128, 4, 512], F32)
        ckv = M.sb("ckv", [128, 512], F32)
        ckvn = M.sb("ckvn", [128, 512], BF16)
        t1 = M.sb("t1", [128, 512], F32)
        t2 = M.sb("t2", [128, 512], F32)
        ofm = [M.sb("ofm", [128, 512], BF16) for _ in range(3)]
        v3t = [M.sb("v3t", [128, 3, 4, 65], BF16) for _ in range(2)]
        v2t = [M.sb("v2t", [128, 2, 65], BF16) for _ in range(2)]
        lft = [M.sb("lft", [128, 4], F32) for _ in range(2)]
        lfe = M.sb("lfe", [128, 4], F32)
        pa = ctx["PS"][:6] if ctx else [M.ps("pa", [128, 512]) for _ in range(6)]
        pk = ["PS%d" % i for i in range(6)]
        for i in range(2):
            P.op("pool", lambda E, i=i: E.memset(v3t[i][:], 1.0), writes=["v3t%d" % i])
            P.op("pool", lambda E, i=i: E.memset(v2t[i][:], 1.0), writes=["v2t%d" % i])

        fm_off = {}
        o = 0
        for nme, n in A_FM_GROUPS:
            fm_off[nme] = (o, n)
            o += n
        ofm_i = [0]
        pa_i = [0]

        def next_pa():
            i = pa_i[0] % 6
            pa_i[0] += 1
            return pa[i], pk[i]

        def next_ofm():
            i = ofm_i[0] % 3
            ofm_i[0] += 1
            return ofm[i], "ofm%d" % i

        def fm_proj(grp):
            off, n = fm_off[grp]
            ps, key = next_pa()
            for c in range(8):
                P.op("pe", lambda E, c=c: E.matmul(ps[:n, :], lhsT=wfm_sb[:, c, off:off + n], rhs=h_sb[:, c, :],
                                                   start=(c == 0), stop=(c == 7)),
                     reads=["wfm", "h"], writes=[key])
            return ps, key

        def rope_out(grp, grp_p, n, tc, ts, row0, tt):
            ps1, k1 = fm_proj(grp)
            ps2, k2 = fm_proj(grp_p)
            rope_combine(ps1, k1, ps2, k2, n, tc, ts, row0, tt)

        def rope_combine(ps1, k1, ps2, k2, n, tc, ts, row0, tt):
            ot, ok = next_ofm()
            P.op("dve", lambda E: E.tensor_tensor(t1[:n, :], ps1[:n, :], tabs[:n, tc, :], ALU.mult), reads=[k1, "tabs"], writes=["t1"])
            P.op("dve", lambda E: E.tensor_tensor(t2[:n, :], ps2[:n, :], tabs[:n, ts, :], ALU.mult), reads=[k2, "tabs"], writes=["t2"])
            P.op("dve", lambda E: E.tensor_tensor(ot[:n, :], t1[:n, :], t2[:n, :], ALU.add), reads=["t1", "t2"], writes=[ok])
            P.dma("sp", kfm_o[row0:row0 + n, tt * 512:(tt + 1) * 512], ot[:n, :], reads=[ok])

        def plain_out(grp, n, row0, tt, eng="act"):
            ps, k = fm_proj(grp)
            ot, ok = next_ofm()
            if eng == "act":
                P.op("act", lambda E: E.copy(ot[:n, :], ps[:n, :]), reads=[k], writes=[ok])
            else:
                P.op("dve", lambda E: E.tensor_copy(ot[:n, :], ps[:n, :]), reads=[k], writes=[ok])
            P.dma("sp", kfm_o[row0:row0 + n, tt * 512:(tt + 1) * 512], ot[:n, :], reads=[ok])

        def do_tile(tt):
            tsl = slice(tt * 512, (tt + 1) * 512)
            P.dma("sp", x_sb[:], xT[:, tsl].rearrange("(c p) t -> p c t", p=128), writes=["x"])
            P.op("act", lambda E: E.activation(sq[:], x_sb[:], AF.Square), reads=["x"], writes=["sq"])
            ps, key = next_pa()
            for c in range(8):
                P.op("pe", lambda E, c=c, ps=ps: E.matmul(ps[:], lhsT=ones[:], rhs=sq[:, c, :], start=(c == 0), stop=(c == 7)),
                     reads=["ones", "sq"], writes=[key])
            P.op("act", lambda E, ps=ps: E.activation(rstd[:], ps[:], AF.Ln, bias=EPS, scale=1.0 / 1024), reads=[key], writes=["rstd"])
            P.op("act", lambda E: E.activation(rstd[:], rstd[:], AF.Exp, scale=-0.5), reads=["rstd"], writes=["rstd"])
            for c in range(8):
                P.op("dve", lambda E, c=c: E.scalar_tensor_tensor(h_sb[:, c, :], x_sb[:, c, :], g_sb[:, c:c + 1], rstd[:],
                                                                  ALU.mult, ALU.mult), reads=["x", "g", "rstd"], writes=["h"])
            P.dma("sp", hT_o[:, tsl].rearrange("(c p) t -> p c t", p=128), h_sb[:], reads=["h"])
            P.dma("sp", posi[:], pos[:, tsl].partition_broadcast(128), writes=["posi"])
            P.op("dve", lambda E: E.tensor_copy(posf[:], posi[:]), reads=["posi"], writes=["posf"])
            MAGIC = 12582912.0
            for (ci, ti) in ((0, 0), (3, 2)):
                for (which, shift) in ((0, 0.5 * PI), (1, 0.0)):
                    P.op("dve", lambda E, ci=ci, shift=shift: E.tensor_scalar(ang[:], posf[:], rc_sb[:, ci:ci + 1], shift, ALU.mult, ALU.add),
                         reads=["posf", "rc"], writes=["ang"])
                    P.op("dve", lambda E: E.tensor_scalar(t1[:], ang[:], 1.0 / (2 * PI), MAGIC, ALU.mult, ALU.add), reads=["ang"], writes=["t1"])
                    P.op("dve", lambda E: E.tensor_scalar(t1[:], t1[:], MAGIC, -2 * PI, ALU.subtract, ALU.mult), reads=["t1"], writes=["t1"])
                    P.op("dve", lambda E: E.tensor_tensor(ang[:], ang[:], t1[:], ALU.add), reads=["ang", "t1"], writes=["ang"])
                    P.op("dve", lambda E: E.tensor_scalar(ang[:], ang[:], PI, -PI, ALU.min, ALU.max), reads=["ang"], writes=["ang"])
                    if which == 0:
                        P.op("act", lambda E, ti=ti: E.activation(tabs[:, ti, :], ang[:], AF.Sin), reads=["ang"], writes=["tabs"])
                    else:
                        P.op("act", lambda E, ci=ci, ti=ti: E.activation(tabs[:, ti + 1, :], ang[:], AF.Sin, scale=rc_sb[:, ci + 1:ci + 2]),
                             reads=["ang", "rc"], writes=["tabs"])
            P.dma("sp", tab_o[:, :, tsl].rearrange("k p t -> p k t"), tabs[:], reads=["tabs"])
            ps, key = fm_proj("ckv")
            P.op("act", lambda E, ps=ps: E.copy(ckv[:], ps[:]), reads=[key], writes=["ckv"])
            P.op("act", lambda E: E.activation(sq[:, 0, :], ckv[:], AF.Square), reads=["ckv"], writes=["sq"])
            ps2, key2 = next_pa()
            P.op("pe", lambda E, ps2=ps2: E.matmul(ps2[:], lhsT=ones[:], rhs=sq[:, 0, :], start=True, stop=True), reads=["ones", "sq"], writes=[key2])
            P.op("act", lambda E, ps2=ps2: E.activation(t1[:], ps2[:], AF.Ln, bias=EPS, scale=1.0 / 128), reads=[key2], writes=["t1"])
            P.op("act", lambda E: E.activation(t1[:], t1[:], AF.Exp, scale=-0.5), reads=["t1"], writes=["t1"])
            P.op("dve", lambda E: E.scalar_tensor_tensor(ckvn[:], ckv[:], kvg_sb[:, 0:1], t1[:], ALU.mult, ALU.mult),
                 reads=["ckv", "kvg", "t1"], writes=["ckvn"])
            for hp in range(2):
                psa, ka = next_pa()
                psb, kb = next_pa()
                P.op("pe", lambda E, hp=hp, psa=psa: E.matmul(psa[:], lhsT=wuk_sb[:, hp * 128:(hp + 1) * 128], rhs=ckvn[:], start=True, stop=True),
                     reads=["wuk", "ckvn"], writes=[ka])
                P.op("pe", lambda E, hp=hp, psb=psb: E.matmul(psb[:], lhsT=wuk_sb[:, 256 + hp * 128:256 + (hp + 1) * 128], rhs=ckvn[:], start=True, stop=True),
                     reads=["wuk", "ckvn"], writes=[kb])
                rope_combine(psa, ka, psb, kb, 128, 0, 1, KFM_ROWS["dsak"] + hp * 128, tt)
            rope_out("idxk", "idxk_p", 32, 2, 3, KFM_ROWS["idxk"], tt)
            plain_out("foxk0", 128, KFM_ROWS["foxk"], tt, "act")
            plain_out("foxk1", 128, KFM_ROWS["foxk"] + 128, tt, "dve")
            plain_out("sbk0", 128, KFM_ROWS["sbk"], tt, "act")
            plain_out("sbk1", 128, KFM_ROWS["sbk"] + 128, tt, "dve")
            rope_out("kcks", "kcks_p", 128, 0, 1, KFM_ROWS["kcks"], tt)
            rope_out("kw", "kw_p", 64, 0, 1, KFM_ROWS["kw"], tt)
            plain_out("vc", 64, KFM_ROWS["vc"], tt, "act")
            for st in range(4):
                tm_part(tt, st)

        def tm_part(tt, st):
            if True:
                tok0 = tt * 512 + st * 128
                bi = (tt * 4 + st) % 2
                v3, v3k = v3t[bi], "v3t%d" % bi
                v2, v2k = v2t[bi], "v2t%d" % bi
                lf, lfk = lft[bi], "lft%d" % bi
                hs = slice(st * 128, (st + 1) * 128)
                ps_v, kv = next_pa()
                P.op("pe", lambda E, ps_v=ps_v: E.matmul(ps_v[:, :256], lhsT=ckvn[:, hs], rhs=wuv_sb[:], start=True, stop=True),
                     reads=["ckvn", "wuv"], writes=[kv])
                P.op("act", lambda E, ps_v=ps_v, v3=v3: E.copy(v3[:, 0, :, 0:64], ps_v[:, :256].rearrange("p (h d) -> p h d", d=64)),
                     reads=[kv], writes=[v3k])
                ps_a, kaa = next_pa()
                for c in range(8):
                    P.op("pe", lambda E, c=c, ps_a=ps_a: E.matmul(ps_a[:], lhsT=h_sb[:, c, hs], rhs=wtm_sb[:, c, 0:512], start=(c == 0), stop=(c == 7)),
                         reads=["h", "wtm"], writes=[kaa])
                P.op("dve", lambda E, ps_a=ps_a, v3=v3: E.tensor_copy(v3[:, 1:3, :, 0:64], ps_a[:].rearrange("p (b h d) -> p b h d", b=2, d=64)),
                     reads=[kaa], writes=[v3k])
                ps_b, kbb = next_pa()
                for c in range(8):
                    P.op("pe", lambda E, c=c, ps_b=ps_b: E.matmul(ps_b[:, :132], lhsT=h_sb[:, c, hs], rhs=wtm_sb[:, c, 512:644], start=(c == 0), stop=(c == 7)),
                         reads=["h", "wtm"], writes=[kbb])
                P.op("act", lambda E, ps_b=ps_b, v2=v2: E.copy(v2[:, :, 0:64], ps_b[:, :128].rearrange("p (b d) -> p b d", d=64)),
                     reads=[kbb], writes=[v2k])
                P.op("dve", lambda E, ps_b=ps_b: E.tensor_tensor(lfe[:], ps_b[:, 128:132], nfoxb[:], ALU.subtract), reads=[kbb, "nfoxb"], writes=["lfe"])
                P.op("act", lambda E: E.activation(lfe[:], lfe[:], AF.Exp, scale=-1.0), reads=["lfe"], writes=["lfe"])
                P.op("act", lambda E, lf=lf: E.activation(lf[:], lfe[:], AF.Ln, bias=1.0), reads=["lfe"], writes=[lfk])
                P.dma("sp", v3_o[:, tok0:tok0 + 128, :].rearrange("b t n -> t b n"), v3[:].rearrange("p b h d -> p b (h d)"), reads=[v3k])
                P.dma("sp", v2_o[:, tok0:tok0 + 128, :].rearrange("b t n -> t b n"), v2[:], reads=[v2k])
                P.dma("sp", lf_o[tok0:tok0 + 128, :], lf[:], reads=[lfk])
        for tt in range(NT):
            do_tile(tt)
        P.barrier()
        P.emit()
    return nc


IN_LAYOUT = (
    ('dsa_q', 256), ('dsa_ckv', 128), ('idx_q', 128), ('idx_k', 32), ('idx_w', 4), ('dsa_z', 256),
    ('fox_q', 256), ('fox_k', 256), ('fox_v', 256), ('fox_f', 4), ('fox_z', 256),
    ('sb_q', 256), ('sb_k', 256), ('sb_v', 256), ('sb_z', 256),
    ('nsa_q', 256), ('nsa_kc', 64), ('nsa_vc', 64), ('nsa_ks', 64), ('nsa_vs', 64),
    ('nsa_kw', 64), ('nsa_vw', 64), ('nsa_g', 12), ('nsa_z', 256),
    ('mem_q', 256), ('mem_z', 256),
    ('merge', 5 * 1024),
)
COL = {}
_o = 0
for _n, _w in IN_LAYOUT:
    COL[_n] = (_o, _w)
    _o += _w
N_IN = _o


def _cols(w_in, name):
    o, n = COL[name]
    return w_in[:, o:o + n]


def _perm_idx(ncols, dh):
    idx = np.arange(ncols)
    base = (idx // dh) * dh
    return base + (idx % dh + dh // 2) % dh


def _rope_consts():
    p = np.arange(128)
    rc = np.zeros((128, 8), np.float32)
    rc[:, 0] = 10000.0 ** (-(2.0 * (p % 32)) / 64.0)
    s64 = np.where((p % 64) < 32, -1.0, 1.0)
    rc[:, 1] = s64
    rc[:, 2] = -PI * s64
    rc[:, 3] = 10000.0 ** (-(2.0 * (p % 16)) / 32.0)
    s32 = np.where((p % 32) < 16, -1.0, 1.0)
    rc[:, 4] = s32
    rc[:, 5] = -PI * s32
    return rc


def host_prep_A(norm_g, w_in, kv_norm, w_uk, w_uv, fox_bias):
    c = lambda n: _cols(w_in, n)
    kcks = np.concatenate([c('nsa_kc'), c('nsa_ks')], 1)
    wfm = np.concatenate([
        c('dsa_ckv'), c('idx_k'), c('idx_k')[:, _perm_idx(32, 32)],
        c('fox_k'), c('sb_k'), kcks, kcks[:, _perm_idx(128, 64)],
        c('nsa_kw'), c('nsa_kw')[:, _perm_idx(64, 64)], c('nsa_vc')], 1)
    wtm = np.concatenate([c('fox_v'), c('sb_v'), c('nsa_vs'), c('nsa_vw'), c('fox_f')], 1)
    assert wfm.shape[1] == A_NFM and wtm.shape[1] == A_NTM
    return {
        "gcol": np.ascontiguousarray(norm_g.reshape(8, 128).T),
        "wfm": np.ascontiguousarray(wfm), "wtm": np.ascontiguousarray(wtm),
        "kvg": np.ascontiguousarray(kv_norm.reshape(128, 1)),
        "wuk2": np.ascontiguousarray(np.concatenate([w_uk, w_uk[:, _perm_idx(256, 64)]], 1)),
        "wuv": np.ascontiguousarray(w_uv),
        "foxb": np.ascontiguousarray(np.broadcast_to(fox_bias.reshape(1, 4), (128, 4))),
        "rc": _rope_consts(),
    }


B_WQ = {"dsa": (0, 512), "idx": (512, 256), "fox": (768, 256), "sb": (1024, 256), "nsa": (1280, 512), "mem": (1792, 256)}
B_NWQ = 2048
NEG_BIG = -1.0e30
BIS_R0 = 512.0
BIS_K = 24


def build_B(T, ctx=None):
    nc = ctx["nc"] if ctx else bass.Bass("TRN2", target_bir_lowering=False)
    ov = ctx["ov"] if ctx else {}
    G = bool(ctx)
    FINAL = ctx["final"] if ctx else True
    S = 4 * T
    NB = S // 128
    NQ = T // 128
    NG = T // 512
    NBS = S // 64
    NCMP = S // 16 - 1
    NCC = (NCMP + 127) // 128
    NCP = NCC * 128
    KSEL = min(256, S // 4)
    SCALE = 0.125
    dr = lambda name, shape, dt, kind="ExternalInput": ov[name] if name in ov else nc.dram_tensor(name, list(shape), dt, kind=kind).ap()
    xT = dr("xT", [1024, T], F32)
    hT = dr("hT", [1024, T], BF16)
    tab = dr("tab", [4, 128, T], F32)
    kfm = dr("kfm", [KFM_N, S], BF16)
    v3 = dr("v3", [3, S, 260], BF16)
    v2 = dr("v2", [2, S, 65], BF16)
    lf = dr("lf", [128, NB * 4], F32)
    wq = dr("wq", [1024, B_NWQ], F32)
    wsm = dr("wsm", [1024, 16], F32)
    wz = dr("wz", [1024, 1280], F32)
    wmg = dr("wmg", [1024, 5120], F32)
    wbr = dr("wbr", [5, 256, 1024], F32)
    wout = dr("wout", [1024, 1024], F32)
    w1 = dr("w1", [128, 32 * 128], F32)
    w2k2 = dr("w2k2", [128, 128], F32)
    w2v = dr("w2v", [128, 64], F32)
    peT = dr("peT", [128, 32], F32)
    memT = dr("memT", [1024, 256], F32)
    mgcol = dr("mgcol", [128, 8], F32)
    wmem = dr("wmem", [1024, 512], F32)
    fgcol = dr("fgcol", [128, 8], F32)
    c_bf = dr("c_bf", [128, 128 * 3 + 512 * 2 + 1024 + 512], BF16)
    c_f = dr("c_f", [128, 128 * 3 + 512 + 4 + 4 + NQ + NCC + NCP], F32)
    trow = dr("trow", [1, T], F32)
    visd = dr("vis", [128, NQ * NBS], F32)
    addd = dr("addc", [128, NQ * NBS], F32)
    xo_o = dr("xo", [1024, T], F32, "ExternalOutput")
    xn_o = dr("xn", [1024, T], F32, "ExternalOutput")
    ydram = dr("ydram", [NQ, 128, 1280], BF16, "ExternalOutput")

    with ExitStack() as stack:
        P = ctx["P"] if ctx else Prog(nc, stack)
        M = Pool2(nc, stack)
        PS = ctx["PS"] if ctx else [M.ps("ps%d" % i, [128, 512]) for i in range(8)]
        PK = ["PS%d" % i for i in range(8)]

        def pe(out, lhsT, rhs, st, sp, r, w):
            P.op("pe", lambda E: E.matmul(out, lhsT=lhsT, rhs=rhs, start=st, stop=sp), reads=r, writes=w)

        def tr(out, in_, r, w, f32=False):
            idn = identF if f32 else ident
            P.op("pe", lambda E: E.matmul(out, lhsT=in_, rhs=idn, start=True, stop=True), reads=list(r) + ["cbf", "cf"], writes=w)

        def act(out, in_, func, r, w, **kw):
            P.op("act", lambda E: E.activation(out, in_, func, **kw), reads=r, writes=w)

        def tt(eng, out, a, b, op, r, w):
            P.op(eng, lambda E: E.tensor_tensor(out, a, b, op), reads=r, writes=w)

        def ts(eng, out, a, s1, s2, op0, op1, r, w, accum=None):
            if accum is None:
                if op1 is None:
                    P.op(eng, lambda E: E.tensor_scalar(out, a, s1, s2, op0), reads=r, writes=w)
                else:
                    P.op(eng, lambda E: E.tensor_scalar(out, a, s1, s2, op0, op1), reads=r, writes=w)
            else:
                P.op(eng, lambda E: E.tensor_scalar(out, a, s1, s2, op0, op1, accum_out=accum), reads=r, writes=w)

        def stt(eng, out, a, s, b, op0, op1, r, w, accum=None):
            if accum is None:
                P.op(eng, lambda E: E.scalar_tensor_tensor(out, a, s, b, op0, op1), reads=r, writes=w)
            else:
                P.op(eng, lambda E: E.scalar_tensor_tensor(out, a, s, b, op0, op1, accum_out=accum), reads=r, writes=w)

        def cp(eng, out, in_, r, w):
            if eng == "act":
                P.op("act", lambda E: E.copy(out, in_), reads=r, writes=w)
            else:
                P.op(eng, lambda E: E.tensor_copy(out, in_), reads=r, writes=w)

        def mset(out, val, w, eng="pool"):
            P.op(eng, lambda E: E.memset(out, val), writes=w)

        cbf = M.sb("cbf", [128, 128 * 3 + 512 * 2 + 1024 + 512], BF16)
        cf = M.sb("cf", [128, 128 * 3 + 512 + 4 + 4 + NQ + NCC + NCP], F32)
        P.dma("sp", cbf[:], c_bf, writes=["cbf"])
        P.dma("sp", cf[:], c_f, writes=["cf"])
        ident = cbf[:, 0:128]
        triS = cbf[:, 128:256]
        ones_b = cbf[:, 256:384]
        dmask = cbf[:, 384:896].rearrange("p (j q) -> p j q", q=128)
        dmaskS = cbf[:, 896:1408].rearrange("p (j q) -> p j q", q=128)
        wmask = cbf[:, 1408:2432].rearrange("p (j q) -> p j q", q=128)
        dmaskN01 = cbf[:, 2432:2944]
        o = 0
        triInc = cf[:, o:o + 128]; o += 128
        ones_f = cf[:, o:o + 128]; o += 128
        identF = cf[:, o:o + 128]; o += 128
        dmaskN = cf[:, o:o + 512]; o += 512
        oh = cf[:, o:o + 4]; o += 4
        sel4 = cf[:, o:o + 4]; o += 4
        tq = cf[:, o:o + NQ]; o += NQ
        ncol = cf[:, o:o + NCC]; o += NCC
        nrow = cf[:, o:o + NCP]; o += NCP
        stg = M.sb("stg", [128, 8, 256], F32)
        cumcol = M.sb("cumcol", [128, 4, NB], F32)
        CPt = M.sb("CPt", [128, 4, NQ], F32)
        kc2T = M.sb("kc2T", [128, NCP], BF16)
        vc1 = M.sb("vc1", [128, NCC, 65], BF16)
        mkT = M.sb("mkT", [128, 2, 256], BF16)
        mv1 = M.sb("mv1", [128, 2, 4, 65], BF16)
        mset(vc1[:], 1.0, ["vc1"])
        mset(mv1[:], 1.0, ["mv1"])

        def ld_kfm(dst, row0, nrows, dkey):
            if not G:
                P.dma("sp", dst, kfm[row0:row0 + nrows, :], writes=[dkey])
            else:
                dv = dst.rearrange("p (i r q) -> p i r q", r=4, q=128)
                ch, off = row0 // 128, row0 % 128
                for r in range(4):
                    P.dma("sp", dv[:, :, r, :], kfm[ch][r * 128 + off:r * 128 + off + nrows, :].rearrange("p (i q) -> p i q", q=128),
                          writes=[dkey])

        def ld_v(dst, src, k, nk, dkey):
            if not G:
                P.dma("sp", dst, src[k].rearrange("(j s) n -> s j n", s=128), writes=[dkey])
            else:
                dv = dst.rearrange("s (i r) n -> s i r n", r=4)
                hq_ = NQ // 2
                for hf in range(2):
                    for r in range(4):
                        P.dma("sp", dv[:, hf * hq_:(hf + 1) * hq_, r, :],
                              src[k][hf][r * (T // 2):(r + 1) * (T // 2), :].rearrange("(i s) n -> s i n", s=128), writes=[dkey])

        def load_w(dst, dkey, src_cols, ncols, kchunks=8):
            c0 = 0
            while c0 < ncols:
                n = min(256, ncols - c0)
                P.dma("sp", stg[:, :kchunks, :n], src_cols(c0, n).rearrange("(c p) n -> p c n", p=128), writes=["stg"])
                P.op("dve", lambda E, c0=c0, n=n: E.tensor_copy(dst[:, :, c0:c0 + n], stg[:, :kchunks, :n]), reads=["stg"], writes=[dkey])
                c0 += n

        def load_small(dst, dkey, src, n):
            P.dma("sp", stg[:, 0, :n], src, writes=["stg"])
            P.op("dve", lambda E: E.tensor_copy(dst, stg[:, 0, :n]), reads=["stg"], writes=[dkey])

        with ExitStack() as st0:
            M0 = Pool2(nc, st0)
            lf_sb = M0.sb("lf", [128, NB * 4], F32)
            tot = M0.sb("tot", [128, NB * 4], F32)
            incl = M0.sb("incl", [128, 4, NB], F32)
            tmpc = M0.sb("tmpc", [128, 4, NB], F32)
            tmp4 = M0.sb("tmp4", [128, NQ, 4], F32)
            if not G:
                P.dma("sp", lf_sb[:], lf, writes=["lf"])
            else:
                lv = lf_sb[:].rearrange("s (i r h) -> s i r h", r=4, h=4)
                for r in range(4):
                    P.dma("sp", lv[:, :, r, :], lf[r * T:(r + 1) * T, :].rearrange("(i s) h -> s i h", s=128), writes=["lf"])
            pe(PS[4][:, :NB * 4], triInc, lf_sb[:], True, True, ["cf", "lf"], [PK[4]])
            pe(PS[6][:, :NB * 4], ones_f, lf_sb[:], True, True, ["cf", "lf"], [PK[6]])
            cp("act", tot[:], PS[6][:, :NB * 4], [PK[6]], ["tot"])
            tot3 = tot[:].rearrange("p (j h) -> p j h", h=4)
            cs3 = PS[4][:, :NB * 4].rearrange("p (j h) -> p j h", h=4)
            for h in range(4):
                P.op("dve", lambda E, h=h: E.tensor_tensor_scan(incl[:, h, :], ones_f[:, :NB], tot3[:, :, h], 0.0, ALU.mult, ALU.add),
                     reads=["cf", "tot"], writes=["incl"])
                tt("dve", tmpc[:, h, :], incl[:, h, :], tot3[:, :, h], ALU.subtract, ["incl", "tot"], ["tmpc"])
                tt("dve", cumcol[:, h, :], cs3[:, :, h], tmpc[:, h, :], ALU.add, [PK[4], "tmpc"], ["cumcol"])
                tt("dve", tmp4[:], incl[:, h, :].rearrange("p (i j) -> p i j", j=4), oh.unsqueeze(1).to_broadcast([128, NQ, 4]),
                   ALU.mult, ["incl", "cf"], ["tmp4"])
                P.op("dve", lambda E, h=h: E.tensor_reduce(CPt[:, h, :], tmp4[:], AX.X, ALU.add), reads=["tmp4"], writes=["CPt"])
            kvtok = M0.sb("kvtok", [128, S], BF16)
            W1 = M0.sb("W1", [128, 32, 128], BF16)
            w2k2_sb = M0.sb("w2k2", [128, 128], BF16)
            w2v_sb = M0.sb("w2v", [128, 64], BF16)
            peT_sb = M0.sb("peT", [128, 32], BF16)
            bH = M0.sb("bH", [128, 2], F32)
            hk = M0.sb("hk", [128, NCP], BF16)
            hv = M0.sb("hv", [128, NCP], BF16)
            ld_kfm(kvtok[0:64, :], KFM_ROWS["kcks"], 64, "kvtok")
            ld_kfm(kvtok[64:128, :], KFM_ROWS["vc"], 64, "kvtok")
            stg2 = stg[:].rearrange("p c n -> p (c n)")
            for half in range(2):
                P.dma("sp", stg2, w1[:, half * 2048:(half + 1) * 2048], writes=["stg"])
                P.op("dve", lambda E, half=half: E.tensor_copy(W1[:, half * 16:(half + 1) * 16, :].rearrange("p l h -> p (l h)"), stg2),
                     reads=["stg"], writes=["W1"])
            load_small(w2k2_sb[:], "w2k2", w2k2, 128)
            load_small(w2v_sb[:], "w2v", w2v, 64)
            load_small(peT_sb[:], "peT", peT, 32)
            mset(hk[:], 0.0, ["hk"])
            mset(hv[:], 0.0, ["hv"])
            for kv_i, (lo, hbuf, hkey) in enumerate(((0, hk, "hk"), (64, hv, "hv"))):
                for l in range(32):
                    pe(PS[4][:, kv_i:kv_i + 1], W1[lo:lo + 64, l, :], peT_sb[lo:lo + 64, l:l + 1], l == 0, l == 31, ["W1", "peT"], [PK[4]])
            cp("act", bH[:], PS[4][:, 0:2], [PK[4]], ["bH"])
            for kv_i, (lo, hbuf, hkey) in enumerate(((0, hk, "hk"), (64, hv, "hv"))):
                for l in range(32):
                    pe(PS[6][:, :NCMP], W1[lo:lo + 64, l, :], kvtok[lo:lo + 64, l:l + 16 * (NCMP - 1) + 1:16], l == 0, l == 31,
                       ["W1", "kvtok"], [PK[6]])
                act(hbuf[:, :NCMP], PS[6][:, :NCMP], AF.Silu, [PK[6], "bH"], [hkey], bias=bH[:, kv_i:kv_i + 1])
            pe(PS[4][:, :NCP], w2k2_sb[:], hk[:], True, True, ["w2k2", "hk"], [PK[4]])
            cp("act", kc2T[:], PS[4][:, :NCP], [PK[4]], ["kc2T"])
            for c in range(NCC):
                pe(PS[6][:, c * 64:(c + 1) * 64], hv[:, c * 128:(c + 1) * 128], w2v_sb[:], True, True, ["hv", "w2v"], [PK[6]])
            cp("act", vc1[:, :, 0:64], PS[6][:, :NCC * 64].rearrange("p (c d) -> p c d", d=64), [PK[6]], ["vc1"])
            xm = M0.sb("xm", [128, 8, 256], F32)
            sqm = M0.sb("sqm", [128, 8, 256], BF16)
            mh = M0.sb("mh", [128, 8, 256], BF16)
            rsm = M0.sb("rsm", [128, 256], F32)
            mg_sb = M0.sb("mg", [128, 8], F32)
            wmem_sb = M0.sb("wmem", [128, 8, 512], BF16)
            P.dma("sp", xm[:], memT.rearrange("(c p) t -> p c t", p=128), writes=["xm"])
            P.dma("sp", mg_sb[:], mgcol, writes=["mg"])
            load_w(wmem_sb, "wmem", lambda c0, n: wmem[:, c0:c0 + n], 512)
            act(sqm[:], xm[:], AF.Square, ["xm"], ["sqm"])
            for c in range(8):
                pe(PS[4][:, :256], ones_b, sqm[:, c, :], c == 0, c == 7, ["cbf", "sqm"], [PK[4]])
            act(rsm[:], PS[4][:, :256], AF.Ln, [PK[4]], ["rsm"], bias=EPS, scale=1.0 / 1024)
            act(rsm[:], rsm[:], AF.Exp, ["rsm"], ["rsm"], scale=-0.5)
            for c in range(8):
                stt("dve", mh[:, c, :], xm[:, c, :], mg_sb[:, c:c + 1], rsm[:], ALU.mult, ALU.mult, ["xm", "mg", "rsm"], ["mh"])
            for hp in range(2):
                for c in range(8):
                    pe(PS[6][:, :256], wmem_sb[:, c, hp * 128:(hp + 1) * 128], mh[:, c, :], c == 0, c == 7, ["wmem", "mh"], [PK[6]])
                cp("act", mkT[:, hp, :], PS[6][:, :256], [PK[6]], ["mkT"])
            for mc in range(2):
                for c in range(8):
                    pe(PS[4][:, :256], mh[:, c, mc * 128:(mc + 1) * 128], wmem_sb[:, c, 256:512], c == 0, c == 7, ["mh", "wmem"], [PK[4]])
                cp("act", mv1[:, mc, :, 0:64], PS[4][:, :256].rearrange("p (h d) -> p h d", d=64), [PK[4]], ["mv1"])
            P.barrier()
            P.emit()

        with ExitStack() as st1:
            M1 = Pool2(nc, st1)
            KT = M1.sb("KT", [128, 2, S], BF16)
            V1 = M1.sb("V1", [128, NB, 260], BF16)
            kiT4 = M1.sb("kiT4", [128, S], BF16)
            sc = M1.sb("sc", [128, S], F32)
            maskT = M1.sb("maskT", [128, NB, 128], BF16)
            junk = maskT[:].rearrange("p j q -> p (j q)")
            wqb = M1.sb("wqb", [128, 8, 512], BF16)
            wsm_sb = M1.sb("wsm", [128, 8, 16], BF16)
            wqb2 = M1.sb("wqb2", [128, 8, 256], BF16)
            hq = M1.sb("hq", [128, 8, 512], BF16)
            tabg = M1.sb("tabg", [128, 2, 512], F32)
            QT = M1.sb("QT", [128, 2, 512], BF16)
            qiT = M1.sb("qiT", [128, 512], BF16)
            qm = M1.sb("qm", [128, 4, 512], BF16)
            t1 = M1.sb("t1", [128, 512], F32)
            t2 = M1.sb("t2", [128, 512], F32)
            PT = [M1.sb("PT", [128, 4, 128], BF16) for _ in range(2)]
            ebuf = M1.sb("ebuf", [128, 512], F32)
            ubuf = M1.sb("ubuf", [128, 4, 128], BF16)
            Rt = M1.sb("Rt", [128, 128], F32)
            Bih = [M1.sb("Bih", [128, NB], F32) for _ in range(2)]
            rd = M1.sb("rd", [128, 4], F32)
            ybuf = [M1.sb("ybuf", [128, 4, 64], BF16) for _ in range(2)]
            rbuf = [M1.sb("rbuf", [128, 512], F32) for _ in range(2)]
            wsmall = M1.sb("wsmall", [128, 16], F32)
            g12 = M1.sb("g12", [128, 12], F32)
            mid = M1.sb("mid", [128, 1], F32)
            cnt = M1.sb("cnt", [128, 1], F32)
            tmpb = M1.sb("tmpb", [128, 1], F32)
            ec = t1[:, :NCP]
            impacc = t2[:, :NCP]
            cmaskN = ebuf[:, :NCP]
            cmaskT = M1.sb("cmaskT", [128, NCC, 128], BF16)
            eT = M1.sb("eT", [128, NCC, 128], BF16)
            rs4 = M1.sb("rs4", [128, 4], F32)
            imp4 = M1.sb("imp4", [128, NBS], F32)
            imp2 = M1.sb("imp2", [128, NBS], F32)
            m8a = M1.sb("m8a", [128, 8], F32)
            m8b = M1.sb("m8b", [128, 8], F32)
            bm = M1.sb("bm", [128, NBS], F32)
            vis_t = M1.sb("vis_t", [128, NBS], F32)
            add_t = M1.sb("add_t", [128, NBS], F32)
            trow_t = Rt
            yacc = M1.sb("yacc", [128, 4, 64], F32)
            ytmp = M1.sb("ytmp", [128, 4, 64], F32)
            coef = M1.sb("coef", [128, 4], F32)
            ctr = {"ps": 0, "pt": 0, "po": 0, "y": 0, "b": 0, "r": 0}

            def nxt(name, n):
                v = ctr[name] % n
                ctr[name] += 1
                return v

            def load_hq(g):
                P.dma("sp", hq[:], hT[:, g * 512:(g + 1) * 512].rearrange("(c p) t -> p c t", p=128), writes=["hq"])

            def fm_q(ps_i, col0, wt=None, wkey="wqb"):
                wt = wqb if wt is None else wt
                for c in range(8):
                    pe(PS[ps_i][:], wt[:, c, col0:col0 + 128], hq[:, c, :], c == 0, c == 7, [wkey, "hq"], [PK[ps_i]])

            def load_tabs(g, k0):
                P.dma("sp", tabg[:], tab[k0:k0 + 2, :, g * 512:(g + 1) * 512].rearrange("k p t -> p k t"), writes=["tabg"])

            def make_QT(g, rope):
                if rope:
                    load_tabs(g, 0)
                for hp in range(2):
                    fm_q(4, hp * 128)
                    if rope:
                        fm_q(5, 256 + hp * 128)
                        tt("dve", t1[:], PS[4][:], tabg[:, 0, :], ALU.mult, [PK[4], "tabg"], ["t1"])
                        tt("dve", t2[:], PS[5][:], tabg[:, 1, :], ALU.mult, [PK[5], "tabg"], ["t2"])
                        tt("dve", QT[:, hp, :], t1[:], t2[:], ALU.add, ["t1", "t2"], ["QT"])
                    else:
                        cp("act", QT[:, hp, :], PS[4][:], [PK[4]], ["QT"])

            def qslice(h, qi):
                lo = (h % 2) * 64
                return QT[lo:lo + 64, h // 2, qi * 128:(qi + 1) * 128]

            def finalize(po_i, i, bidx):
                po3 = PS[po_i][:, :260].rearrange("p (h d) -> p h d", d=65)
                yb = nxt("y", 2)
                ts("dve", rd[:], po3[:, :, 64], 1e-30, None, ALU.max, None, [PK[po_i]], ["rd"])
                P.op("dve", lambda E: E.reciprocal(rd[:], rd[:]), reads=["rd"], writes=["rd"])
                tt("dve", ybuf[yb][:], po3[:, :, 0:64], rd[:].unsqueeze(2).to_broadcast([128, 4, 64]), ALU.mult,
                   [PK[po_i], "rd"], ["ybuf%d" % yb])
                P.dma("sp", ydram[i, :, bidx * 256:(bidx + 1) * 256], ybuf[yb][:].rearrange("p h d -> p (h d)"), reads=["ybuf%d" % yb], writes=["ydram"])

            def load_branch_kv(krow0, vidx):
                for hp in range(2):
                    ld_kfm(KT[:, hp, :], krow0 + hp * 128, 128, "KT")
                ld_v(V1[:], v3, vidx, 3, "V1")

            def kslice(h, j):
                lo = (h % 2) * 64
                return KT[lo:lo + 64, h // 2, j * 128:(j + 1) * 128]

            def fox_tile(g, qi):
                i = 4 * g + qi
                po_i = 2 + nxt("po", 2)
                for h in range(4):
                    b = nxt("b", 2)
                    bk = "Bih%d" % b
                    nblk = 4 * (i + 1)
                    ts("dve", Bih[b][:, :nblk], cumcol[:, h, :nblk], CPt[:, h, i:i + 1], 0.0, ALU.subtract, ALU.min,
                       ["cumcol", "CPt"], [bk])
                    for gg in range(i + 1):
                        ps_i = nxt("ps", 2)
                        p_i = nxt("pt", 2)
                        psv = PS[ps_i][:].rearrange("p (j q) -> p j q", q=128)
                        for jj in range(4):
                            pe(psv[:, jj, :], kslice(h, 4 * gg + jj), qslice(h, qi), True, True, ["KT", "QT"], [PK[ps_i]])
                        for jj in range(4):
                            act(PT[p_i][:, jj, :], psv[:, jj, :], AF.Exp, [PK[ps_i], bk], ["PT%d" % p_i],
                                bias=Bih[b][:, 4 * gg + jj:4 * gg + jj + 1], scale=SCALE)
                        if gg == i:
                            tt("dve", PT[p_i][:], PT[p_i][:], dmask, ALU.mult, ["PT%d" % p_i, "cbf"], ["PT%d" % p_i])
                        for jj in range(4):
                            pe(PS[po_i][:, h * 65:(h + 1) * 65], PT[p_i][:, jj, :], V1[:, 4 * gg + jj, h * 65:(h + 1) * 65],
                               gg == 0 and jj == 0, gg == i and jj == 3, ["PT%d" % p_i, "V1"], [PK[po_i]])
                finalize(po_i, i, 1)

            def sb_tile(g, qi):
                i = 4 * g + qi
                po_i = 2 + nxt("po", 2)
                for h in range(4):
                    mset(Rt[:], 0.0, ["Rt"])
                    for gg in range(i, -1, -1):
                        ps_i = nxt("ps", 2)
                        p_i = nxt("pt", 2)
                        psv = PS[ps_i][:].rearrange("p (j q) -> p j q", q=128)
                        ps6 = PS[6][:].rearrange("p (j q) -> p j q", q=128)
                        for jj in range(4):
                            pe(psv[:, jj, :], kslice(h, 4 * gg + jj), qslice(h, qi), True, True, ["KT", "QT"], [PK[ps_i]])
                        act(ebuf[:], PS[ps_i][:], AF.Exp, [PK[ps_i]], ["ebuf"], scale=SCALE)
                        act(ubuf[:].rearrange("p j q -> p (j q)"), ebuf[:], AF.Ln, ["ebuf"], ["ubuf"], bias=1.0)
                        if gg == i:
                            tt("dve", ubuf[:], ubuf[:], dmaskS, ALU.mult, ["ubuf", "cbf"], ["ubuf"])
                        for jj in range(4):
                            pe(ps6[:, jj, :], triS, ubuf[:, jj, :], True, jj == 3, ["cbf", "ubuf"], [PK[6]])
                            for j2 in range(jj + 1, 4):
                                pe(ps6[:, jj, :], ones_b, ubuf[:, j2, :], False, j2 == 3, ["cbf", "ubuf"], [PK[6]])
                        if gg > 0:
                            for jj in range(4):
                                pe(PS[7][:, :128], ones_b, ubuf[:, jj, :], jj == 0, jj == 3, ["cbf", "ubuf"], [PK[7]])
                        tt("dve", t1[:].rearrange("p (j q) -> p j q", q=128), ps6, Rt[:].unsqueeze(1).to_broadcast([128, 4, 128]),
                           ALU.add, [PK[6], "Rt"], ["t1"])
                        stt("dve", ebuf[:], PS[ps_i][:], SCALE, t1[:], ALU.mult, ALU.subtract, [PK[ps_i], "t1"], ["ebuf"])
                        act(PT[p_i][:].rearrange("p j q -> p (j q)"), ebuf[:], AF.Exp, ["ebuf"], ["PT%d" % p_i])
                        if gg == i:
                            tt("dve", PT[p_i][:], PT[p_i][:], dmaskS, ALU.mult, ["PT%d" % p_i, "cbf"], ["PT%d" % p_i])
                        for jj in range(4):
                            pe(PS[po_i][:, h * 65:(h + 1) * 65], PT[p_i][:, jj, :], V1[:, 4 * gg + jj, h * 65:(h + 1) * 65],
                               gg == i and jj == 0, gg == 0 and jj == 3, ["PT%d" % p_i, "V1"], [PK[po_i]])
                        if gg > 0:
                            tt("dve", Rt[:], Rt[:], PS[7][:, :128], ALU.add, ["Rt", PK[7]], ["Rt"])
                yb = nxt("y", 2)
                po3 = PS[po_i][:, :260].rearrange("p (h d) -> p h d", d=65)
                cp("act", ybuf[yb][:], po3[:, :, 0:64], [PK[po_i]], ["ybuf%d" % yb])
                P.dma("sp", ydram[i, :, 2 * 256:3 * 256], ybuf[yb][:].rearrange("p h d -> p (h d)"), reads=["ybuf%d" % yb], writes=["ydram"])

            def masked_attn(i, qi, po_i, kfn, vfn, kkeys, vkeys):
                for h in range(4):
                    for gg in range(i + 1):
                        ps_i = nxt("ps", 2)
                        p_i = nxt("pt", 2)
                        psv = PS[ps_i][:].rearrange("p (j q) -> p j q", q=128)
                        for jj in range(4):
                            pe(psv[:, jj, :], kfn(h, 4 * gg + jj), qslice(h, qi), True, True, kkeys + ["QT"], [PK[ps_i]])
                        act(PT[p_i][:].rearrange("p j q -> p (j q)"), PS[ps_i][:], AF.Exp, [PK[ps_i]], ["PT%d" % p_i], scale=SCALE)
                        tt("pool", PT[p_i][:], PT[p_i][:], maskT[:, 4 * gg:4 * gg + 4, :], ALU.mult, ["PT%d" % p_i, "maskT"], ["PT%d" % p_i])
                        for jj in range(4):
                            pe(PS[po_i][:, h * 65:(h + 1) * 65], PT[p_i][:, jj, :], vfn(h, 4 * gg + jj),
                               gg == 0 and jj == 0, gg == i and jj == 3, ["PT%d" % p_i] + vkeys, [PK[po_i]])

            def build_maskT(i):
                nblk = 4 * (i + 1)
                for j0 in range(0, nblk, 4):
                    ps_i = 6 + nxt("r", 2)
                    psv = PS[ps_i][:].rearrange("p (j q) -> p j q", q=128)
                    for jj in range(4):
                        tr(psv[:, jj, :], sc[:, (j0 + jj) * 128:(j0 + jj + 1) * 128], ["sc"], [PK[ps_i]], f32=True)
                    cp("act", maskT[:, j0:j0 + 4, :], psv, [PK[ps_i]], ["maskT"])

            def small_proj(qi):
                for c in range(8):
                    pe(PS[5][:, :16], hq[:, c, qi * 128:(qi + 1) * 128], wsm_sb[:, c, :], c == 0, c == 7, ["hq", "wsm"], [PK[5]])
                cp("act", wsmall[:], PS[5][:, :16], [PK[5]], ["wsmall"])

            def dsa_tile(g, qi):
                i = 4 * g + qi
                L = 512 * (i + 1)
                small_proj(qi)
                for gg in range(i + 1):
                    for h in range(4):
                        ps_i = 6 + nxt("r", 2)
                        rb = nxt("b", 2)
                        pe(PS[ps_i][:], qm[:, h, qi * 128:(qi + 1) * 128], kiT4[:, gg * 512:(gg + 1) * 512], True, True, ["qm", "kiT4"], [PK[ps_i]])
                        act(rbuf[rb][:], PS[ps_i][:], AF.Relu, [PK[ps_i]], ["rbuf%d" % rb])
                        if h == 0:
                            ts("dve", sc[:, gg * 512:(gg + 1) * 512], rbuf[rb][:], wsmall[:, 0:1], None, ALU.mult, None,
                               ["rbuf%d" % rb, "wsmall"], ["sc"])
                        else:
                            stt("dve", sc[:, gg * 512:(gg + 1) * 512], rbuf[rb][:], wsmall[:, h:h + 1], sc[:, gg * 512:(gg + 1) * 512],
                                ALU.mult, ALU.add, ["rbuf%d" % rb, "wsmall", "sc"], ["sc"])
                tt("dve", sc[:, i * 512:(i + 1) * 512], sc[:, i * 512:(i + 1) * 512], dmaskN, ALU.add, ["sc", "cf"], ["sc"])
                mset(mid[:], 0.0, ["mid"], eng="dve")
                w = BIS_R0
                for _ in range(BIS_K):
                    ts("dve", junk[:, :L], sc[:, :L], mid[:, 0:1], None, ALU.is_ge, ALU.add, ["sc", "mid"], ["maskT", "cnt"], accum=cnt[:, 0:1])
                    ts("dve", tmpb[:], cnt[:], KSEL - 0.5, w, ALU.is_ge, ALU.mult, ["cnt"], ["tmpb"])
                    stt("dve", mid[:], tmpb[:], -0.5 * w, mid[:], ALU.add, ALU.add, ["tmpb", "mid"], ["mid"])
                    w *= 0.5
                ts("dve", mid[:], mid[:], -w, None, ALU.add, None, ["mid"], ["mid"])
                ts("dve", sc[:, :L], sc[:, :L], mid[:, 0:1], None, ALU.is_ge, None, ["sc", "mid"], ["sc"])
                build_maskT(i)
                po_i = 2 + nxt("po", 2)
                masked_attn(i, qi, po_i, kslice, lambda h, j: V1[:, j, h * 65:(h + 1) * 65], ["KT"], ["V1"])
                finalize(po_i, i, 0)

            def mem_tile(g, qi):
                i = 4 * g + qi
                po_i = 2 + nxt("po", 2)
                for h in range(4):
                    ps_i = nxt("ps", 2)
                    p_i = nxt("pt", 2)
                    psv = PS[ps_i][:].rearrange("p (j q) -> p j q", q=128)
                    lo = (h % 2) * 64
                    for c in range(2):
                        pe(psv[:, c, :], mkT[lo:lo + 64, h // 2, c * 128:(c + 1) * 128], qslice(h, qi), True, True, ["mkT", "QT"], [PK[ps_i]])
                    act(PT[p_i][:, 0:2, :], psv[:, 0:2, :], AF.Exp, [PK[ps_i]], ["PT%d" % p_i], scale=SCALE)
                    for c in range(2):
                        pe(PS[po_i][:, h * 65:(h + 1) * 65], PT[p_i][:, c, :], mv1[:, c, h, :], c == 0, c == 1, ["PT%d" % p_i, "mv1"], [PK[po_i]])
                finalize(po_i, i, 4)

            def nsa_tile(g, qi):
                i = 4 * g + qi
                L = 512 * (i + 1)
                po_c, po_s, po_w = 2, 3, 5
                small_proj(qi)
                act(g12[:], wsmall[:, 4:16], AF.Sigmoid, ["wsmall"], ["g12"])
                P.dma("sp", vis_t[:], visd[:, i * NBS:(i + 1) * NBS], writes=["vis_t"])
                P.dma("sp", add_t[:], addd[:, i * NBS:(i + 1) * NBS], writes=["add_t"])
                P.dma("sp", trow_t[:], trow[:, i * 128:(i + 1) * 128].partition_broadcast(128), writes=["Rt"])
                ts("dve", cmaskN, nrow, tq[:, i:i + 1], None, ALU.is_le, None, ["cf"], ["ebuf"])
                for c in range(NCC):
                    ts("dve", cmaskT[:, c, :], trow_t[:], ncol[:, c:c + 1], None, ALU.is_ge, None, ["Rt", "cf"], ["cmaskT"])
                for h in range(4):
                    lo = (h % 2) * 64
                    pe(PS[4][:, :NCP], qslice(h, qi), kc2T[lo:lo + 64, :], True, True, ["QT", "kc2T"], [PK[4]])
                    act(ec, PS[4][:, :NCP], AF.Exp, [PK[4]], ["t1"], scale=SCALE)
                    stt("dve", ec, ec, 1.0, cmaskN, ALU.mult, ALU.mult, ["t1", "ebuf"], ["t1", "rs4"], accum=rs4[:, h:h + 1])
                    ts("dve", rs4[:, h:h + 1], rs4[:, h:h + 1], 1e-30, None, ALU.max, None, ["rs4"], ["rs4"])
                    P.op("dve", lambda E, h=h: E.reciprocal(rs4[:, h:h + 1], rs4[:, h:h + 1]), reads=["rs4"], writes=["rs4"])
                    if h == 0:
                        ts("dve", impacc, ec, rs4[:, 0:1], None, ALU.mult, None, ["t1", "rs4"], ["t2"])
                    else:
                        stt("dve", impacc, ec, rs4[:, h:h + 1], impacc, ALU.mult, ALU.add, ["t1", "rs4", "t2"], ["t2"])
                P.op("dve", lambda E: E.tensor_reduce(imp4[:], impacc.rearrange("p (b u) -> p b u", u=4), AX.X, ALU.add),
                     reads=["t2"], writes=["imp4"])
                tt("dve", imp4[:], imp4[:], vis_t[:], ALU.mult, ["imp4", "vis_t"], ["imp4"])
                tt("dve", imp4[:], imp4[:], add_t[:], ALU.add, ["imp4", "add_t"], ["imp4"])
                P.op("dve", lambda E: E.max(out=m8a[:], in_=imp4[:]), reads=["imp4"], writes=["m8a"])
                P.op("dve", lambda E: E.match_replace(out=imp2[:], in_to_replace=m8a[:], in_values=imp4[:], imm_value=-1.0e30),
                     reads=["imp4", "m8a"], writes=["imp2"])
                P.op("dve", lambda E: E.max(out=m8b[:], in_=imp2[:]), reads=["imp2"], writes=["m8b"])
                ts("dve", bm[:], imp4[:], m8b[:, 7:8], None, ALU.is_ge, None, ["imp4", "m8b"], ["bm"])
                nsb = 8 * (i + 1)
                cp("dve", sc[:, :L].rearrange("p (b u) -> p b u", u=64), bm[:, :nsb].unsqueeze(2).to_broadcast([128, nsb, 64]), ["bm"], ["sc"])
                tt("dve", sc[:, i * 512:(i + 1) * 512], sc[:, i * 512:(i + 1) * 512], dmaskN01, ALU.mult, ["sc", "cbf"], ["sc"])
                build_maskT(i)
                masked_attn(i, qi, po_s, lambda h, j: KT[(h % 2) * 64:(h % 2) * 64 + 64, 0, j * 128:(j + 1) * 128],
                            lambda h, j: V1[:, j, 0:65], ["KT"], ["V1"])
                for h in range(4):
                    lo = (h % 2) * 64
                    ps_i = nxt("ps", 2)
                    psv = PS[ps_i][:].rearrange("p (j q) -> p j q", q=128)
                    for c in range(NCC):
                        pe(psv[:, c, :], kc2T[lo:lo + 64, c * 128:(c + 1) * 128], qslice(h, qi), True, True, ["kc2T", "QT"], [PK[ps_i]])
                    act(eT[:], psv[:, 0:NCC, :], AF.Exp, [PK[ps_i]], ["eT"], scale=SCALE)
                    tt("dve", eT[:], eT[:], cmaskT[:], ALU.mult, ["eT", "cmaskT"], ["eT"])
                    for c in range(NCC):
                        pe(PS[po_c][:, h * 65:(h + 1) * 65], eT[:, c, :], vc1[:, c, :], c == 0, c == NCC - 1, ["eT", "vc1"], [PK[po_c]])
                for h in range(4):
                    lo = (h % 2) * 64
                    grps = [gr for gr in range(2) if 4 * i - 4 + 4 * gr >= 0]
                    for gr in grps:
                        kb0 = 4 * i - 4 + 4 * gr
                        ps_i = nxt("ps", 2)
                        p_i = nxt("pt", 2)
                        psv = PS[ps_i][:].rearrange("p (j q) -> p j q", q=128)
                        for jj in range(4):
                            pe(psv[:, jj, :], KT[lo:lo + 64, 1, (kb0 + jj) * 128:(kb0 + jj + 1) * 128], qslice(h, qi), True, True, ["KT", "QT"], [PK[ps_i]])
                        act(PT[p_i][:].rearrange("p j q -> p (j q)"), PS[ps_i][:], AF.Exp, [PK[ps_i]], ["PT%d" % p_i], scale=SCALE)
                        tt("dve", PT[p_i][:], PT[p_i][:], wmask[:, 4 * gr:4 * gr + 4, :], ALU.mult, ["PT%d" % p_i, "cbf"], ["PT%d" % p_i])
                        for jj in range(4):
                            pe(PS[po_w][:, h * 65:(h + 1) * 65], PT[p_i][:, jj, :], V1[:, kb0 + jj, 65:130],
                               gr == grps[0] and jj == 0, gr == grps[-1] and jj == 3, ["PT%d" % p_i, "V1"], [PK[po_w]])
                for b, po_i in enumerate((po_c, po_s, po_w)):
                    po3 = PS[po_i][:, :260].rearrange("p (h d) -> p h d", d=65)
                    ts("dve", rd[:], po3[:, :, 64], 1e-30, None, ALU.max, None, [PK[po_i]], ["rd"])
                    P.op("dve", lambda E: E.reciprocal(rd[:], rd[:]), reads=["rd"], writes=["rd"])
                    tt("dve", coef[:], rd[:], g12[:, b * 4:(b + 1) * 4], ALU.mult, ["rd", "g12"], ["coef"])
                    if b == 0:
                        tt("dve", yacc[:], po3[:, :, 0:64], coef[:].unsqueeze(2).to_broadcast([128, 4, 64]), ALU.mult, [PK[po_i], "coef"], ["yacc"])
                    else:
                        tt("dve", ytmp[:], po3[:, :, 0:64], coef[:].unsqueeze(2).to_broadcast([128, 4, 64]), ALU.mult, [PK[po_i], "coef"], ["ytmp"])
                        tt("dve", yacc[:], yacc[:], ytmp[:], ALU.add, ["yacc", "ytmp"], ["yacc"])
                yb = nxt("y", 2)
                cp("act", ybuf[yb][:], yacc[:], ["yacc"], ["ybuf%d" % yb])
                P.dma("sp", ydram[i, :, 3 * 256:4 * 256], ybuf[yb][:].rearrange("p h d -> p (h d)"), reads=["ybuf%d" % yb], writes=["ydram"])

            def load_wq(name, wt=None, wkey="wqb"):
                off, n = B_WQ[name]
                load_w(wqb if wt is None else wt, wkey, lambda c0, nn: wq[:, off + c0:off + c0 + nn], n)

            def run_branch(qgroup_fn, tile_fn):
                for g in range(NG):
                    load_hq(g)
                    qgroup_fn(g)
                    for qi in range(4):
                        tile_fn(g, qi)

            load_w(wsm_sb, "wsm", lambda c0, nn: wsm[:, c0:c0 + nn], 16)
            load_branch_kv(KFM_ROWS["foxk"], 1)
            load_wq("fox")
            run_branch(lambda g: make_QT(g, False), fox_tile)
            load_branch_kv(KFM_ROWS["sbk"], 2)
            load_wq("sb")
            run_branch(lambda g: make_QT(g, False), sb_tile)
            load_wq("mem")
            run_branch(lambda g: make_QT(g, False), mem_tile)
            load_branch_kv(KFM_ROWS["dsak"], 0)
            for h in range(4):
                ld_kfm(kiT4[32 * h:32 * h + 32, :], KFM_ROWS["idxk"], 32, "kiT4")
            load_wq("dsa")
            load_wq("idx", wqb2, "wqb2")

            def dsa_group(g):
                make_QT(g, True)
                load_tabs(g, 2)
                fm_q(4, 0, wqb2, "wqb2")
                fm_q(5, 128, wqb2, "wqb2")
                tt("dve", t1[:], PS[4][:], tabg[:, 0, :], ALU.mult, [PK[4], "tabg"], ["t1"])
                tt("dve", t2[:], PS[5][:], tabg[:, 1, :], ALU.mult, [PK[5], "tabg"], ["t2"])
                tt("dve", qiT[:], t1[:], t2[:], ALU.add, ["t1", "t2"], ["qiT"])
                for h in range(4):
                    ts("dve", qm[:, h, :], qiT[:], sel4[:, h:h + 1], None, ALU.mult, None, ["qiT", "cf"], ["qm"])

            run_branch(dsa_group, dsa_tile)
            ks0 = KFM_ROWS["kcks"] + 64
            for half in range(2):
                ld_kfm(KT[64 * half:64 * half + 64, 0, :], ks0, 64, "KT")
                ld_kfm(KT[64 * half:64 * half + 64, 1, :], KFM_ROWS["kw"], 64, "KT")
            ld_v(V1[:, :, 0:65], v2, 0, 2, "V1")
            ld_v(V1[:, :, 65:130], v2, 1, 2, "V1")
            load_wq("nsa")
            run_branch(lambda g: make_QT(g, True), nsa_tile)
            P.barrier()
            P.emit()

        with ExitStack() as st2:
            M2 = Pool2(nc, st2)
            NT3 = T // 256
            wz_sb = M2.sb("wz", [128, 8, 1280], BF16)
            wmg_sb = M2.sb("wmg", [128, 8, 5120], BF16)
            wbr_sb = M2.sb("wbr", [128, 5, 2, 1024], BF16)
            wout_sb = M2.sb("wout", [128, 8, 1024], BF16)
            fg_sb = M2.sb("fg", [128, 8], F32)
            h3 = M2.sb("h3", [128, 8, 256], BF16)
            x3 = M2.sb("x3", [128, 8, 256], F32)
            yl = M2.sb("yl", [128, 1280], BF16)
            zs = M2.sb("zs", [128, 1280], BF16)
            ysT = M2.sb("ysT", [128, 10, 256], BF16)
            sg = [M2.sb("sg", [128, 256], F32) for _ in range(2)]
            tmpm = M2.sb("tmpm", [128, 256], F32)
            macc = M2.sb("macc", [128, 256], F32)
            mergedT = M2.sb("mergedT", [128, 8, 256], BF16)
            xo = M2.sb("xo", [128, 8, 256], F32)
            rstd3 = M2.sb("rstd3", [128, 256], F32)
            c3 = {"ps": 0, "r": 0, "sg": 0}

            def nx3(name, n):
                v = c3[name] % n
                c3[name] += 1
                return v

            P.dma("sp", fg_sb[:], fgcol, writes=["fg"])
            load_w(wz_sb, "wz", lambda c0, nn: wz[:, c0:c0 + nn], 1280)
            load_w(wmg_sb, "wmg", lambda c0, nn: wmg[:, c0:c0 + nn], 5120)
            load_w(wout_sb, "wout", lambda c0, nn: wout[:, c0:c0 + nn], 1024)
            stgf = stg[:].rearrange("p c n -> p (c n)")
            for n in range(5):
                for kk in range(2):
                    P.dma("sp", stgf[:, :1024], wbr[n, kk * 128:(kk + 1) * 128, :], writes=["stg"])
                    P.op("dve", lambda E, n=n, kk=kk: E.tensor_copy(wbr_sb[:, n, kk, :], stgf[:, :1024]), reads=["stg"], writes=["wbr"])

            def p3_group(tg):
                tsl = slice(tg * 256, (tg + 1) * 256)
                P.dma("sp", h3[:], hT[:, tsl].rearrange("(c p) t -> p c t", p=128), writes=["h3"])
                P.dma("sp", x3[:], xT[:, tsl].rearrange("(c p) t -> p c t", p=128), writes=["x3"])
                for sub in range(2):
                    i = 2 * tg + sub
                    qs = slice(sub * 128, (sub + 1) * 128)
                    P.dma("sp", yl[:], ydram[i], reads=["ydram"], writes=["yl"])
                    for (pi, c0, n) in ((4, 0, 512), (5, 512, 512), (6, 1024, 256)):
                        for c in range(8):
                            pe(PS[pi][:, :n], h3[:, c, qs], wz_sb[:, c, c0:c0 + n], c == 0, c == 7, ["h3", "wz"], [PK[pi]])
                        act(zs[:, c0:c0 + n], PS[pi][:, :n], AF.Silu, [PK[pi]], ["zs"])
                    tt("dve", zs[:], zs[:], yl[:], ALU.mult, ["zs", "yl"], ["zs"])
                    for k0 in (0, 4, 8):
                        nb = min(4, 10 - k0)
                        ps_i = nx3("ps", 2)
                        psv = PS[ps_i][:].rearrange("p (j q) -> p j q", q=128)
                        for kk in range(nb):
                            tr(psv[:, kk, :], zs[:, (k0 + kk) * 128:(k0 + kk + 1) * 128], ["zs"], [PK[ps_i]])
                        cp("act", ysT[:, k0:k0 + nb, qs], psv[:, 0:nb, :], [PK[ps_i]], ["ysT"])
                for dch in range(8):
                    ds_ = slice(dch * 128, (dch + 1) * 128)
                    for n in range(5):
                        pb = nx3("ps", 2)
                        pg = 6 + nx3("r", 2)
                        si = nx3("sg", 2)
                        for kk in range(2):
                            pe(PS[pb][:, :256], wbr_sb[:, n, kk, ds_], ysT[:, 2 * n + kk, :], kk == 0, kk == 1, ["wbr", "ysT"], [PK[pb]])
                        for c in range(8):
                            pe(PS[pg][:, :256], wmg_sb[:, c, n * 1024 + dch * 128:n * 1024 + (dch + 1) * 128], h3[:, c, :], c == 0, c == 7,
                               ["wmg", "h3"], [PK[pg]])
                        act(sg[si][:], PS[pg][:, :256], AF.Sigmoid, [PK[pg]], ["sg%d" % si])
                        if n == 0:
                            tt("dve", macc[:], PS[pb][:, :256], sg[si][:], ALU.mult, [PK[pb], "sg%d" % si], ["macc"])
                        else:
                            tt("dve", tmpm[:], PS[pb][:, :256], sg[si][:], ALU.mult, [PK[pb], "sg%d" % si], ["tmpm"])
                            tt("pool", macc[:], macc[:], tmpm[:], ALU.add, ["macc", "tmpm"], ["macc"])
                    cp("act", mergedT[:, dch, :], macc[:], ["macc"], ["mergedT"])
                for dch in range(8):
                    ds_ = slice(dch * 128, (dch + 1) * 128)
                    for c in range(8):
                        pe(PS[4][:, :256], wout_sb[:, c, ds_], mergedT[:, c, :], c == 0, c == 7, ["wout", "mergedT"], [PK[4]])
                    tt("dve", xo[:, dch, :], PS[4][:, :256], x3[:, dch, :], ALU.add, [PK[4], "x3"], ["xo"])
                P.dma("sp", xo_o[:, tsl].rearrange("(c p) t -> p c t", p=128), xo[:], reads=["xo"], writes=["xo_o"])
                if not FINAL:
                    return
                act(h3[:], xo[:], AF.Square, ["xo"], ["h3"])
                for c in range(8):
                    pe(PS[5][:, :256], ones_b, h3[:, c, :], c == 0, c == 7, ["cbf", "h3"], [PK[5]])
                act(rstd3[:], PS[5][:, :256], AF.Ln, [PK[5]], ["rstd3"], bias=EPS, scale=1.0 / 1024)
                act(rstd3[:], rstd3[:], AF.Exp, ["rstd3"], ["rstd3"], scale=-0.5)
                for c in range(8):
                    stt("dve", x3[:, c, :], xo[:, c, :], fg_sb[:, c:c + 1], rstd3[:], ALU.mult, ALU.mult, ["xo", "fg", "rstd3"], ["x3"])
                P.dma("sp", xn_o[:, tsl].rearrange("(c p) t -> p c t", p=128), x3[:], reads=["x3"], writes=["xn_o"])

            for tg in range(NT3):
                p3_group(tg)
            P.barrier()
            P.emit()
    return nc


def host_prep_B(w_in, pe_k, pe_v, wc1_k, wc2_k, wc1_v, wc2_v, mem_norm, w_mem_kv, w_branch, w_out, final_norm):
    c = lambda n: _cols(w_in, n)
    p64 = _perm_idx(256, 64)
    wq = np.concatenate([
        c('dsa_q'), c('dsa_q')[:, p64],
        c('idx_q'), c('idx_q')[:, _perm_idx(128, 32)],
        c('fox_q'), c('sb_q'),
        c('nsa_q'), c('nsa_q')[:, p64],
        c('mem_q')], 1)
    assert wq.shape[1] == B_NWQ
    w1 = np.concatenate([wc1_k.reshape(32, 64, 128).transpose(1, 0, 2), wc1_v.reshape(32, 64, 128).transpose(1, 0, 2)], 0)
    return {
        "wq": np.ascontiguousarray(wq),
        "wsm": np.ascontiguousarray(np.concatenate([c('idx_w'), c('nsa_g')], 1)),
        "wz": np.ascontiguousarray(np.concatenate([c('dsa_z'), c('fox_z'), c('sb_z'), c('nsa_z'), c('mem_z')], 1)),
        "wmg": np.ascontiguousarray(c('merge')),
        "wbr": np.ascontiguousarray(w_branch),
        "wout": np.ascontiguousarray(w_out),
        "w1": np.ascontiguousarray(w1.reshape(128, 32 * 128)),
        "w2k2": np.ascontiguousarray(np.concatenate([wc2_k, wc2_k], 1)),
        "w2v": np.ascontiguousarray(wc2_v),
        "peT": np.ascontiguousarray(np.concatenate([pe_k.T, pe_v.T], 0)),
        "mgcol": np.ascontiguousarray(mem_norm.reshape(8, 128).T),
        "wmem": np.ascontiguousarray(w_mem_kv),
        "fgcol": np.ascontiguousarray(final_norm.reshape(8, 128).T),
    }


def host_consts_B(T, r):
    S = 4 * T
    NQ = T // 128
    NBS = S // 64
    NCMP = S // 16 - 1
    NCC = (NCMP + 127) // 128
    NCP = NCC * 128
    s = np.arange(128)[:, None]
    q = np.arange(128)[None, :]
    ident = (s == q).astype(np.float32)
    triS = (s >= q).astype(np.float32)
    ones = np.ones((128, 128), np.float32)
    dmask = np.zeros((128, 4, 128), np.float32)
    dmaskS = np.zeros((128, 4, 128), np.float32)
    for jj in range(4):
        if jj < r:
            dmask[:, jj, :] = 1.0
            dmaskS[:, jj, :] = 1.0
        elif jj == r:
            dmask[:, jj, :] = (s <= q)
            dmaskS[:, jj, :] = (s < q)
    wmask = np.zeros((128, 8, 128), np.float32)
    for jj in range(8):
        diff = (r + 4 - jj) * 128 + q - s
        wmask[:, jj, :] = ((diff >= 0) & (diff < 512))
    dmaskN01 = dmask.transpose(2, 1, 0).reshape(128, 512)
    c_bf = np.concatenate([ident, triS, ones, dmask.reshape(128, 512), dmaskS.reshape(128, 512),
                           wmask.reshape(128, 1024), dmaskN01], 1).astype(NPBF)
    triInc = (s <= q).astype(np.float32)
    dmaskN = np.where(dmaskN01 > 0, 0.0, NEG_BIG).astype(np.float32)
    oh = np.zeros((128, 4), np.float32)
    oh[:, r] = 1.0
    sel4 = (np.arange(128)[:, None] // 32 == np.arange(4)[None, :]).astype(np.float32)
    tq = ((4 * np.arange(NQ)[None, :] + r) * 128 + np.arange(128)[:, None]).astype(np.float32)
    ncol = (16.0 * (128 * np.arange(NCC)[None, :] + np.arange(128)[:, None]) + 31.0).astype(np.float32)
    nrow = np.broadcast_to((16.0 * np.arange(NCP) + 31.0)[None, :], (128, NCP)).astype(np.float32)
    c_f = np.concatenate([triInc, ones, ident, dmaskN, oh, sel4, tq, ncol, nrow], 1).astype(np.float32)
    trow = np.ascontiguousarray(tq.T.reshape(1, T))
    cur = (tq // 64)[:, :, None]
    blk = np.arange(NBS)[None, None, :]
    forced = (blk == 0) | (blk == cur) | (blk == cur - 1)
    visible = blk <= cur
    vis = (visible & ~forced).astype(np.float32)
    addc = np.where(forced, 1.0e4 + blk, np.where(visible, 0.0, -1.0)).astype(np.float32)
    return {"c_bf": np.ascontiguousarray(c_bf), "c_f": np.ascontiguousarray(c_f), "trow": trow.astype(np.float32),
            "vis": np.ascontiguousarray(vis.reshape(128, NQ * NBS)), "addc": np.ascontiguousarray(addc.reshape(128, NQ * NBS))}


def shard_tokens(a, T):
    NQ = T // 128
    v = a.reshape((NQ, 4, 128) + a.shape[1:])
    return [np.ascontiguousarray(v[:, r].reshape((T,) + a.shape[1:])) for r in range(4)]


def unshard_tokens(parts, T):
    NQ = T // 128
    tail = parts[0].shape[1:]
    v = np.stack([p.reshape((NQ, 128) + tail) for p in parts], 1)
    return v.reshape((4 * T,) + tail)


A_W = {"gcol": [128, 8], "wfm": [1024, A_NFM], "wtm": [1024, A_NTM], "kvg": [128, 1], "wuk2": [128, 512],
       "wuv": [128, 256], "foxb": [128, 4]}
B_W = {"wq": [1024, B_NWQ], "wsm": [1024, 16], "wz": [1024, 1280], "wmg": [1024, 5120], "wbr": [5, 256, 1024],
       "wout": [1024, 1024], "w1": [128, 32 * 128], "w2k2": [128, 128], "w2v": [128, 64], "peT": [128, 32],
       "mgcol": [128, 8], "wmem": [1024, 512]}
GROUPS = [[0, 1, 2, 3], [4, 5, 6, 7]]


def build_F(T, depth):
    nc = bass.Bass("TRN2", target_bir_lowering=False)
    S = 4 * T
    NB = S // 128
    NQ = T // 128
    NBS = S // 64
    NCMP = S // 16 - 1
    NCC = (NCMP + 127) // 128
    NCP = NCC * 128
    EI = lambda name, shape, dt=F32: nc.dram_tensor(name, list(shape), dt, kind="ExternalInput").ap()
    IN = lambda name, shape, dt: nc.dram_tensor(name, list(shape), dt).ap()
    xT = EI("xT", [1024, T])
    pos = EI("pos", [1, T], I32)
    memT = EI("memT", [1024, 256])
    rc = EI("rc", [128, 8])
    fgcol = EI("fgcol", [128, 8])
    cst = {"c_bf": EI("c_bf", [128, 128 * 3 + 512 * 2 + 1024 + 512], BF16),
           "c_f": EI("c_f", [128, 128 * 3 + 512 + 4 + 4 + NQ + NCC + NCP]),
           "trow": EI("trow", [1, T]), "vis": EI("vis", [128, NQ * NBS]), "addc": EI("addc", [128, NQ * NBS])}
    WA = {k: EI(k, [depth] + v) for k, v in A_W.items()}
    WB = {k: EI(k, [depth] + v) for k, v in B_W.items()}
    xn_o = nc.dram_tensor("xn", [1024, T], F32, kind="ExternalOutput").ap()
    ydram = IN("ydram", [NQ, 128, 1280], BF16)
    with ExitStack() as stack:
        P = Prog(nc, stack)
        M = Pool2(nc, stack)
        PS = [M.ps("ps%d" % i, [128, 512]) for i in range(8)]
        x_cur = xT
        for l in range(depth):
            hT_l = IN("hT%d" % l, [1024, T], BF16)
            tab_l = IN("tab%d" % l, [4, 128, T], F32)
            kfmL = IN("kfmL%d" % l, [KFM_N, T], BF16)
            v3L = IN("v3L%d" % l, [3, T, 260], BF16)
            v2L = IN("v2L%d" % l, [2, T, 65], BF16)
            lfL = IN("lfL%d" % l, [T, 4], F32)
            kfmG = [IN("kfmG%d_%d" % (l, c), [512, T], BF16) for c in range(KFM_N // 128)]
            v3G = [[IN("v3G%d_%d_%d" % (l, k, hf), [4 * (T // 2), 260], BF16) for hf in range(2)] for k in range(3)]
            v2G = [[IN("v2G%d_%d_%d" % (l, k, hf), [4 * (T // 2), 65], BF16) for hf in range(2)] for k in range(2)]
            lfG = IN("lfG%d" % l, [4 * T, 4], F32)
            x_nxt = IN("x%d" % (l + 1), [1024, T], F32)
            ovA = {k: v[l] for k, v in WA.items()}
            ovA.update({"xT": x_cur, "pos": pos, "rc": rc, "hT": hT_l, "kfm": kfmL, "v3": v3L, "v2": v2L, "lf": lfL, "tab": tab_l})
            build_A(T, {"nc": nc, "P": P, "PS": PS, "ov": ovA})
            for c in range(KFM_N // 128):
                P.coll([kfmL[c * 128:(c + 1) * 128, :]], [kfmG[c]], GROUPS)
            for k in range(3):
                for hf in range(2):
                    P.coll([v3L[k, hf * (T // 2):(hf + 1) * (T // 2), :]], [v3G[k][hf]], GROUPS)
            for k in range(2):
                for hf in range(2):
                    P.coll([v2L[k, hf * (T // 2):(hf + 1) * (T // 2), :]], [v2G[k][hf]], GROUPS)
            P.coll([lfL], [lfG], GROUPS)
            P.barrier()
            ovB = {k: v[l] for k, v in WB.items()}
            ovB.update(cst)
            ovB.update({"xT": x_cur, "hT": hT_l, "tab": tab_l, "kfm": kfmG, "v3": v3G, "v2": v2G, "lf": lfG, "memT": memT,
                        "fgcol": fgcol, "xo": x_nxt, "xn": xn_o, "ydram": ydram})
            build_B(T, {"nc": nc, "P": P, "PS": PS, "ov": ovB, "final": l == depth - 1})
            x_cur = x_nxt
        P.barrier()
        P.emit()
    return nc


_NC_CACHE = {}


def _get_nc(kind, T):
    key = (kind, T)
    if key not in _NC_CACHE:
        _NC_CACHE[key] = build_A(T) if kind == "A" else build_B(T)
    return _NC_CACHE[key]


FUSED = True


def kernel_fused(x, mem, positions, norm_g, w_in, kv_norm, w_uk, w_uv, fox_bias, nsa_pe_k, nsa_pe_v,
                 nsa_wc1_k, nsa_wc2_k, nsa_wc1_v, nsa_wc2_v, mem_norm, w_mem_kv, w_branch, w_out, final_norm):
    f = lambda a: np.asarray(a, dtype=np.float32)
    x = f(x)
    mem = f(mem)
    positions = np.asarray(positions).astype(np.int32)
    Bn, S, D = x.shape
    T = S // 4
    depth = np.asarray(norm_g).shape[0]
    key = ("F", T, depth)
    if key not in _NC_CACHE:
        _NC_CACHE[key] = build_F(T, depth)
    nc = _NC_CACHE[key]
    cores = list(range(8))
    wA = [host_prep_A(f(norm_g[l]), f(w_in[l]), f(kv_norm[l]), f(w_uk[l]), f(w_uv[l]), f(fox_bias[l])) for l in range(depth)]
    wB = [host_prep_B(f(w_in[l]), f(nsa_pe_k[l]), f(nsa_pe_v[l]), f(nsa_wc1_k[l]), f(nsa_wc2_k[l]), f(nsa_wc1_v[l]),
                      f(nsa_wc2_v[l]), f(mem_norm[l]), f(w_mem_kv[l]), f(w_branch[l]), f(w_out[l]), f(final_norm)) for l in range(depth)]
    shared = {k: np.ascontiguousarray(np.stack([wA[l][k] for l in range(depth)], 0)) for k in A_W}
    shared.update({k: np.ascontiguousarray(np.stack([wB[l][k] for l in range(depth)], 0)) for k in B_W})
    shared["rc"] = wA[0]["rc"]
    shared["fgcol"] = wB[0]["fgcol"]
    del wA, wB
    consts = [host_consts_B(T, r) for r in range(4)]
    xs = [shard_tokens(x[b], T) for b in range(Bn)]
    ps = [shard_tokens(positions[b], T) for b in range(Bn)]
    in_maps = []
    for c in cores:
        b, r = c // 4, c % 4
        m = dict(shared)
        m.update(consts[r])
        m["xT"] = np.ascontiguousarray(xs[b][r].T)
        m["pos"] = ps[b][r].reshape(1, T)
        m["memT"] = np.ascontiguousarray(mem[b].T)
        in_maps.append(m)
    R = run_bass_kernel_spmd(nc, in_maps, core_ids=cores).results
    out = np.stack([unshard_tokens([np.ascontiguousarray(np.asarray(R[4 * b + r]["xn"]).T) for r in range(4)], T) for b in range(Bn)], 0)
    return out.astype(np.float32)


def kernel(x, mem, positions, norm_g, w_in, kv_norm, w_uk, w_uv, fox_bias, nsa_pe_k, nsa_pe_v,
           nsa_wc1_k, nsa_wc2_k, nsa_wc1_v, nsa_wc2_v, mem_norm, w_mem_kv, w_branch, w_out, final_norm):
    if FUSED:
        return kernel_fused(x, mem, positions, norm_g, w_in, kv_norm, w_uk, w_uv, fox_bias, nsa_pe_k, nsa_pe_v,
                            nsa_wc1_k, nsa_wc2_k, nsa_wc1_v, nsa_wc2_v, mem_norm, w_mem_kv, w_branch, w_out, final_norm)
    f = lambda a: np.asarray(a, dtype=np.float32)
    x = f(x)
    mem = f(mem)
    positions = np.asarray(positions).astype(np.int32)
    Bn, S, D = x.shape
    T = S // 4
    depth = np.asarray(norm_g).shape[0]
    ncA = _get_nc("A", T)
    ncB = _get_nc("B", T)
    cores = list(range(8))
    consts = [host_consts_B(T, r) for r in range(4)]
    xs = [shard_tokens(x[b], T) for b in range(Bn)]
    ps = [shard_tokens(positions[b], T) for b in range(Bn)]
    xT = [np.ascontiguousarray(xs[c // 4][c % 4].T) for c in cores]
    posr = [ps[c // 4][c % 4].reshape(1, T) for c in cores]
    memT = [np.ascontiguousarray(mem[b].T) for b in range(Bn)]
    xn = None
    for l in range(depth):
        wA = host_prep_A(f(norm_g[l]), f(w_in[l]), f(kv_norm[l]), f(w_uk[l]), f(w_uv[l]), f(fox_bias[l]))
        wB = host_prep_B(f(w_in[l]), f(nsa_pe_k[l]), f(nsa_pe_v[l]), f(nsa_wc1_k[l]), f(nsa_wc2_k[l]), f(nsa_wc1_v[l]),
                         f(nsa_wc2_v[l]), f(mem_norm[l]), f(w_mem_kv[l]), f(w_branch[l]), f(w_out[l]), f(final_norm))
        in_maps = []
        for c in cores:
            m = dict(wA)
            m["xT"] = xT[c]
            m["pos"] = posr[c]
            in_maps.append(m)
        RA = run_bass_kernel_spmd(ncA, in_maps, core_ids=cores).results
        del in_maps
        full = []
        for b in range(Bn):
            kfm = unshard_tokens([np.ascontiguousarray(np.asarray(RA[4 * b + r]["kfm"]).T) for r in range(4)], T)
            v3 = np.stack([unshard_tokens([np.asarray(RA[4 * b + r]["v3"])[k] for r in range(4)], T) for k in range(3)], 0)
            v2 = np.stack([unshard_tokens([np.asarray(RA[4 * b + r]["v2"])[k] for r in range(4)], T) for k in range(2)], 0)
            lf = unshard_tokens([np.asarray(RA[4 * b + r]["lf"]) for r in range(4)], T)
            full.append({
                "kfm": np.ascontiguousarray(kfm.T), "v3": np.ascontiguousarray(v3), "v2": np.ascontiguousarray(v2),
                "lf": np.ascontiguousarray(lf.reshape(S // 128, 128, 4).transpose(1, 0, 2).reshape(128, -1)),
            })
        in_maps = []
        for c in cores:
            b, r = c // 4, c % 4
            m = dict(wB)
            m.update(consts[r])
            m.update(full[b])
            m["xT"] = xT[c]
            m["hT"] = np.asarray(RA[c]["hT"])
            m["tab"] = np.asarray(RA[c]["tab"])
            m["memT"] = memT[b]
            in_maps.append(m)
        del RA
        RB = run_bass_kernel_spmd(ncB, in_maps, core_ids=cores).results
        del in_maps, full
        xT = [np.ascontiguousarray(np.asarray(RB[c]["xo"])) for c in cores]
        if l == depth - 1:
            xn = [np.asarray(RB[c]["xn"]) for c in cores]
        del RB
    out = np.stack([unshard_tokens([np.ascontiguousarray(xn[4 * b + r].T) for r in range(4)], T) for b in range(Bn)], 0)
    return out.astype(np.float32)
```

```python
import math
from contextlib import ExitStack
import numpy as np
import ml_dtypes
import concourse.bass as bass
import concourse.mybir as mybir
from concourse.bass_utils import run_bass_kernel_spmd

F32 = mybir.dt.float32
BF16 = mybir.dt.bfloat16
I32 = mybir.dt.int32
U8 = mybir.dt.uint8
AF = mybir.ActivationFunctionType
ALU = mybir.AluOpType
AX = mybir.AxisListType
NPBF = ml_dtypes.bfloat16

D_MODEL = 1024
NCH = 8
EPS = 1e-6
PI = math.pi


class Prog:
    ENG = ("pe", "act", "dve", "pool", "sp")
    CH = 20000
    ND = 24

    def __init__(self, nc, stack):
        self.nc = nc
        self.stack = stack
        self.q = {e: [] for e in self.ENG}
        self.n = {e: 0 for e in self.ENG}
        self.esems = {e: [] for e in self.ENG}
        self.seen = {e: {} for e in self.ENG}
        self.lastw = {}
        self.readers = {}
        self.dsems = [stack.enter_context(nc.semaphore(f"dq{i}")) for i in range(self.ND)]
        self.dcount = [0] * self.ND
        self.dlast = [None] * self.ND
        self.dnext = 0
        self.latest = {}
        self.nsem = 0

    def _esem(self, eng, idx):
        c = (idx - 1) // self.CH
        while len(self.esems[eng]) <= c:
            self.esems[eng].append(self.stack.enter_context(self.nc.semaphore(f"s_{eng}{len(self.esems[eng])}")))
        return self.esems[eng][c], (idx - 1) % self.CH + 1

    def _deps(self, eng, reads, writes):
        toks = []
        for k in reads:
            t = self.lastw.get(k)
            if t is not None:
                toks.append(t)
        for k in writes:
            t = self.lastw.get(k)
            if t is not None:
                toks.append(t)
            for t in self.readers.get(k, {}).values():
                if t[2] != eng:
                    toks.append(t)
        return toks

    def _waits(self, eng, toks):
        need = {}
        for (sem, val, _e, sid) in toks:
            if self.seen[eng].get(sid, 0) < val:
                if sid not in need or need[sid][1] < val:
                    need[sid] = (sem, val)
        for sid, (sem, val) in need.items():
            self.seen[eng][sid] = val
        return list(need.values())

    def _commit(self, tok, reads, writes):
        for k in writes:
            self.lastw[k] = tok
            self.readers[k] = {}
        for k in reads:
            self.readers.setdefault(k, {})[tok[3]] = tok

    def op(self, eng, fn, reads=(), writes=()):
        waits = self._waits(eng, self._deps(eng, reads, writes))
        self.n[eng] += 1
        sem, val = self._esem(eng, self.n[eng])
        tok = (sem, val, eng, ("e", eng, (self.n[eng] - 1) // self.CH))
        self.latest[eng] = tok

        def emit(E, waits=waits, fn=fn, sem=sem):
            for (s, v) in waits:
                E.wait_ge(s, v)
            fn(E).then_inc(sem, 1)

        self.q[eng].append(emit)
        self._commit(tok, reads, writes)
        return tok

    def dma(self, eng, out, in_, reads=(), writes=(), **kw):
        d = self.dnext % self.ND
        self.dnext += 1
        toks = self._deps(None, reads, writes)
        if self.dlast[d] is not None:
            toks.append(self.dlast[d])
        waits = self._waits(eng, toks)
        self.dcount[d] += 16
        sem = self.dsems[d]
        tok = (sem, self.dcount[d], "dma", ("d", d))
        self.dlast[d] = tok

        def emit(E, waits=waits, sem=sem, out=out, in_=in_, kw=kw):
            for (s, v) in waits:
                E.wait_ge(s, v)
            E.dma_start(out=out, in_=in_, **kw).then_inc(sem, 16)

        self.q[eng].append(emit)
        self._commit(tok, reads, writes)
        return tok

    def coll(self, ins, outs, groups, reads=(), writes=()):
        if not hasattr(self, "ccsem"):
            self.ccsem = self.stack.enter_context(self.nc.semaphore("ccsem"))
            self.ccn = 0
            self.cclast = None
        toks = self._deps(None, reads, writes)
        if self.cclast is not None:
            toks.append(self.cclast)
        waits = self._waits("pool", toks)
        self.ccn += 1
        sem = self.ccsem
        tok = (sem, self.ccn, "cc", ("cc",))
        self.cclast = tok
        self.latest["cc"] = tok
        ins = [a.opt() for a in ins]
        outs = [a.opt() for a in outs]

        def emit(E, waits=waits, sem=sem):
            for (s_, v) in waits:
                E.wait_ge(s_, v)
            E.collective_compute("AllGather", ALU.bypass, replica_groups=groups, ins=ins, outs=outs).then_inc(sem)

        self.q["pool"].append(emit)
        self._commit(tok, reads, writes)
        return tok

    def barrier(self, engines=None):
        toks = list(self.latest.values()) + [t for t in self.dlast if t is not None]
        for eng in (engines or self.ENG):
            waits = self._waits(eng, toks)
            if waits:
                def emit(E, waits=waits):
                    for (s, v) in waits:
                        E.wait_ge(s, v)
                self.q[eng].append(emit)
        self.lastw = {}
        self.readers = {}

    def emit(self):
        nc = self.nc
        with nc.Block() as blk:
            for eng, reg in (("sp", blk.sync), ("pe", blk.tensor), ("act", blk.scalar),
                             ("dve", blk.vector), ("pool", blk.gpsimd)):
                fns = self.q[eng]
                if fns:
                    reg(lambda E, fns=fns: [f(E) for f in fns])
        self.q = {e: [] for e in self.ENG}


class Pool2:
    CNT = [0]

    def __init__(self, nc, stack):
        self.nc = nc
        self.stack = stack

    def sb(self, name, shape, dt):
        Pool2.CNT[0] += 1
        return self.stack.enter_context(self.nc.sbuf_tensor(f"{name}_{Pool2.CNT[0]}", list(shape), dt))

    def ps(self, name, shape, dt=F32):
        Pool2.CNT[0] += 1
        return self.stack.enter_context(self.nc.psum_tensor(f"{name}_{Pool2.CNT[0]}", list(shape), dt))


A_FM_GROUPS = [("ckv", 128), ("idxk", 32), ("idxk_p", 32), ("foxk0", 128), ("foxk1", 128),
               ("sbk0", 128), ("sbk1", 128), ("kcks", 128), ("kcks_p", 128), ("kw", 64), ("kw_p", 64), ("vc", 64)]
A_NFM = sum(n for _, n in A_FM_GROUPS)
A_NTM = 512 + 132
KFM_ROWS = {"dsak": 0, "foxk": 256, "sbk": 512, "kcks": 768, "kw": 896, "vc": 960, "idxk": 1024}
KFM_N = 1152


def build_A(T, ctx=None):
    nc = ctx["nc"] if ctx else bass.Bass("TRN2", target_bir_lowering=False)
    NT = T // 512
    ov = ctx["ov"] if ctx else {}
    dr = lambda name, shape, dt, kind: ov[name] if name in ov else nc.dram_tensor(name, list(shape), dt, kind=kind).ap()
    xT = dr("xT", [1024, T], F32, "ExternalInput")
    pos = dr("pos", [1, T], I32, "ExternalInput")
    gcol = dr("gcol", [128, 8], F32, "ExternalInput")
    wfm = dr("wfm", [1024, A_NFM], F32, "ExternalInput")
    wtm = dr("wtm", [1024, A_NTM], F32, "ExternalInput")
    kvg = dr("kvg", [128, 1], F32, "ExternalInput")
    wuk2 = dr("wuk2", [128, 512], F32, "ExternalInput")
    wuv = dr("wuv", [128, 256], F32, "ExternalInput")
    foxb = dr("foxb", [128, 4], F32, "ExternalInput")
    rc = dr("rc", [128, 8], F32, "ExternalInput")
    hT_o = dr("hT", [1024, T], BF16, "ExternalOutput")
    kfm_o = dr("kfm", [KFM_N, T], BF16, "ExternalOutput")
    v3_o = dr("v3", [3, T, 260], BF16, "ExternalOutput")
    v2_o = dr("v2", [2, T, 65], BF16, "ExternalOutput")
    lf_o = dr("lf", [T, 4], F32, "ExternalOutput")
    tab_o = dr("tab", [4, 128, T], F32, "ExternalOutput")

    with ExitStack() as stack:
        P = ctx["P"] if ctx else Prog(nc, stack)
        M = Pool2(nc, stack)
        ones = M.sb("ones", [128, 128], BF16)
        g_sb = M.sb("g", [128, 8], F32)
        kvg_sb = M.sb("kvg", [128, 1], F32)
        rc_sb = M.sb("rc", [128, 8], F32)
        foxb_sb = M.sb("foxb", [128, 4], F32)
        nfoxb = M.sb("nfoxb", [128, 4], F32)
        wfm_sb = M.sb("wfm", [128, 8, A_NFM], BF16)
        wtm_sb = M.sb("wtm", [128, 8, A_NTM], BF16)
        wuk_sb = M.sb("wuk", [128, 512], BF16)
        wuv_sb = M.sb("wuv", [128, 256], BF16)
        stg = M.sb("stg", [128, 8, 512], F32)
        P.op("pool", lambda E: E.memset(ones[:], 1.0), writes=["ones"])
        P.dma("sp", g_sb[:], gcol, writes=["g"])
        P.dma("sp", kvg_sb[:], kvg, writes=["kvg"])
        P.dma("sp", rc_sb[:], rc, writes=["rc"])
        P.dma("sp", foxb_sb[:], foxb, writes=["foxb"])
        P.op("dve", lambda E: E.tensor_scalar(nfoxb[:], foxb_sb[:], -1.0, None, ALU.mult), reads=["foxb"], writes=["nfoxb"])
        def load_w(dst, src, ncols, key):
            c0 = 0
            while c0 < ncols:
                n = min(512, ncols - c0)
                P.dma("sp", stg[:, :, :n], src[:, c0:c0 + n].rearrange("(c p) n -> p c n", p=128), writes=["stg"])
                P.op("dve", lambda E, c0=c0, n=n: E.tensor_copy(dst[:, :, c0:c0 + n], stg[:, :, :n]), reads=["stg"], writes=[key])
                c0 += n
        load_w(wfm_sb, wfm, A_NFM, "wfm")
        load_w(wtm_sb, wtm, A_NTM, "wtm")
        P.dma("sp", stg[:, 0, :], wuk2, writes=["stg"])
        P.op("dve", lambda E: E.tensor_copy(wuk_sb[:], stg[:, 0, :]), reads=["stg"], writes=["wuk"])
        P.dma("sp", stg[:, 0, :256], wuv, writes=["stg"])
        P.op("dve", lambda E: E.tensor_copy(wuv_sb[:], stg[:, 0, :256]), reads=["stg"], writes=["wuv"])

        x_sb = M.sb("x", [128, 8, 512], F32)
        sq = M.sb("sq", [128, 8, 512], BF16)
        rstd = M.sb("rstd", [128, 512], F32)
        h_sb = M.sb("h", [128, 8, 512], BF16)
        posi = M.sb("posi", [128, 512], I32)
        posf = M.sb("posf", [128, 512], F32)
        ang = M.sb("ang", [128, 512], F32)
        tabs = M.sb("tabs", [128, 4, 512], F32)
        ckv = M.sb("ckv", [128, 512], F32)
        ckvn = M.sb("ckvn", [128, 512], BF16)
        t1 = M.sb("t1", [128, 512], F32)
        t2 = M.sb("t2", [128, 512], F32)
        ofm = [M.sb("ofm", [128, 512], BF16) for _ in range(3)]
        v3t = [M.sb("v3t", [128, 3, 4, 65], BF16) for _ in range(2)]
        v2t = [M.sb("v2t", [128, 2, 65], BF16) for _ in range(2)]
        lft = [M.sb("lft", [128, 4], F32) for _ in range(2)]
        lfe = M.sb("lfe", [128, 4], F32)
        pa = ctx["PS"][:6] if ctx else [M.ps("pa", [128, 512]) for _ in range(6)]
        pk = ["PS%d" % i for i in range(6)]
        for i in range(2):
            P.op("pool", lambda E, i=i: E.memset(v3t[i][:], 1.0), writes=["v3t%d" % i])
            P.op("pool", lambda E, i=i: E.memset(v2t[i][:], 1.0), writes=["v2t%d" % i])

        fm_off = {}
        o = 0
        for nme, n in A_FM_GROUPS:
            fm_off[nme] = (o, n)
            o += n
        ofm_i = [0]
        pa_i = [0]

        def next_pa():
            i = pa_i[0] % 6
            pa_i[0] += 1
            return pa[i], pk[i]

        def next_ofm():
            i = ofm_i[0] % 3
            ofm_i[0] += 1
            return ofm[i], "ofm%d" % i

        def fm_proj(grp):
            off, n = fm_off[grp]
            ps, key = next_pa()
            for c in range(8):
                P.op("pe", lambda E, c=c: E.matmul(ps[:n, :], lhsT=wfm_sb[:, c, off:off + n], rhs=h_sb[:, c, :],
                                                   start=(c == 0), stop=(c == 7)),
                     reads=["wfm", "h"], writes=[key])
            return ps, key

        def rope_out(grp, grp_p, n, tc, ts, row0, tt):
            ps1, k1 = fm_proj(grp)
            ps2, k2 = fm_proj(grp_p)
            rope_combine(ps1, k1, ps2, k2, n, tc, ts, row0, tt)

        def rope_combine(ps1, k1, ps2, k2, n, tc, ts, row0, tt):
            ot, ok = next_ofm()
            P.op("dve", lambda E: E.tensor_tensor(t1[:n, :], ps1[:n, :], tabs[:n, tc, :], ALU.mult), reads=[k1, "tabs"], writes=["t1"])
            P.op("dve", lambda E: E.tensor_tensor(t2[:n, :], ps2[:n, :], tabs[:n, ts, :], ALU.mult), reads=[k2, "tabs"], writes=["t2"])
            P.op("dve", lambda E: E.tensor_tensor(ot[:n, :], t1[:n, :], t2[:n, :], ALU.add), reads=["t1", "t2"], writes=[ok])
            P.dma("sp", kfm_o[row0:row0 + n, tt * 512:(tt + 1) * 512], ot[:n, :], reads=[ok])

        def plain_out(grp, n, row0, tt, eng="act"):
            ps, k = fm_proj(grp)
            ot, ok = next_ofm()
            if eng == "act":
                P.op("act", lambda E: E.copy(ot[:n, :], ps[:n, :]), reads=[k], writes=[ok])
            else:
                P.op("dve", lambda E: E.tensor_copy(ot[:n, :], ps[:n, :]), reads=[k], writes=[ok])
            P.dma("sp", kfm_o[row0:row0 + n, tt * 512:(tt + 1) * 512], ot[:n, :], reads=[ok])

        def do_tile(tt):
            tsl = slice(tt * 512, (tt + 1) * 512)
            P.dma("sp", x_sb[:], xT[:, tsl].rearrange("(c p) t -> p c t", p=128), writes=["x"])
            P.op("act", lambda E: E.activation(sq[:], x_sb[:], AF.Square), reads=["x"], writes=["sq"])
            ps, key = next_pa()
            for c in range(8):
                P.op("pe", lambda E, c=c, ps=ps: E.matmul(ps[:], lhsT=ones[:], rhs=sq[:, c, :], start=(c == 0), stop=(c == 7)),
                     reads=["ones", "sq"], writes=[key])
            P.op("act", lambda E, ps=ps: E.activation(rstd[:], ps[:], AF.Ln, bias=EPS, scale=1.0 / 1024), reads=[key], writes=["rstd"])
            P.op("act", lambda E: E.activation(rstd[:], rstd[:], AF.Exp, scale=-0.5), reads=["rstd"], writes=["rstd"])
            for c in range(8):
                P.op("dve", lambda E, c=c: E.scalar_tensor_tensor(h_sb[:, c, :], x_sb[:, c, :], g_sb[:, c:c + 1], rstd[:],
                                                                  ALU.mult, ALU.mult), reads=["x", "g", "rstd"], writes=["h"])
            P.dma("sp", hT_o[:, tsl].rearrange("(c p) t -> p c t", p=128), h_sb[:], reads=["h"])
            P.dma("sp", posi[:], pos[:, tsl].partition_broadcast(128), writes=["posi"])
            P.op("dve", lambda E: E.tensor_copy(posf[:], posi[:]), reads=["posi"], writes=["posf"])
            MAGIC = 12582912.0
            for (ci, ti) in ((0, 0), (3, 2)):
                for (which, shift) in ((0, 0.5 * PI), (1, 0.0)):
                    P.op("dve", lambda E, ci=ci, shift=shift: E.tensor_scalar(ang[:], posf[:], rc_sb[:, ci:ci + 1], shift, ALU.mult, ALU.add),
                         reads=["posf", "rc"], writes=["ang"])
                    P.op("dve", lambda E: E.tensor_scalar(t1[:], ang[:], 1.0 / (2 * PI), MAGIC, ALU.mult, ALU.add), reads=["ang"], writes=["t1"])
                    P.op("dve", lambda E: E.tensor_scalar(t1[:], t1[:], MAGIC, -2 * PI, ALU.subtract, ALU.mult), reads=["t1"], writes=["t1"])
                    P.op("dve", lambda E: E.tensor_tensor(ang[:], ang[:], t1[:], ALU.add), reads=["ang", "t1"], writes=["ang"])
                    P.op("dve", lambda E: E.tensor_scalar(ang[:], ang[:], PI, -PI, ALU.min, ALU.max), reads=["ang"], writes=["ang"])
                    if which == 0:
                        P.op("act", lambda E, ti=ti: E.activation(tabs[:, ti, :], ang[:], AF.Sin), reads=["ang"], writes=["tabs"])
                    else:
                        P.op("act", lambda E, ci=ci, ti=ti: E.activation(tabs[:, ti + 1, :], ang[:], AF.Sin, scale=rc_sb[:, ci + 1:ci + 2]),
                             reads=["ang", "rc"], writes=["tabs"])
            P.dma("sp", tab_o[:, :, tsl].rearrange("k p t -> p k t"), tabs[:], reads=["tabs"])
            ps, key = fm_proj("ckv")
            P.op("act", lambda E, ps=ps: E.copy(ckv[:], ps[:]), reads=[key], writes=["ckv"])
            P.op("act", lambda E: E.activation(sq[:, 0, :], ckv[:], AF.Square), reads=["ckv"], writes=["sq"])
            ps2, key2 = next_pa()
            P.op("pe", lambda E, ps2=ps2: E.matmul(ps2[:], lhsT=ones[:], rhs=sq[:, 0, :], start=True, stop=True), reads=["ones", "sq"], writes=[key2])
            P.op("act", lambda E, ps2=ps2: E.activation(t1[:], ps2[:], AF.Ln, bias=EPS, scale=1.0 / 128), reads=[key2], writes=["t1"])
            P.op("act", lambda E: E.activation(t1[:], t1[:], AF.Exp, scale=-0.5), reads=["t1"], writes=["t1"])
            P.op("dve", lambda E: E.scalar_tensor_tensor(ckvn[:], ckv[:], kvg_sb[:, 0:1], t1[:], ALU.mult, ALU.mult),
                 reads=["ckv", "kvg", "t1"], writes=["ckvn"])
            for hp in range(2):
                psa, ka = next_pa()
                psb, kb = next_pa()
                P.op("pe", lambda E, hp=hp, psa=psa: E.matmul(psa[:], lhsT=wuk_sb[:, hp * 128:(hp + 1) * 128], rhs=ckvn[:], start=True, stop=True),
                     reads=["wuk", "ckvn"], writes=[ka])
                P.op("pe", lambda E, hp=hp, psb=psb: E.matmul(psb[:], lhsT=wuk_sb[:, 256 + hp * 128:256 + (hp + 1) * 128], rhs=ckvn[:], start=True, stop=True),
                     reads=["wuk", "ckvn"], writes=[kb])
                rope_combine(psa, ka, psb, kb, 128, 0, 1, KFM_ROWS["dsak"] + hp * 128, tt)
            rope_out("idxk", "idxk_p", 32, 2, 3, KFM_ROWS["idxk"], tt)
            plain_out("foxk0", 128, KFM_ROWS["foxk"], tt, "act")
            plain_out("foxk1", 128, KFM_ROWS["foxk"] + 128, tt, "dve")
            plain_out("sbk0", 128, KFM_ROWS["sbk"], tt, "act")
            plain_out("sbk1", 128, KFM_ROWS["sbk"] + 128, tt, "dve")
            rope_out("kcks", "kcks_p", 128, 0, 1, KFM_ROWS["kcks"], tt)
            rope_out("kw", "kw_p", 64, 0, 1, KFM_ROWS["kw"], tt)
            plain_out("vc", 64, KFM_ROWS["vc"], tt, "act")
            for st in range(4):
                tm_part(tt, st)

        def tm_part(tt, st):
            if True:
                tok0 = tt * 512 + st * 128
                bi = (tt * 4 + st) % 2
                v3, v3k = v3t[bi], "v3t%d" % bi
                v2, v2k = v2t[bi], "v2t%d" % bi
                lf, lfk = lft[bi], "lft%d" % bi
                hs = slice(st * 128, (st + 1) * 128)
                ps_v, kv = next_pa()
                P.op("pe", lambda E, ps_v=ps_v: E.matmul(ps_v[:, :256], lhsT=ckvn[:, hs], rhs=wuv_sb[:], start=True, stop=True),
                     reads=["ckvn", "wuv"], writes=[kv])
                P.op("act", lambda E, ps_v=ps_v, v3=v3: E.copy(v3[:, 0, :, 0:64], ps_v[:, :256].rearrange("p (h d) -> p h d", d=64)),
                     reads=[kv], writes=[v3k])
                ps_a, kaa = next_pa()
                for c in range(8):
                    P.op("pe", lambda E, c=c, ps_a=ps_a: E.matmul(ps_a[:], lhsT=h_sb[:, c, hs], rhs=wtm_sb[:, c, 0:512], start=(c == 0), stop=(c == 7)),
                         reads=["h", "wtm"], writes=[kaa])
                P.op("dve", lambda E, ps_a=ps_a, v3=v3: E.tensor_copy(v3[:, 1:3, :, 0:64], ps_a[:].rearrange("p (b h d) -> p b h d", b=2, d=64)),
                     reads=[kaa], writes=[v3k])
                ps_b, kbb = next_pa()
                for c in range(8):
                    P.op("pe", lambda E, c=c, ps_b=ps_b: E.matmul(ps_b[:, :132], lhsT=h_sb[:, c, hs], rhs=wtm_sb[:, c, 512:644], start=(c == 0), stop=(c == 7)),
                         reads=["h", "wtm"], writes=[kbb])
                P.op("act", lambda E, ps_b=ps_b, v2=v2: E.copy(v2[:, :, 0:64], ps_b[:, :128].rearrange("p (b d) -> p b d", d=64)),
                     reads=[kbb], writes=[v2k])
                P.op("dve", lambda E, ps_b=ps_b: E.tensor_tensor(lfe[:], ps_b[:, 128:132], nfoxb[:], ALU.subtract), reads=[kbb, "nfoxb"], writes=["lfe"])
                P.op("act", lambda E: E.activation(lfe[:], lfe[:], AF.Exp, scale=-1.0), reads=["lfe"], writes=["lfe"])
                P.op("act", lambda E, lf=lf: E.activation(lf[:], lfe[:], AF.Ln, bias=1.0), reads=["lfe"], writes=[lfk])
                P.dma("sp", v3_o[:, tok0:tok0 + 128, :].rearrange("b t n -> t b n"), v3[:].rearrange("p b h d -> p b (h d)"), reads=[v3k])
                P.dma("sp", v2_o[:, tok0:tok0 + 128, :].rearrange("b t n -> t b n"), v2[:], reads=[v2k])
                P.dma("sp", lf_o[tok0:tok0 + 128, :], lf[:], reads=[lfk])
        for tt in range(NT):
            do_tile(tt)
        P.barrier()
        P.emit()
    return nc


IN_LAYOUT = (
    ('dsa_q', 256), ('dsa_ckv', 128), ('idx_q', 128), ('idx_k', 32), ('idx_w', 4), ('dsa_z', 256),
    ('fox_q', 256), ('fox_k', 256), ('fox_v', 256), ('fox_f', 4), ('fox_z', 256),
    ('sb_q', 256), ('sb_k', 256), ('sb_v', 256), ('sb_z', 256),
    ('nsa_q', 256), ('nsa_kc', 64), ('nsa_vc', 64), ('nsa_ks', 64), ('nsa_vs', 64),
    ('nsa_kw', 64), ('nsa_vw', 64), ('nsa_g', 12), ('nsa_z', 256),
    ('mem_q', 256), ('mem_z', 256),
    ('merge', 5 * 1024),
)
COL = {}
_o = 0
for _n, _w in IN_LAYOUT:
    COL[_n] = (_o, _w)
    _o += _w
N_IN = _o


def _cols(w_in, name):
    o, n = COL[name]
    return w_in[:, o:o + n]


def _perm_idx(ncols, dh):
    idx = np.arange(ncols)
    base = (idx // dh) * dh
    return base + (idx % dh + dh // 2) % dh


def _rope_consts():
    p = np.arange(128)
    rc = np.zeros((128, 8), np.float32)
    rc[:, 0] = 10000.0 ** (-(2.0 * (p % 32)) / 64.0)
    s64 = np.where((p % 64) < 32, -1.0, 1.0)
    rc[:, 1] = s64
    rc[:, 2] = -PI * s64
    rc[:, 3] = 10000.0 ** (-(2.0 * (p % 16)) / 32.0)
    s32 = np.where((p % 32) < 16, -1.0, 1.0)
    rc[:, 4] = s32
    rc[:, 5] = -PI * s32
    return rc


def host_prep_A(norm_g, w_in, kv_norm, w_uk, w_uv, fox_bias):
    c = lambda n: _cols(w_in, n)
    kcks = np.concatenate([c('nsa_kc'), c('nsa_ks')], 1)
    wfm = np.concatenate([
        c('dsa_ckv'), c('idx_k'), c('idx_k')[:, _perm_idx(32, 32)],
        c('fox_k'), c('sb_k'), kcks, kcks[:, _perm_idx(128, 64)],
        c('nsa_kw'), c('nsa_kw')[:, _perm_idx(64, 64)], c('nsa_vc')], 1)
    wtm = np.concatenate([c('fox_v'), c('sb_v'), c('nsa_vs'), c('nsa_vw'), c('fox_f')], 1)
    assert wfm.shape[1] == A_NFM and wtm.shape[1] == A_NTM
    return {
        "gcol": np.ascontiguousarray(norm_g.reshape(8, 128).T),
        "wfm": np.ascontiguousarray(wfm), "wtm": np.ascontiguousarray(wtm),
        "kvg": np.ascontiguousarray(kv_norm.reshape(128, 1)),
        "wuk2": np.ascontiguousarray(np.concatenate([w_uk, w_uk[:, _perm_idx(256, 64)]], 1)),
        "wuv": np.ascontiguousarray(w_uv),
        "foxb": np.ascontiguousarray(np.broadcast_to(fox_bias.reshape(1, 4), (128, 4))),
        "rc": _rope_consts(),
    }


B_WQ = {"dsa": (0, 512), "idx": (512, 256), "fox": (768, 256), "sb": (1024, 256), "nsa": (1280, 512), "mem": (1792, 256)}
B_NWQ = 2048
NEG_BIG = -1.0e30
BIS_R0 = 512.0
BIS_K = 20


def build_B(T, ctx=None):
    nc = ctx["nc"] if ctx else bass.Bass("TRN2", target_bir_lowering=False)
    ov = ctx["ov"] if ctx else {}
    G = bool(ctx)
    FINAL = ctx["final"] if ctx else True
    S = 4 * T
    NB = S // 128
    NQ = T // 128
    NG = T // 512
    NBS = S // 64
    NCMP = S // 16 - 1
    NCC = (NCMP + 127) // 128
    NCP = NCC * 128
    KSEL = min(256, S // 4)
    SCALE = 0.125
    dr = lambda name, shape, dt, kind="ExternalInput": ov[name] if name in ov else nc.dram_tensor(name, list(shape), dt, kind=kind).ap()
    xT = dr("xT", [1024, T], F32)
    hT = dr("hT", [1024, T], BF16)
    tab = dr("tab", [4, 128, T], F32)
    kfm = dr("kfm", [KFM_N, S], BF16)
    v3 = dr("v3", [3, S, 260], BF16)
    v2 = dr("v2", [2, S, 65], BF16)
    lf = dr("lf", [128, NB * 4], F32)
    wq = dr("wq", [1024, B_NWQ], F32)
    wsm = dr("wsm", [1024, 16], F32)
    wz = dr("wz", [1024, 1280], F32)
    wmg = dr("wmg", [1024, 5120], F32)
    wbr = dr("wbr", [5, 256, 1024], F32)
    wout = dr("wout", [1024, 1024], F32)
    w1 = dr("w1", [128, 32 * 128], F32)
    w2k2 = dr("w2k2", [128, 128], F32)
    w2v = dr("w2v", [128, 64], F32)
    peT = dr("peT", [128, 32], F32)
    memT = dr("memT", [1024, 256], F32)
    mgcol = dr("mgcol", [128, 8], F32)
    wmem = dr("wmem", [1024, 512], F32)
    fgcol = dr("fgcol", [128, 8], F32)
    c_bf = dr("c_bf", [128, 128 * 3 + 512 * 2 + 1024 + 512], BF16)
    c_f = dr("c_f", [128, 128 * 3 + 512 + 4 + 4 + NQ + NCC + NCP], F32)
    trow = dr("trow", [1, T], F32)
    visd = dr("vis", [128, NQ * NBS], F32)
    addd = dr("addc", [128, NQ * NBS], F32)
    xo_o = dr("xo", [1024, T], F32, "ExternalOutput")
    xn_o = dr("xn", [1024, T], F32, "ExternalOutput")
    ydram = dr("ydram", [NQ, 128, 1280], BF16, "ExternalOutput")

    with ExitStack() as stack:
        P = ctx["P"] if ctx else Prog(nc, stack)
        M = Pool2(nc, stack)
        PS = ctx["PS"] if ctx else [M.ps("ps%d" % i, [128, 512]) for i in range(8)]
        PK = ["PS%d" % i for i in range(8)]

        def pe(out, lhsT, rhs, st, sp, r, w):
            P.op("pe", lambda E: E.matmul(out, lhsT=lhsT, rhs=rhs, start=st, stop=sp), reads=r, writes=w)

        def tr(out, in_, r, w, f32=False):
            idn = identF if f32 else ident
            P.op("pe", lambda E: E.matmul(out, lhsT=in_, rhs=idn, start=True, stop=True), reads=list(r) + ["cbf", "cf"], writes=w)

        def act(out, in_, func, r, w, **kw):
            P.op("act", lambda E: E.activation(out, in_, func, **kw), reads=r, writes=w)

        def tt(eng, out, a, b, op, r, w):
            P.op(eng, lambda E: E.tensor_tensor(out, a, b, op), reads=r, writes=w)

        def ts(eng, out, a, s1, s2, op0, op1, r, w, accum=None):
            if accum is None:
                if op1 is None:
                    P.op(eng, lambda E: E.tensor_scalar(out, a, s1, s2, op0), reads=r, writes=w)
                else:
                    P.op(eng, lambda E: E.tensor_scalar(out, a, s1, s2, op0, op1), reads=r, writes=w)
            else:
                P.op(eng, lambda E: E.tensor_scalar(out, a, s1, s2, op0, op1, accum_out=accum), reads=r, writes=w)

        def stt(eng, out, a, s, b, op0, op1, r, w, accum=None):
            if accum is None:
                P.op(eng, lambda E: E.scalar_tensor_tensor(out, a, s, b, op0, op1), reads=r, writes=w)
            else:
                P.op(eng, lambda E: E.scalar_tensor_tensor(out, a, s, b, op0, op1, accum_out=accum), reads=r, writes=w)

        def cp(eng, out, in_, r, w):
            if eng == "act":
                P.op("act", lambda E: E.copy(out, in_), reads=r, writes=w)
            else:
                P.op(eng, lambda E: E.tensor_copy(out, in_), reads=r, writes=w)

        def mset(out, val, w, eng="pool"):
            P.op(eng, lambda E: E.memset(out, val), writes=w)

        cbf = M.sb("cbf", [128, 128 * 3 + 512 * 2 + 1024 + 512], BF16)
        cf = M.sb("cf", [128, 128 * 3 + 512 + 4 + 4 + NQ + NCC + NCP], F32)
        P.dma("sp", cbf[:], c_bf, writes=["cbf"])
        P.dma("sp", cf[:], c_f, writes=["cf"])
        ident = cbf[:, 0:128]
        triS = cbf[:, 128:256]
        ones_b = cbf[:, 256:384]
        dmask = cbf[:, 384:896].rearrange("p (j q) -> p j q", q=128)
        dmaskS = cbf[:, 896:1408].rearrange("p (j q) -> p j q", q=128)
        wmask = cbf[:, 1408:2432].rearrange("p (j q) -> p j q", q=128)
        dmaskN01 = cbf[:, 2432:2944]
        o = 0
        triInc = cf[:, o:o + 128]; o += 128
        ones_f = cf[:, o:o + 128]; o += 128
        identF = cf[:, o:o + 128]; o += 128
        dmaskN = cf[:, o:o + 512]; o += 512
        oh = cf[:, o:o + 4]; o += 4
        sel4 = cf[:, o:o + 4]; o += 4
        tq = cf[:, o:o + NQ]; o += NQ
        ncol = cf[:, o:o + NCC]; o += NCC
        nrow = cf[:, o:o + NCP]; o += NCP
        stg = M.sb("stg", [128, 8, 256], F32)
        cumcol = M.sb("cumcol", [128, 4, NB], F32)
        CPt = M.sb("CPt", [128, 4, NQ], F32)
        kc2T = M.sb("kc2T", [128, NCP], BF16)
        vc1 = M.sb("vc1", [128, NCC, 65], BF16)
        mkT = M.sb("mkT", [128, 2, 256], BF16)
        mv1 = M.sb("mv1", [128, 2, 4, 65], BF16)
        mset(vc1[:], 1.0, ["vc1"])
        mset(mv1[:], 1.0, ["mv1"])

        def ld_kfm(dst, row0, nrows, dkey):
            if not G:
                P.dma("sp", dst, kfm[row0:row0 + nrows, :], writes=[dkey])
            else:
                dv = dst.rearrange("p (i r q) -> p i r q", r=4, q=128)
                ch, off = row0 // 128, row0 % 128
                for r in range(4):
                    P.dma("sp", dv[:, :, r, :], kfm[ch][r * 128 + off:r * 128 + off + nrows, :].rearrange("p (i q) -> p i q", q=128),
                          writes=[dkey])

        def ld_v(dst, src, k, nk, dkey):
            if not G:
                P.dma("sp", dst, src[k].rearrange("(j s) n -> s j n", s=128), writes=[dkey])
            else:
                dv = dst.rearrange("s (i r) n -> s i r n", r=4)
                hq_ = NQ // 2
                for hf in range(2):
                    for r in range(4):
                        P.dma("sp", dv[:, hf * hq_:(hf + 1) * hq_, r, :],
                              src[k][hf][r * (T // 2):(r + 1) * (T // 2), :].rearrange("(i s) n -> s i n", s=128), writes=[dkey])

        def load_w(dst, dkey, src_cols, ncols, kchunks=8):
            c0 = 0
            while c0 < ncols:
                n = min(256, ncols - c0)
                P.dma("sp", stg[:, :kchunks, :n], src_cols(c0, n).rearrange("(c p) n -> p c n", p=128), writes=["stg"])
                P.op("dve", lambda E, c0=c0, n=n: E.tensor_copy(dst[:, :, c0:c0 + n], stg[:, :kchunks, :n]), reads=["stg"], writes=[dkey])
                c0 += n

        def load_small(dst, dkey, src, n):
            P.dma("sp", stg[:, 0, :n], src, writes=["stg"])
            P.op("dve", lambda E: E.tensor_copy(dst, stg[:, 0, :n]), reads=["stg"], writes=[dkey])

        with ExitStack() as st0:
            M0 = Pool2(nc, st0)
            lf_sb = M0.sb("lf", [128, NB * 4], F32)
            tot = M0.sb("tot", [128, NB * 4], F32)
            incl = M0.sb("incl", [128, 4, NB], F32)
            tmpc = M0.sb("tmpc", [128, 4, NB], F32)
            tmp4 = M0.sb("tmp4", [128, NQ, 4], F32)
            if not G:
                P.dma("sp", lf_sb[:], lf, writes=["lf"])
            else:
                lv = lf_sb[:].rearrange("s (i r h) -> s i r h", r=4, h=4)
                for r in range(4):
                    P.dma("sp", lv[:, :, r, :], lf[r * T:(r + 1) * T, :].rearrange("(i s) h -> s i h", s=128), writes=["lf"])
            pe(PS[4][:, :NB * 4], triInc, lf_sb[:], True, True, ["cf", "lf"], [PK[4]])
            pe(PS[6][:, :NB * 4], ones_f, lf_sb[:], True, True, ["cf", "lf"], [PK[6]])
            cp("act", tot[:], PS[6][:, :NB * 4], [PK[6]], ["tot"])
            tot3 = tot[:].rearrange("p (j h) -> p j h", h=4)
            cs3 = PS[4][:, :NB * 4].rearrange("p (j h) -> p j h", h=4)
            for h in range(4):
                P.op("dve", lambda E, h=h: E.tensor_tensor_scan(incl[:, h, :], ones_f[:, :NB], tot3[:, :, h], 0.0, ALU.mult, ALU.add),
                     reads=["cf", "tot"], writes=["incl"])
                tt("dve", tmpc[:, h, :], incl[:, h, :], tot3[:, :, h], ALU.subtract, ["incl", "tot"], ["tmpc"])
                tt("dve", cumcol[:, h, :], cs3[:, :, h], tmpc[:, h, :], ALU.add, [PK[4], "tmpc"], ["cumcol"])
                tt("dve", tmp4[:], incl[:, h, :].rearrange("p (i j) -> p i j", j=4), oh.unsqueeze(1).to_broadcast([128, NQ, 4]),
                   ALU.mult, ["incl", "cf"], ["tmp4"])
                P.op("dve", lambda E, h=h: E.tensor_reduce(CPt[:, h, :], tmp4[:], AX.X, ALU.add), reads=["tmp4"], writes=["CPt"])
            kvtok = M0.sb("kvtok", [128, S], BF16)
            W1 = M0.sb("W1", [128, 32, 128], BF16)
            w2k2_sb = M0.sb("w2k2", [128, 128], BF16)
            w2v_sb = M0.sb("w2v", [128, 64], BF16)
            peT_sb = M0.sb("peT", [128, 32], BF16)
            bH = M0.sb("bH", [128, 2], F32)
            hk = M0.sb("hk", [128, NCP], BF16)
            hv = M0.sb("hv", [128, NCP], BF16)
            ld_kfm(kvtok[0:64, :], KFM_ROWS["kcks"], 64, "kvtok")
            ld_kfm(kvtok[64:128, :], KFM_ROWS["vc"], 64, "kvtok")
            stg2 = stg[:].rearrange("p c n -> p (c n)")
            for half in range(2):
                P.dma("sp", stg2, w1[:, half * 2048:(half + 1) * 2048], writes=["stg"])
                P.op("dve", lambda E, half=half: E.tensor_copy(W1[:, half * 16:(half + 1) * 16, :].rearrange("p l h -> p (l h)"), stg2),
                     reads=["stg"], writes=["W1"])
            load_small(w2k2_sb[:], "w2k2", w2k2, 128)
            load_small(w2v_sb[:], "w2v", w2v, 64)
            load_small(peT_sb[:], "peT", peT, 32)
            mset(hk[:], 0.0, ["hk"])
            mset(hv[:], 0.0, ["hv"])
            for kv_i, (lo, hbuf, hkey) in enumerate(((0, hk, "hk"), (64, hv, "hv"))):
                for l in range(32):
                    pe(PS[4][:, kv_i:kv_i + 1], W1[lo:lo + 64, l, :], peT_sb[lo:lo + 64, l:l + 1], l == 0, l == 31, ["W1", "peT"], [PK[4]])
            cp("act", bH[:], PS[4][:, 0:2], [PK[4]], ["bH"])
            for kv_i, (lo, hbuf, hkey) in enumerate(((0, hk, "hk"), (64, hv, "hv"))):
                for l in range(32):
                    pe(PS[6][:, :NCMP], W1[lo:lo + 64, l, :], kvtok[lo:lo + 64, l:l + 16 * (NCMP - 1) + 1:16], l == 0, l == 31,
                       ["W1", "kvtok"], [PK[6]])
                act(hbuf[:, :NCMP], PS[6][:, :NCMP], AF.Silu, [PK[6], "bH"], [hkey], bias=bH[:, kv_i:kv_i + 1])
            pe(PS[4][:, :NCP], w2k2_sb[:], hk[:], True, True, ["w2k2", "hk"], [PK[4]])
            cp("act", kc2T[:], PS[4][:, :NCP], [PK[4]], ["kc2T"])
            for c in range(NCC):
                pe(PS[6][:, c * 64:(c + 1) * 64], hv[:, c * 128:(c + 1) * 128], w2v_sb[:], True, True, ["hv", "w2v"], [PK[6]])
            cp("act", vc1[:, :, 0:64], PS[6][:, :NCC * 64].rearrange("p (c d) -> p c d", d=64), [PK[6]], ["vc1"])
            xm = M0.sb("xm", [128, 8, 256], F32)
            sqm = M0.sb("sqm", [128, 8, 256], BF16)
            mh = M0.sb("mh", [128, 8, 256], BF16)
            rsm = M0.sb("rsm", [128, 256], F32)
            mg_sb = M0.sb("mg", [128, 8], F32)
            wmem_sb = M0.sb("wmem", [128, 8, 512], BF16)
            P.dma("sp", xm[:], memT.rearrange("(c p) t -> p c t", p=128), writes=["xm"])
            P.dma("sp", mg_sb[:], mgcol, writes=["mg"])
            load_w(wmem_sb, "wmem", lambda c0, n: wmem[:, c0:c0 + n], 512)
            act(sqm[:], xm[:], AF.Square, ["xm"], ["sqm"])
            for c in range(8):
                pe(PS[4][:, :256], ones_b, sqm[:, c, :], c == 0, c == 7, ["cbf", "sqm"], [PK[4]])
            act(rsm[:], PS[4][:, :256], AF.Ln, [PK[4]], ["rsm"], bias=EPS, scale=1.0 / 1024)
            act(rsm[:], rsm[:], AF.Exp, ["rsm"], ["rsm"], scale=-0.5)
            for c in range(8):
                stt("dve", mh[:, c, :], xm[:, c, :], mg_sb[:, c:c + 1], rsm[:], ALU.mult, ALU.mult, ["xm", "mg", "rsm"], ["mh"])
            for hp in range(2):
                for c in range(8):
                    pe(PS[6][:, :256], wmem_sb[:, c, hp * 128:(hp + 1) * 128], mh[:, c, :], c == 0, c == 7, ["wmem", "mh"], [PK[6]])
                cp("act", mkT[:, hp, :], PS[6][:, :256], [PK[6]], ["mkT"])
            for mc in range(2):
                for c in range(8):
                    pe(PS[4][:, :256], mh[:, c, mc * 128:(mc + 1) * 128], wmem_sb[:, c, 256:512], c == 0, c == 7, ["mh", "wmem"], [PK[4]])
                cp("act", mv1[:, mc, :, 0:64], PS[4][:, :256].rearrange("p (h d) -> p h d", d=64), [PK[4]], ["mv1"])
            P.barrier()
            P.emit()

        with ExitStack() as st1:
            M1 = Pool2(nc, st1)
            KT = M1.sb("KT", [128, 2, S], BF16)
            V1 = M1.sb("V1", [128, NB, 260], BF16)
            kiT4 = M1.sb("kiT4", [128, S], BF16)
            sc = M1.sb("sc", [128, S], F32)
            maskT = M1.sb("maskT", [128, NB, 128], BF16)
            junk = maskT[:].rearrange("p j q -> p (j q)")
            wqb = M1.sb("wqb", [128, 8, 512], BF16)
            wsm_sb = M1.sb("wsm", [128, 8, 16], BF16)
            wqb2 = M1.sb("wqb2", [128, 8, 256], BF16)
            hq = M1.sb("hq", [128, 8, 512], BF16)
            tabg = M1.sb("tabg", [128, 2, 512], F32)
            QT = M1.sb("QT", [128, 2, 512], BF16)
            qiT = M1.sb("qiT", [128, 512], BF16)
            qm = M1.sb("qm", [128, 4, 512], BF16)
            t1 = M1.sb("t1", [128, 512], F32)
            t2 = M1.sb("t2", [128, 512], F32)
            PT = [M1.sb("PT", [128, 4, 128], BF16) for _ in range(2)]
            ebuf = M1.sb("ebuf", [128, 512], F32)
            ubuf = M1.sb("ubuf", [128, 4, 128], BF16)
            Rt = M1.sb("Rt", [128, 128], F32)
            Bih = [M1.sb("Bih", [128, NB], F32) for _ in range(2)]
            rd = M1.sb("rd", [128, 4], F32)
            ybuf = [M1.sb("ybuf", [128, 4, 64], BF16) for _ in range(2)]
            rbuf = [M1.sb("rbuf", [128, 512], F32) for _ in range(2)]
            wsmall = M1.sb("wsmall", [128, 16], F32)
            g12 = M1.sb("g12", [128, 12], F32)
            mid = M1.sb("mid", [128, 1], F32)
            cnt = M1.sb("cnt", [128, 1], F32)
            tmpb = M1.sb("tmpb", [128, 1], F32)
            ec = t1[:, :NCP]
            impacc = t2[:, :NCP]
            cmaskN = ebuf[:, :NCP]
            cmaskT = M1.sb("cmaskT", [128, NCC, 128], BF16)
            eT = M1.sb("eT", [128, NCC, 128], BF16)
            rs4 = M1.sb("rs4", [128, 4], F32)
            imp4 = M1.sb("imp4", [128, NBS], F32)
            imp2 = M1.sb("imp2", [128, NBS], F32)
            m8a = M1.sb("m8a", [128, 8], F32)
            m8b = M1.sb("m8b", [128, 8], F32)
            bm = M1.sb("bm", [128, NBS], F32)
            vis_t = M1.sb("vis_t", [128, NBS], F32)
            add_t = M1.sb("add_t", [128, NBS], F32)
            trow_t = Rt
            yacc = M1.sb("yacc", [128, 4, 64], F32)
            ytmp = M1.sb("ytmp", [128, 4, 64], F32)
            coef = M1.sb("coef", [128, 4], F32)
            ctr = {"ps": 0, "pt": 0, "po": 0, "y": 0, "b": 0, "r": 0}

            def nxt(name, n):
                v = ctr[name] % n
                ctr[name] += 1
                return v

            def load_hq(g):
                P.dma("sp", hq[:], hT[:, g * 512:(g + 1) * 512].rearrange("(c p) t -> p c t", p=128), writes=["hq"])

            def fm_q(ps_i, col0, wt=None, wkey="wqb"):
                wt = wqb if wt is None else wt
                for c in range(8):
                    pe(PS[ps_i][:], wt[:, c, col0:col0 + 128], hq[:, c, :], c == 0, c == 7, [wkey, "hq"], [PK[ps_i]])

            def load_tabs(g, k0):
                P.dma("sp", tabg[:], tab[k0:k0 + 2, :, g * 512:(g + 1) * 512].rearrange("k p t -> p k t"), writes=["tabg"])

            def make_QT(g, rope):
                if rope:
                    load_tabs(g, 0)
                for hp in range(2):
                    fm_q(4, hp * 128)
                    if rope:
                        fm_q(5, 256 + hp * 128)
                        tt("dve", t1[:], PS[4][:], tabg[:, 0, :], ALU.mult, [PK[4], "tabg"], ["t1"])
                        tt("dve", t2[:], PS[5][:], tabg[:, 1, :], ALU.mult, [PK[5], "tabg"], ["t2"])
                        tt("dve", QT[:, hp, :], t1[:], t2[:], ALU.add, ["t1", "t2"], ["QT"])
                    else:
                        cp("act", QT[:, hp, :], PS[4][:], [PK[4]], ["QT"])

            def qslice(h, qi):
                lo = (h % 2) * 64
                return QT[lo:lo + 64, h // 2, qi * 128:(qi + 1) * 128]

            def finalize(po_i, i, bidx):
                po3 = PS[po_i][:, :260].rearrange("p (h d) -> p h d", d=65)
                yb = nxt("y", 2)
                ts("dve", rd[:], po3[:, :, 64], 1e-30, None, ALU.max, None, [PK[po_i]], ["rd"])
                P.op("dve", lambda E: E.reciprocal(rd[:], rd[:]), reads=["rd"], writes=["rd"])
                tt("dve", ybuf[yb][:], po3[:, :, 0:64], rd[:].unsqueeze(2).to_broadcast([128, 4, 64]), ALU.mult,
                   [PK[po_i], "rd"], ["ybuf%d" % yb])
                P.dma("sp", ydram[i, :, bidx * 256:(bidx + 1) * 256], ybuf[yb][:].rearrange("p h d -> p (h d)"), reads=["ybuf%d" % yb], writes=["ydram"])

            def load_branch_kv(krow0, vidx):
                for hp in range(2):
                    ld_kfm(KT[:, hp, :], krow0 + hp * 128, 128, "KT")
                ld_v(V1[:], v3, vidx, 3, "V1")

            def kslice(h, j):
                lo = (h % 2) * 64
                return KT[lo:lo + 64, h // 2, j * 128:(j + 1) * 128]

            def fox_tile(g, qi):
                i = 4 * g + qi
                po_i = 2 + nxt("po", 2)
                for h in range(4):
                    b = nxt("b", 2)
                    bk = "Bih%d" % b
                    nblk = 4 * (i + 1)
                    ts("dve", Bih[b][:, :nblk], cumcol[:, h, :nblk], CPt[:, h, i:i + 1], 0.0, ALU.subtract, ALU.min,
                       ["cumcol", "CPt"], [bk])
                    for gg in range(i + 1):
                        ps_i = nxt("ps", 2)
                        p_i = nxt("pt", 2)
                        psv = PS[ps_i][:].rearrange("p (j q) -> p j q", q=128)
                        for jj in range(4):
                            pe(psv[:, jj, :], kslice(h, 4 * gg + jj), qslice(h, qi), True, True, ["KT", "QT"], [PK[ps_i]])
                        for jj in range(4):
                            act(PT[p_i][:, jj, :], psv[:, jj, :], AF.Exp, [PK[ps_i], bk], ["PT%d" % p_i],
                                bias=Bih[b][:, 4 * gg + jj:4 * gg + jj + 1], scale=SCALE)
                        if gg == i:
                            tt("dve", PT[p_i][:], PT[p_i][:], dmask, ALU.mult, ["PT%d" % p_i, "cbf"], ["PT%d" % p_i])
                        for jj in range(4):
                            pe(PS[po_i][:, h * 65:(h + 1) * 65], PT[p_i][:, jj, :], V1[:, 4 * gg + jj, h * 65:(h + 1) * 65],
                               gg == 0 and jj == 0, gg == i and jj == 3, ["PT%d" % p_i, "V1"], [PK[po_i]])
                finalize(po_i, i, 1)

            def sb_tile(g, qi):
                i = 4 * g + qi
                po_i = 2 + nxt("po", 2)
                for h in range(4):
                    mset(Rt[:], 0.0, ["Rt"])
                    for gg in range(i, -1, -1):
                        ps_i = nxt("ps", 2)
                        p_i = nxt("pt", 2)
                        psv = PS[ps_i][:].rearrange("p (j q) -> p j q", q=128)
                        ps6 = PS[6][:].rearrange("p (j q) -> p j q", q=128)
                        for jj in range(4):
                            pe(psv[:, jj, :], kslice(h, 4 * gg + jj), qslice(h, qi), True, True, ["KT", "QT"], [PK[ps_i]])
                        act(ebuf[:], PS[ps_i][:], AF.Exp, [PK[ps_i]], ["ebuf"], scale=SCALE)
                        act(ubuf[:].rearrange("p j q -> p (j q)"), ebuf[:], AF.Ln, ["ebuf"], ["ubuf"], bias=1.0)
                        if gg == i:
                            tt("dve", ubuf[:], ubuf[:], dmaskS, ALU.mult, ["ubuf", "cbf"], ["ubuf"])
                        for jj in range(4):
                            pe(ps6[:, jj, :], triS, ubuf[:, jj, :], True, jj == 3, ["cbf", "ubuf"], [PK[6]])
                            for j2 in range(jj + 1, 4):
                                pe(ps6[:, jj, :], ones_b, ubuf[:, j2, :], False, j2 == 3, ["cbf", "ubuf"], [PK[6]])
                        if gg > 0:
                            for jj in range(4):
                                pe(PS[7][:, :128], ones_b, ubuf[:, jj, :], jj == 0, jj == 3, ["cbf", "ubuf"], [PK[7]])
                        tt("dve", t1[:].rearrange("p (j q) -> p j q", q=128), ps6, Rt[:].unsqueeze(1).to_broadcast([128, 4, 128]),
                           ALU.add, [PK[6], "Rt"], ["t1"])
                        stt("dve", ebuf[:], PS[ps_i][:], SCALE, t1[:], ALU.mult, ALU.subtract, [PK[ps_i], "t1"], ["ebuf"])
                        act(PT[p_i][:].rearrange("p j q -> p (j q)"), ebuf[:], AF.Exp, ["ebuf"], ["PT%d" % p_i])
                        if gg == i:
                            tt("dve", PT[p_i][:], PT[p_i][:], dmaskS, ALU.mult, ["PT%d" % p_i, "cbf"], ["PT%d" % p_i])
                        for jj in range(4):
                            pe(PS[po_i][:, h * 65:(h + 1) * 65], PT[p_i][:, jj, :], V1[:, 4 * gg + jj, h * 65:(h + 1) * 65],
                               gg == i and jj == 0, gg == 0 and jj == 3, ["PT%d" % p_i, "V1"], [PK[po_i]])
                        if gg > 0:
                            tt("dve", Rt[:], Rt[:], PS[7][:, :128], ALU.add, ["Rt", PK[7]], ["Rt"])
                yb = nxt("y", 2)
                po3 = PS[po_i][:, :260].rearrange("p (h d) -> p h d", d=65)
                cp("act", ybuf[yb][:], po3[:, :, 0:64], [PK[po_i]], ["ybuf%d" % yb])
                P.dma("sp", ydram[i, :, 2 * 256:3 * 256], ybuf[yb][:].rearrange("p h d -> p (h d)"), reads=["ybuf%d" % yb], writes=["ydram"])

            def masked_attn(i, qi, po_i, kfn, vfn, kkeys, vkeys):
                for h in range(4):
                    for gg in range(i + 1):
                        ps_i = nxt("ps", 2)
                        p_i = nxt("pt", 2)
                        psv = PS[ps_i][:].rearrange("p (j q) -> p j q", q=128)
                        for jj in range(4):
                            pe(psv[:, jj, :], kfn(h, 4 * gg + jj), qslice(h, qi), True, True, kkeys + ["QT"], [PK[ps_i]])
                        act(PT[p_i][:].rearrange("p j q -> p (j q)"), PS[ps_i][:], AF.Exp, [PK[ps_i]], ["PT%d" % p_i], scale=SCALE)
                        tt("dve", PT[p_i][:], PT[p_i][:], maskT[:, 4 * gg:4 * gg + 4, :], ALU.mult, ["PT%d" % p_i, "maskT"], ["PT%d" % p_i])
                        for jj in range(4):
                            pe(PS[po_i][:, h * 65:(h + 1) * 65], PT[p_i][:, jj, :], vfn(h, 4 * gg + jj),
                               gg == 0 and jj == 0, gg == i and jj == 3, ["PT%d" % p_i] + vkeys, [PK[po_i]])

            def build_maskT(i):
                nblk = 4 * (i + 1)
                for j0 in range(0, nblk, 4):
                    ps_i = 6 + nxt("r", 2)
                    psv = PS[ps_i][:].rearrange("p (j q) -> p j q", q=128)
                    for jj in range(4):
                        tr(psv[:, jj, :], sc[:, (j0 + jj) * 128:(j0 + jj + 1) * 128], ["sc"], [PK[ps_i]], f32=True)
                    cp("act", maskT[:, j0:j0 + 4, :], psv, [PK[ps_i]], ["maskT"])

            def small_proj(qi):
                for c in range(8):
                    pe(PS[5][:, :16], hq[:, c, qi * 128:(qi + 1) * 128], wsm_sb[:, c, :], c == 0, c == 7, ["hq", "wsm"], [PK[5]])
                cp("act", wsmall[:], PS[5][:, :16], [PK[5]], ["wsmall"])

            def dsa_tile(g, qi):
                i = 4 * g + qi
                L = 512 * (i + 1)
                small_proj(qi)
                for gg in range(i + 1):
                    for h in range(4):
                        ps_i = 6 + nxt("r", 2)
                        rb = nxt("b", 2)
                        pe(PS[ps_i][:], qm[:, h, qi * 128:(qi + 1) * 128], kiT4[:, gg * 512:(gg + 1) * 512], True, True, ["qm", "kiT4"], [PK[ps_i]])
                        act(rbuf[rb][:], PS[ps_i][:], AF.Relu, [PK[ps_i]], ["rbuf%d" % rb])
                        if h == 0:
                            ts("dve", sc[:, gg * 512:(gg + 1) * 512], rbuf[rb][:], wsmall[:, 0:1], None, ALU.mult, None,
                               ["rbuf%d" % rb, "wsmall"], ["sc"])
                        else:
                            stt("dve", sc[:, gg * 512:(gg + 1) * 512], rbuf[rb][:], wsmall[:, h:h + 1], sc[:, gg * 512:(gg + 1) * 512],
                                ALU.mult, ALU.add, ["rbuf%d" % rb, "wsmall", "sc"], ["sc"])
                tt("dve", sc[:, i * 512:(i + 1) * 512], sc[:, i * 512:(i + 1) * 512], dmaskN, ALU.add, ["sc", "cf"], ["sc"])
                mset(mid[:], 0.0, ["mid"], eng="dve")
                w = BIS_R0
                for _ in range(BIS_K):
                    ts("dve", junk[:, :L], sc[:, :L], mid[:, 0:1], None, ALU.is_ge, ALU.add, ["sc", "mid"], ["maskT", "cnt"], accum=cnt[:, 0:1])
                    ts("dve", tmpb[:], cnt[:], KSEL - 0.5, w, ALU.is_ge, ALU.mult, ["cnt"], ["tmpb"])
                    stt("dve", mid[:], tmpb[:], -0.5 * w, mid[:], ALU.add, ALU.add, ["tmpb", "mid"], ["mid"])
                    w *= 0.5
                ts("dve", mid[:], mid[:], -w, None, ALU.add, None, ["mid"], ["mid"])
                ts("dve", sc[:, :L], sc[:, :L], mid[:, 0:1], None, ALU.is_ge, None, ["sc", "mid"], ["sc"])
                build_maskT(i)
                po_i = 2 + nxt("po", 2)
                masked_attn(i, qi, po_i, kslice, lambda h, j: V1[:, j, h * 65:(h + 1) * 65], ["KT"], ["V1"])
                finalize(po_i, i, 0)

            def mem_tile(g, qi):
                i = 4 * g + qi
                po_i = 2 + nxt("po", 2)
                for h in range(4):
                    ps_i = nxt("ps", 2)
                    p_i = nxt("pt", 2)
                    psv = PS[ps_i][:].rearrange("p (j q) -> p j q", q=128)
                    lo = (h % 2) * 64
                    for c in range(2):
                        pe(psv[:, c, :], mkT[lo:lo + 64, h // 2, c * 128:(c + 1) * 128], qslice(h, qi), True, True, ["mkT", "QT"], [PK[ps_i]])
                    act(PT[p_i][:, 0:2, :], psv[:, 0:2, :], AF.Exp, [PK[ps_i]], ["PT%d" % p_i], scale=SCALE)
                    for c in range(2):
                        pe(PS[po_i][:, h * 65:(h + 1) * 65], PT[p_i][:, c, :], mv1[:, c, h, :], c == 0, c == 1, ["PT%d" % p_i, "mv1"], [PK[po_i]])
                finalize(po_i, i, 4)

            def nsa_tile(g, qi):
                i = 4 * g + qi
                L = 512 * (i + 1)
                po_c, po_s, po_w = 2, 3, 5
                small_proj(qi)
                act(g12[:], wsmall[:, 4:16], AF.Sigmoid, ["wsmall"], ["g12"])
                P.dma("sp", vis_t[:], visd[:, i * NBS:(i + 1) * NBS], writes=["vis_t"])
                P.dma("sp", add_t[:], addd[:, i * NBS:(i + 1) * NBS], writes=["add_t"])
                P.dma("sp", trow_t[:], trow[:, i * 128:(i + 1) * 128].partition_broadcast(128), writes=["Rt"])
                ts("dve", cmaskN, nrow, tq[:, i:i + 1], None, ALU.is_le, None, ["cf"], ["ebuf"])
                for c in range(NCC):
                    ts("dve", cmaskT[:, c, :], trow_t[:], ncol[:, c:c + 1], None, ALU.is_ge, None, ["Rt", "cf"], ["cmaskT"])
                for h in range(4):
                    lo = (h % 2) * 64
                    pe(PS[4][:, :NCP], qslice(h, qi), kc2T[lo:lo + 64, :], True, True, ["QT", "kc2T"], [PK[4]])
                    act(ec, PS[4][:, :NCP], AF.Exp, [PK[4]], ["t1"], scale=SCALE)
                    stt("dve", ec, ec, 1.0, cmaskN, ALU.mult, ALU.mult, ["t1", "ebuf"], ["t1", "rs4"], accum=rs4[:, h:h + 1])
                    ts("dve", rs4[:, h:h + 1], rs4[:, h:h + 1], 1e-30, None, ALU.max, None, ["rs4"], ["rs4"])
                    P.op("dve", lambda E, h=h: E.reciprocal(rs4[:, h:h + 1], rs4[:, h:h + 1]), reads=["rs4"], writes=["rs4"])
                    if h == 0:
                        ts("dve", impacc, ec, rs4[:, 0:1], None, ALU.mult, None, ["t1", "rs4"], ["t2"])
                    else:
                        stt("dve", impacc, ec, rs4[:, h:h + 1], impacc, ALU.mult, ALU.add, ["t1", "rs4", "t2"], ["t2"])
                P.op("dve", lambda E: E.tensor_reduce(imp4[:], impacc.rearrange("p (b u) -> p b u", u=4), AX.X, ALU.add),
                     reads=["t2"], writes=["imp4"])
                tt("dve", imp4[:], imp4[:], vis_t[:], ALU.mult, ["imp4", "vis_t"], ["imp4"])
                tt("dve", imp4[:], imp4[:], add_t[:], ALU.add, ["imp4", "add_t"], ["imp4"])
                P.op("dve", lambda E: E.max(out=m8a[:], in_=imp4[:]), reads=["imp4"], writes=["m8a"])
                P.op("dve", lambda E: E.match_replace(out=imp2[:], in_to_replace=m8a[:], in_values=imp4[:], imm_value=-1.0e30),
                     reads=["imp4", "m8a"], writes=["imp2"])
                P.op("dve", lambda E: E.max(out=m8b[:], in_=imp2[:]), reads=["imp2"], writes=["m8b"])
                ts("dve", bm[:], imp4[:], m8b[:, 7:8], None, ALU.is_ge, None, ["imp4", "m8b"], ["bm"])
                nsb = 8 * (i + 1)
                cp("dve", sc[:, :L].rearrange("p (b u) -> p b u", u=64), bm[:, :nsb].unsqueeze(2).to_broadcast([128, nsb, 64]), ["bm"], ["sc"])
                tt("dve", sc[:, i * 512:(i + 1) * 512], sc[:, i * 512:(i + 1) * 512], dmaskN01, ALU.mult, ["sc", "cbf"], ["sc"])
                build_maskT(i)
                masked_attn(i, qi, po_s, lambda h, j: KT[(h % 2) * 64:(h % 2) * 64 + 64, 0, j * 128:(j + 1) * 128],
                            lambda h, j: V1[:, j, 0:65], ["KT"], ["V1"])
                for h in range(4):
                    lo = (h % 2) * 64
                    ps_i = nxt("ps", 2)
                    psv = PS[ps_i][:].rearrange("p (j q) -> p j q", q=128)
                    for c in range(NCC):
                        pe(psv[:, c, :], kc2T[lo:lo + 64, c * 128:(c + 1) * 128], qslice(h, qi), True, True, ["kc2T", "QT"], [PK[ps_i]])
                    act(eT[:], psv[:, 0:NCC, :], AF.Exp, [PK[ps_i]], ["eT"], scale=SCALE)
                    tt("dve", eT[:], eT[:], cmaskT[:], ALU.mult, ["eT", "cmaskT"], ["eT"])
                    for c in range(NCC):
                        pe(PS[po_c][:, h * 65:(h + 1) * 65], eT[:, c, :], vc1[:, c, :], c == 0, c == NCC - 1, ["eT", "vc1"], [PK[po_c]])
                for h in range(4):
                    lo = (h % 2) * 64
                    grps = [gr for gr in range(2) if 4 * i - 4 + 4 * gr >= 0]
                    for gr in grps:
                        kb0 = 4 * i - 4 + 4 * gr
                        ps_i = nxt("ps", 2)
                        p_i = nxt("pt", 2)
                        psv = PS[ps_i][:].rearrange("p (j q) -> p j q", q=128)
                        for jj in range(4):
                            pe(psv[:, jj, :], KT[lo:lo + 64, 1, (kb0 + jj) * 128:(kb0 + jj + 1) * 128], qslice(h, qi), True, True, ["KT", "QT"], [PK[ps_i]])
                        act(PT[p_i][:].rearrange("p j q -> p (j q)"), PS[ps_i][:], AF.Exp, [PK[ps_i]], ["PT%d" % p_i], scale=SCALE)
                        tt("dve", PT[p_i][:], PT[p_i][:], wmask[:, 4 * gr:4 * gr + 4, :], ALU.mult, ["PT%d" % p_i, "cbf"], ["PT%d" % p_i])
                        for jj in range(4):
                            pe(PS[po_w][:, h * 65:(h + 1) * 65], PT[p_i][:, jj, :], V1[:, kb0 + jj, 65:130],
                               gr == grps[0] and jj == 0, gr == grps[-1] and jj == 3, ["PT%d" % p_i, "V1"], [PK[po_w]])
                for b, po_i in enumerate((po_c, po_s, po_w)):
                    po3 = PS[po_i][:, :260].rearrange("p (h d) -> p h d", d=65)
                    ts("dve", rd[:], po3[:, :, 64], 1e-30, None, ALU.max, None, [PK[po_i]], ["rd"])
                    P.op("dve", lambda E: E.reciprocal(rd[:], rd[:]), reads=["rd"], writes=["rd"])
                    tt("dve", coef[:], rd[:], g12[:, b * 4:(b + 1) * 4], ALU.mult, ["rd", "g12"], ["coef"])
                    if b == 0:
                        tt("dve", yacc[:], po3[:, :, 0:64], coef[:].unsqueeze(2).to_broadcast([128, 4, 64]), ALU.mult, [PK[po_i], "coef"], ["yacc"])
                    else:
                        tt("dve", ytmp[:], po3[:, :, 0:64], coef[:].unsqueeze(2).to_broadcast([128, 4, 64]), ALU.mult, [PK[po_i], "coef"], ["ytmp"])
                        tt("dve", yacc[:], yacc[:], ytmp[:], ALU.add, ["yacc", "ytmp"], ["yacc"])
                yb = nxt("y", 2)
                cp("act", ybuf[yb][:], yacc[:], ["yacc"], ["ybuf%d" % yb])
                P.dma("sp", ydram[i, :, 3 * 256:4 * 256], ybuf[yb][:].rearrange("p h d -> p (h d)"), reads=["ybuf%d" % yb], writes=["ydram"])

            def load_wq(name, wt=None, wkey="wqb"):
                off, n = B_WQ[name]
                load_w(wqb if wt is None else wt, wkey, lambda c0, nn: wq[:, off + c0:off + c0 + nn], n)

            def run_branch(qgroup_fn, tile_fn):
                for g in range(NG):
                    load_hq(g)
                    qgroup_fn(g)
                    for qi in range(4):
                        tile_fn(g, qi)

            load_w(wsm_sb, "wsm", lambda c0, nn: wsm[:, c0:c0 + nn], 16)
            load_branch_kv(KFM_ROWS["foxk"], 1)
            load_wq("fox")
            run_branch(lambda g: make_QT(g, False), fox_tile)
            load_branch_kv(KFM_ROWS["sbk"], 2)
            load_wq("sb")
            run_branch(lambda g: make_QT(g, False), sb_tile)
            load_wq("mem")
            run_branch(lambda g: make_QT(g, False), mem_tile)
            load_branch_kv(KFM_ROWS["dsak"], 0)
            for h in range(4):
                ld_kfm(kiT4[32 * h:32 * h + 32, :], KFM_ROWS["idxk"], 32, "kiT4")
            load_wq("dsa")
            load_wq("idx", wqb2, "wqb2")

            def dsa_group(g):
                make_QT(g, True)
                load_tabs(g, 2)
                fm_q(4, 0, wqb2, "wqb2")
                fm_q(5, 128, wqb2, "wqb2")
                tt("dve", t1[:], PS[4][:], tabg[:, 0, :], ALU.mult, [PK[4], "tabg"], ["t1"])
                tt("dve", t2[:], PS[5][:], tabg[:, 1, :], ALU.mult, [PK[5], "tabg"], ["t2"])
                tt("dve", qiT[:], t1[:], t2[:], ALU.add, ["t1", "t2"], ["qiT"])
                for h in range(4):
                    ts("dve", qm[:, h, :], qiT[:], sel4[:, h:h + 1], None, ALU.mult, None, ["qiT", "cf"], ["qm"])

            run_branch(dsa_group, dsa_tile)
            ks0 = KFM_ROWS["kcks"] + 64
            for half in range(2):
                ld_kfm(KT[64 * half:64 * half + 64, 0, :], ks0, 64, "KT")
                ld_kfm(KT[64 * half:64 * half + 64, 1, :], KFM_ROWS["kw"], 64, "KT")
            ld_v(V1[:, :, 0:65], v2, 0, 2, "V1")
            ld_v(V1[:, :, 65:130], v2, 1, 2, "V1")
            load_wq("nsa")
            run_branch(lambda g: make_QT(g, True), nsa_tile)
            P.barrier()
            P.emit()

        with ExitStack() as st2:
            M2 = Pool2(nc, st2)
            NT3 = T // 256
            wz_sb = M2.sb("wz", [128, 8, 1280], BF16)
            wmg_sb = M2.sb("wmg", [128, 8, 5120], BF16)
            wbr_sb = M2.sb("wbr", [128, 5, 2, 1024], BF16)
            wout_sb = M2.sb("wout", [128, 8, 1024], BF16)
            fg_sb = M2.sb("fg", [128, 8], F32)
            h3 = M2.sb("h3", [128, 8, 256], BF16)
            x3 = M2.sb("x3", [128, 8, 256], F32)
            yl = M2.sb("yl", [128, 1280], BF16)
            zs = M2.sb("zs", [128, 1280], BF16)
            ysT = M2.sb("ysT", [128, 10, 256], BF16)
            sg = [M2.sb("sg", [128, 256], F32) for _ in range(2)]
            tmpm = M2.sb("tmpm", [128, 256], F32)
            macc = M2.sb("macc", [128, 256], F32)
            mergedT = M2.sb("mergedT", [128, 8, 256], BF16)
            xo = M2.sb("xo", [128, 8, 256], F32)
            rstd3 = M2.sb("rstd3", [128, 256], F32)
            c3 = {"ps": 0, "r": 0, "sg": 0}

            def nx3(name, n):
                v = c3[name] % n
                c3[name] += 1
                return v

            P.dma("sp", fg_sb[:], fgcol, writes=["fg"])
            load_w(wz_sb, "wz", lambda c0, nn: wz[:, c0:c0 + nn], 1280)
            load_w(wmg_sb, "wmg", lambda c0, nn: wmg[:, c0:c0 + nn], 5120)
            load_w(wout_sb, "wout", lambda c0, nn: wout[:, c0:c0 + nn], 1024)
            stgf = stg[:].rearrange("p c n -> p (c n)")
            for n in range(5):
                for kk in range(2):
                    P.dma("sp", stgf[:, :1024], wbr[n, kk * 128:(kk + 1) * 128, :], writes=["stg"])
                    P.op("dve", lambda E, n=n, kk=kk: E.tensor_copy(wbr_sb[:, n, kk, :], stgf[:, :1024]), reads=["stg"], writes=["wbr"])

            def p3_group(tg):
                tsl = slice(tg * 256, (tg + 1) * 256)
                P.dma("sp", h3[:], hT[:, tsl].rearrange("(c p) t -> p c t", p=128), writes=["h3"])
                P.dma("sp", x3[:], xT[:, tsl].rearrange("(c p) t -> p c t", p=128), writes=["x3"])
                for sub in range(2):
                    i = 2 * tg + sub
                    qs = slice(sub * 128, (sub + 1) * 128)
                    P.dma("sp", yl[:], ydram[i], reads=["ydram"], writes=["yl"])
                    for (pi, c0, n) in ((4, 0, 512), (5, 512, 512), (6, 1024, 256)):
                        for c in range(8):
                            pe(PS[pi][:, :n], h3[:, c, qs], wz_sb[:, c, c0:c0 + n], c == 0, c == 7, ["h3", "wz"], [PK[pi]])
                        act(zs[:, c0:c0 + n], PS[pi][:, :n], AF.Silu, [PK[pi]], ["zs"])
                    tt("dve", zs[:], zs[:], yl[:], ALU.mult, ["zs", "yl"], ["zs"])
                    for k0 in (0, 4, 8):
                        nb = min(4, 10 - k0)
                        ps_i = nx3("ps", 2)
                        psv = PS[ps_i][:].rearrange("p (j q) -> p j q", q=128)
                        for kk in range(nb):
                            tr(psv[:, kk, :], zs[:, (k0 + kk) * 128:(k0 + kk + 1) * 128], ["zs"], [PK[ps_i]])
                        cp("act", ysT[:, k0:k0 + nb, qs], psv[:, 0:nb, :], [PK[ps_i]], ["ysT"])
                for dch in range(8):
                    ds_ = slice(dch * 128, (dch + 1) * 128)
                    for n in range(5):
                        pb = nx3("ps", 2)
                        pg = 6 + nx3("r", 2)
                        si = nx3("sg", 2)
                        for kk in range(2):
                            pe(PS[pb][:, :256], wbr_sb[:, n, kk, ds_], ysT[:, 2 * n + kk, :], kk == 0, kk == 1, ["wbr", "ysT"], [PK[pb]])
                        for c in range(8):
                            pe(PS[pg][:, :256], wmg_sb[:, c, n * 1024 + dch * 128:n * 1024 + (dch + 1) * 128], h3[:, c, :], c == 0, c == 7,
                               ["wmg", "h3"], [PK[pg]])
                        act(sg[si][:], PS[pg][:, :256], AF.Sigmoid, [PK[pg]], ["sg%d" % si])
                        if n == 0:
                            tt("dve", macc[:], PS[pb][:, :256], sg[si][:], ALU.mult, [PK[pb], "sg%d" % si], ["macc"])
                        else:
                            tt("dve", tmpm[:], PS[pb][:, :256], sg[si][:], ALU.mult, [PK[pb], "sg%d" % si], ["tmpm"])
                            tt("pool", macc[:], macc[:], tmpm[:], ALU.add, ["macc", "tmpm"], ["macc"])
                    cp("act", mergedT[:, dch, :], macc[:], ["macc"], ["mergedT"])
                for dch in range(8):
                    ds_ = slice(dch * 128, (dch + 1) * 128)
                    for c in range(8):
                        pe(PS[4][:, :256], wout_sb[:, c, ds_], mergedT[:, c, :], c == 0, c == 7, ["wout", "mergedT"], [PK[4]])
                    tt("dve", xo[:, dch, :], PS[4][:, :256], x3[:, dch, :], ALU.add, [PK[4], "x3"], ["xo"])
                P.dma("sp", xo_o[:, tsl].rearrange("(c p) t -> p c t", p=128), xo[:], reads=["xo"], writes=["xo_o"])
                if not FINAL:
                    return
                act(h3[:], xo[:], AF.Square, ["xo"], ["h3"])
                for c in range(8):
                    pe(PS[5][:, :256], ones_b, h3[:, c, :], c == 0, c == 7, ["cbf", "h3"], [PK[5]])
                act(rstd3[:], PS[5][:, :256], AF.Ln, [PK[5]], ["rstd3"], bias=EPS, scale=1.0 / 1024)
                act(rstd3[:], rstd3[:], AF.Exp, ["rstd3"], ["rstd3"], scale=-0.5)
                for c in range(8):
                    stt("dve", x3[:, c, :], xo[:, c, :], fg_sb[:, c:c + 1], rstd3[:], ALU.mult, ALU.mult, ["xo", "fg", "rstd3"], ["x3"])
                P.dma("sp", xn_o[:, tsl].rearrange("(c p) t -> p c t", p=128), x3[:], reads=["x3"], writes=["xn_o"])

            for tg in range(NT3):
                p3_group(tg)
            P.barrier()
            P.emit()
    return nc


def host_prep_B(w_in, pe_k, pe_v, wc1_k, wc2_k, wc1_v, wc2_v, mem_norm, w_mem_kv, w_branch, w_out, final_norm):
    c = lambda n: _cols(w_in, n)
    p64 = _perm_idx(256, 64)
    wq = np.concatenate([
        c('dsa_q'), c('dsa_q')[:, p64],
        c('idx_q'), c('idx_q')[:, _perm_idx(128, 32)],
        c('fox_q'), c('sb_q'),
        c('nsa_q'), c('nsa_q')[:, p64],
        c('mem_q')], 1)
    assert wq.shape[1] == B_NWQ
    w1 = np.concatenate([wc1_k.reshape(32, 64, 128).transpose(1, 0, 2), wc1_v.reshape(32, 64, 128).transpose(1, 0, 2)], 0)
    return {
        "wq": np.ascontiguousarray(wq),
        "wsm": np.ascontiguousarray(np.concatenate([c('idx_w'), c('nsa_g')], 1)),
        "wz": np.ascontiguousarray(np.concatenate([c('dsa_z'), c('fox_z'), c('sb_z'), c('nsa_z'), c('mem_z')], 1)),
        "wmg": np.ascontiguousarray(c('merge')),
        "wbr": np.ascontiguousarray(w_branch),
        "wout": np.ascontiguousarray(w_out),
        "w1": np.ascontiguousarray(w1.reshape(128, 32 * 128)),
        "w2k2": np.ascontiguousarray(np.concatenate([wc2_k, wc2_k], 1)),
        "w2v": np.ascontiguousarray(wc2_v),
        "peT": np.ascontiguousarray(np.concatenate([pe_k.T, pe_v.T], 0)),
        "mgcol": np.ascontiguousarray(mem_norm.reshape(8, 128).T),
        "wmem": np.ascontiguousarray(w_mem_kv),
        "fgcol": np.ascontiguousarray(final_norm.reshape(8, 128).T),
    }


def host_consts_B(T, r):
    S = 4 * T
    NQ = T // 128
    NBS = S // 64
    NCMP = S // 16 - 1
    NCC = (NCMP + 127) // 128
    NCP = NCC * 128
    s = np.arange(128)[:, None]
    q = np.arange(128)[None, :]
    ident = (s == q).astype(np.float32)
    triS = (s >= q).astype(np.float32)
    ones = np.ones((128, 128), np.float32)
    dmask = np.zeros((128, 4, 128), np.float32)
    dmaskS = np.zeros((128, 4, 128), np.float32)
    for jj in range(4):
        if jj < r:
            dmask[:, jj, :] = 1.0
            dmaskS[:, jj, :] = 1.0
        elif jj == r:
            dmask[:, jj, :] = (s <= q)
            dmaskS[:, jj, :] = (s < q)
    wmask = np.zeros((128, 8, 128), np.float32)
    for jj in range(8):
        diff = (r + 4 - jj) * 128 + q - s
        wmask[:, jj, :] = ((diff >= 0) & (diff < 512))
    dmaskN01 = dmask.transpose(2, 1, 0).reshape(128, 512)
    c_bf = np.concatenate([ident, triS, ones, dmask.reshape(128, 512), dmaskS.reshape(128, 512),
                           wmask.reshape(128, 1024), dmaskN01], 1).astype(NPBF)
    triInc = (s <= q).astype(np.float32)
    dmaskN = np.where(dmaskN01 > 0, 0.0, NEG_BIG).astype(np.float32)
    oh = np.zeros((128, 4), np.float32)
    oh[:, r] = 1.0
    sel4 = (np.arange(128)[:, None] // 32 == np.arange(4)[None, :]).astype(np.float32)
    tq = ((4 * np.arange(NQ)[None, :] + r) * 128 + np.arange(128)[:, None]).astype(np.float32)
    ncol = (16.0 * (128 * np.arange(NCC)[None, :] + np.arange(128)[:, None]) + 31.0).astype(np.float32)
    nrow = np.broadcast_to((16.0 * np.arange(NCP) + 31.0)[None, :], (128, NCP)).astype(np.float32)
    c_f = np.concatenate([triInc, ones, ident, dmaskN, oh, sel4, tq, ncol, nrow], 1).astype(np.float32)
    trow = np.ascontiguousarray(tq.T.reshape(1, T))
    cur = (tq // 64)[:, :, None]
    blk = np.arange(NBS)[None, None, :]
    forced = (blk == 0) | (blk == cur) | (blk == cur - 1)
    visible = blk <= cur
    vis = (visible & ~forced).astype(np.float32)
    addc = np.where(forced, 1.0e4 + blk, np.where(visible, 0.0, -1.0)).astype(np.float32)
    return {"c_bf": np.ascontiguousarray(c_bf), "c_f": np.ascontiguousarray(c_f), "trow": trow.astype(np.float32),
            "vis": np.ascontiguousarray(vis.reshape(128, NQ * NBS)), "addc": np.ascontiguousarray(addc.reshape(128, NQ * NBS))}


def shard_tokens(a, T):
    NQ = T // 128
    v = a.reshape((NQ, 4, 128) + a.shape[1:])
    return [np.ascontiguousarray(v[:, r].reshape((T,) + a.shape[1:])) for r in range(4)]


def unshard_tokens(parts, T):
    NQ = T // 128
    tail = parts[0].shape[1:]
    v = np.stack([p.reshape((NQ, 128) + tail) for p in parts], 1)
    return v.reshape((4 * T,) + tail)


A_W = {"gcol": [128, 8], "wfm": [1024, A_NFM], "wtm": [1024, A_NTM], "kvg": [128, 1], "wuk2": [128, 512],
       "wuv": [128, 256], "foxb": [128, 4]}
B_W = {"wq": [1024, B_NWQ], "wsm": [1024, 16], "wz": [1024, 1280], "wmg": [1024, 5120], "wbr": [5, 256, 1024],
       "wout": [1024, 1024], "w1": [128, 32 * 128], "w2k2": [128, 128], "w2v": [128, 64], "peT": [128, 32],
       "mgcol": [128, 8], "wmem": [1024, 512]}
GROUPS = [[0, 1, 2, 3], [4, 5, 6, 7]]


def build_F(T, depth):
    nc = bass.Bass("TRN2", target_bir_lowering=False)
    S = 4 * T
    NB = S // 128
    NQ = T // 128
    NBS = S // 64
    NCMP = S // 16 - 1
    NCC = (NCMP + 127) // 128
    NCP = NCC * 128
    EI = lambda name, shape, dt=F32: nc.dram_tensor(name, list(shape), dt, kind="ExternalInput").ap()
    IN = lambda name, shape, dt: nc.dram_tensor(name, list(shape), dt).ap()
    xT = EI("xT", [1024, T])
    pos = EI("pos", [1, T], I32)
    memT = EI("memT", [1024, 256])
    rc = EI("rc", [128, 8])
    fgcol = EI("fgcol", [128, 8])
    cst = {"c_bf": EI("c_bf", [128, 128 * 3 + 512 * 2 + 1024 + 512], BF16),
           "c_f": EI("c_f", [128, 128 * 3 + 512 + 4 + 4 + NQ + NCC + NCP]),
           "trow": EI("trow", [1, T]), "vis": EI("vis", [128, NQ * NBS]), "addc": EI("addc", [128, NQ * NBS])}
    WA = {k: EI(k, [depth] + v) for k, v in A_W.items()}
    WB = {k: EI(k, [depth] + v) for k, v in B_W.items()}
    xn_o = nc.dram_tensor("xn", [1024, T], F32, kind="ExternalOutput").ap()
    ydram = IN("ydram", [NQ, 128, 1280], BF16)
    with ExitStack() as stack:
        P = Prog(nc, stack)
        M = Pool2(nc, stack)
        PS = [M.ps("ps%d" % i, [128, 512]) for i in range(8)]
        x_cur = xT
        for l in range(depth):
            hT_l = IN("hT%d" % l, [1024, T], BF16)
            tab_l = IN("tab%d" % l, [4, 128, T], F32)
            kfmL = IN("kfmL%d" % l, [KFM_N, T], BF16)
            v3L = IN("v3L%d" % l, [3, T, 260], BF16)
            v2L = IN("v2L%d" % l, [2, T, 65], BF16)
            lfL = IN("lfL%d" % l, [T, 4], F32)
            kfmG = [IN("kfmG%d_%d" % (l, c), [512, T], BF16) for c in range(KFM_N // 128)]
            v3G = [[IN("v3G%d_%d_%d" % (l, k, hf), [4 * (T // 2), 260], BF16) for hf in range(2)] for k in range(3)]
            v2G = [[IN("v2G%d_%d_%d" % (l, k, hf), [4 * (T // 2), 65], BF16) for hf in range(2)] for k in range(2)]
            lfG = IN("lfG%d" % l, [4 * T, 4], F32)
            x_nxt = IN("x%d" % (l + 1), [1024, T], F32)
            ovA = {k: v[l] for k, v in WA.items()}
            ovA.update({"xT": x_cur, "pos": pos, "rc": rc, "hT": hT_l, "kfm": kfmL, "v3": v3L, "v2": v2L, "lf": lfL, "tab": tab_l})
            build_A(T, {"nc": nc, "P": P, "PS": PS, "ov": ovA})
            for c in range(KFM_N // 128):
                P.coll([kfmL[c * 128:(c + 1) * 128, :]], [kfmG[c]], GROUPS)
            for k in range(3):
                for hf in range(2):
                    P.coll([v3L[k, hf * (T // 2):(hf + 1) * (T // 2), :]], [v3G[k][hf]], GROUPS)
            for k in range(2):
                for hf in range(2):
                    P.coll([v2L[k, hf * (T // 2):(hf + 1) * (T // 2), :]], [v2G[k][hf]], GROUPS)
            P.coll([lfL], [lfG], GROUPS)
            P.barrier()
            ovB = {k: v[l] for k, v in WB.items()}
            ovB.update(cst)
            ovB.update({"xT": x_cur, "hT": hT_l, "tab": tab_l, "kfm": kfmG, "v3": v3G, "v2": v2G, "lf": lfG, "memT": memT,
                        "fgcol": fgcol, "xo": x_nxt, "xn": xn_o, "ydram": ydram})
            build_B(T, {"nc": nc, "P": P, "PS": PS, "ov": ovB, "final": l == depth - 1})
            x_cur = x_nxt
        P.barrier()
        P.emit()
    return nc


_NC_CACHE = {}


def _get_nc(kind, T):
    key = (kind, T)
    if key not in _NC_CACHE:
        _NC_CACHE[key] = build_A(T) if kind == "A" else build_B(T)
    return _NC_CACHE[key]


FUSED = True


def kernel_fused(x, mem, positions, norm_g, w_in, kv_norm, w_uk, w_uv, fox_bias, nsa_pe_k, nsa_pe_v,
                 nsa_wc1_k, nsa_wc2_k, nsa_wc1_v, nsa_wc2_v, mem_norm, w_mem_kv, w_branch, w_out, final_norm):
    f = lambda a: np.asarray(a, dtype=np.float32)
    x = f(x)
    mem = f(mem)
    positions = np.asarray(positions).astype(np.int32)
    Bn, S, D = x.shape
    T = S // 4
    depth = np.asarray(norm_g).shape[0]
    key = ("F", T, depth)
    if key not in _NC_CACHE:
        _NC_CACHE[key] = build_F(T, depth)
    nc = _NC_CACHE[key]
    cores = list(range(8))
    wA = [host_prep_A(f(norm_g[l]), f(w_in[l]), f(kv_norm[l]), f(w_uk[l]), f(w_uv[l]), f(fox_bias[l])) for l in range(depth)]
    wB = [host_prep_B(f(w_in[l]), f(nsa_pe_k[l]), f(nsa_pe_v[l]), f(nsa_wc1_k[l]), f(nsa_wc2_k[l]), f(nsa_wc1_v[l]),
                      f(nsa_wc2_v[l]), f(mem_norm[l]), f(w_mem_kv[l]), f(w_branch[l]), f(w_out[l]), f(final_norm)) for l in range(depth)]
    shared = {k: np.ascontiguousarray(np.stack([wA[l][k] for l in range(depth)], 0)) for k in A_W}
    shared.update({k: np.ascontiguousarray(np.stack([wB[l][k] for l in range(depth)], 0)) for k in B_W})
    shared["rc"] = wA[0]["rc"]
    shared["fgcol"] = wB[0]["fgcol"]
    del wA, wB
    consts = [host_consts_B(T, r) for r in range(4)]
    xs = [shard_tokens(x[b], T) for b in range(Bn)]
    ps = [shard_tokens(positions[b], T) for b in range(Bn)]
    in_maps = []
    for c in cores:
        b, r = c // 4, c % 4
        m = dict(shared)
        m.update(consts[r])
        m["xT"] = np.ascontiguousarray(xs[b][r].T)
        m["pos"] = ps[b][r].reshape(1, T)
        m["memT"] = np.ascontiguousarray(mem[b].T)
        in_maps.append(m)
    R = run_bass_kernel_spmd(nc, in_maps, core_ids=cores).results
    out = np.stack([unshard_tokens([np.ascontiguousarray(np.asarray(R[4 * b + r]["xn"]).T) for r in range(4)], T) for b in range(Bn)], 0)
    return out.astype(np.float32)


def kernel(x, mem, positions, norm_g, w_in, kv_norm, w_uk, w_uv, fox_bias, nsa_pe_k, nsa_pe_v,
           nsa_wc1_k, nsa_wc2_k, nsa_wc1_v, nsa_wc2_v, mem_norm, w_mem_kv, w_branch, w_out, final_norm):
    if FUSED:
        return kernel_fused(x, mem, positions, norm_g, w_in, kv_norm, w_uk, w_uv, fox_bias, nsa_pe_k, nsa_pe_v,
                            nsa_wc1_k, nsa_wc2_k, nsa_wc1_v, nsa_wc2_v, mem_norm, w_mem_kv, w_branch, w_out, final_norm)
    f = lambda a: np.asarray(a, dtype=np.float32)
    x = f(x)
    mem = f(mem)
    positions = np.asarray(positions).astype(np.int32)
    Bn, S, D = x.shape
    T = S // 4
    depth = np.asarray(norm_g).shape[0]
    ncA = _get_nc("A", T)
    ncB = _get_nc("B", T)
    cores = list(range(8))
    consts = [host_consts_B(T, r) for r in range(4)]
    xs = [shard_tokens(x[b], T) for b in range(Bn)]
    ps = [shard_tokens(positions[b], T) for b in range(Bn)]
    xT = [np.ascontiguousarray(xs[c // 4][c % 4].T) for c in cores]
    posr = [ps[c // 4][c % 4].reshape(1, T) for c in cores]
    memT = [np.ascontiguousarray(mem[b].T) for b in range(Bn)]
    xn = None
    for l in range(depth):
        wA = host_prep_A(f(norm_g[l]), f(w_in[l]), f(kv_norm[l]), f(w_uk[l]), f(w_uv[l]), f(fox_bias[l]))
        wB = host_prep_B(f(w_in[l]), f(nsa_pe_k[l]), f(nsa_pe_v[l]), f(nsa_wc1_k[l]), f(nsa_wc2_k[l]), f(nsa_wc1_v[l]),
                         f(nsa_wc2_v[l]), f(mem_norm[l]), f(w_mem_kv[l]), f(w_branch[l]), f(w_out[l]), f(final_norm))
        in_maps = []
        for c in cores:
            m = dict(wA)
            m["xT"] = xT[c]
            m["pos"] = posr[c]
            in_maps.append(m)
        RA = run_bass_kernel_spmd(ncA, in_maps, core_ids=cores).results
        del in_maps
        full = []
        for b in range(Bn):
            kfm = unshard_tokens([np.ascontiguousarray(np.asarray(RA[4 * b + r]["kfm"]).T) for r in range(4)], T)
            v3 = np.stack([unshard_tokens([np.asarray(RA[4 * b + r]["v3"])[k] for r in range(4)], T) for k in range(3)], 0)
            v2 = np.stack([unshard_tokens([np.asarray(RA[4 * b + r]["v2"])[k] for r in range(4)], T) for k in range(2)], 0)
            lf = unshard_tokens([np.asarray(RA[4 * b + r]["lf"]) for r in range(4)], T)
            full.append({
                "kfm": np.ascontiguousarray(kfm.T), "v3": np.ascontiguousarray(v3), "v2": np.ascontiguousarray(v2),
                "lf": np.ascontiguousarray(lf.reshape(S // 128, 128, 4).transpose(1, 0, 2).reshape(128, -1)),
            })
        in_maps = []
        for c in cores:
            b, r = c // 4, c % 4
            m = dict(wB)
            m.update(consts[r])
            m.update(full[b])
            m["xT"] = xT[c]
            m["hT"] = np.asarray(RA[c]["hT"])
            m["tab"] = np.asarray(RA[c]["tab"])
            m["memT"] = memT[b]
            in_maps.append(m)
        del RA
        RB = run_bass_kernel_spmd(ncB, in_maps, core_ids=cores).results
        del in_maps, full
        xT = [np.ascontiguousarray(np.asarray(RB[c]["xo"])) for c in cores]
        if l == depth - 1:
            xn = [np.asarray(RB[c]["xn"]) for c in cores]
        del RB
    out = np.stack([unshard_tokens([np.ascontiguousarray(xn[4 * b + r].T) for r in range(4)], T) for b in range(Bn)], 0)
    return out.astype(np.float32)
```

```python
import math
from contextlib import ExitStack
import numpy as np
import ml_dtypes
import concourse.bass as bass
import concourse.mybir as mybir
from concourse.bass_utils import run_bass_kernel_spmd

F32 = mybir.dt.float32
BF16 = mybir.dt.bfloat16
I32 = mybir.dt.int32
U8 = mybir.dt.uint8
AF = mybir.ActivationFunctionType
ALU = mybir.AluOpType
AX = mybir.AxisListType
NPBF = ml_dtypes.bfloat16

D_MODEL = 1024
NCH = 8
EPS = 1e-6
PI = math.pi


class Prog:
    ENG = ("pe", "act", "dve", "pool", "sp")
    CH = 20000
    ND = 24

    def __init__(self, nc, stack):
        self.nc = nc
        self.stack = stack
        self.q = {e: [] for e in self.ENG}
        self.n = {e: 0 for e in self.ENG}
        self.esems = {e: [] for e in self.ENG}
        self.seen = {e: {} for e in self.ENG}
        self.lastw = {}
        self.readers = {}
        self.dsems = [stack.enter_context(nc.semaphore(f"dq{i}")) for i in range(self.ND)]
        self.dcount = [0] * self.ND
        self.dlast = [None] * self.ND
        self.dnext = 0
        self.latest = {}
        self.nsem = 0

    def _esem(self, eng, idx):
        c = (idx - 1) // self.CH
        while len(self.esems[eng]) <= c:
            self.esems[eng].append(self.stack.enter_context(self.nc.semaphore(f"s_{eng}{len(self.esems[eng])}")))
        return self.esems[eng][c], (idx - 1) % self.CH + 1

    def _deps(self, eng, reads, writes):
        toks = []
        for k in reads:
            t = self.lastw.get(k)
            if t is not None:
                toks.append(t)
        for k in writes:
            t = self.lastw.get(k)
            if t is not None:
                toks.append(t)
            for t in self.readers.get(k, {}).values():
                if t[2] != eng:
                    toks.append(t)
        return toks

    def _waits(self, eng, toks):
        need = {}
        for (sem, val, _e, sid) in toks:
            if self.seen[eng].get(sid, 0) < val:
                if sid not in need or need[sid][1] < val:
                    need[sid] = (sem, val)
        for sid, (sem, val) in need.items():
            self.seen[eng][sid] = val
        return list(need.values())

    def _commit(self, tok, reads, writes):
        for k in writes:
            self.lastw[k] = tok
            self.readers[k] = {}
        for k in reads:
            self.readers.setdefault(k, {})[tok[3]] = tok

    def op(self, eng, fn, reads=(), writes=()):
        waits = self._waits(eng, self._deps(eng, reads, writes))
        self.n[eng] += 1
        sem, val = self._esem(eng, self.n[eng])
        tok = (sem, val, eng, ("e", eng, (self.n[eng] - 1) // self.CH))
        self.latest[eng] = tok

        def emit(E, waits=waits, fn=fn, sem=sem):
            for (s, v) in waits:
                E.wait_ge(s, v)
            fn(E).then_inc(sem, 1)

        self.q[eng].append(emit)
        self._commit(tok, reads, writes)
        return tok

    def dma(self, eng, out, in_, reads=(), writes=(), **kw):
        d = self.dnext % self.ND
        self.dnext += 1
        toks = self._deps(None, reads, writes)
        if self.dlast[d] is not None:
            toks.append(self.dlast[d])
        waits = self._waits(eng, toks)
        self.dcount[d] += 16
        sem = self.dsems[d]
        tok = (sem, self.dcount[d], "dma", ("d", d))
        self.dlast[d] = tok

        def emit(E, waits=waits, sem=sem, out=out, in_=in_, kw=kw):
            for (s, v) in waits:
                E.wait_ge(s, v)
            E.dma_start(out=out, in_=in_, **kw).then_inc(sem, 16)

        self.q[eng].append(emit)
        self._commit(tok, reads, writes)
        return tok

    def coll(self, ins, outs, groups, reads=(), writes=()):
        if not hasattr(self, "ccsem"):
            self.ccsem = self.stack.enter_context(self.nc.semaphore("ccsem"))
            self.ccn = 0
            self.cclast = None
        toks = self._deps(None, reads, writes)
        if self.cclast is not None:
            toks.append(self.cclast)
        waits = self._waits("pool", toks)
        self.ccn += 1
        sem = self.ccsem
        tok = (sem, self.ccn, "cc", ("cc",))
        self.cclast = tok
        self.latest["cc"] = tok
        ins = [a.opt() for a in ins]
        outs = [a.opt() for a in outs]

        def emit(E, waits=waits, sem=sem):
            for (s_, v) in waits:
                E.wait_ge(s_, v)
            E.collective_compute("AllGather", ALU.bypass, replica_groups=groups, ins=ins, outs=outs).then_inc(sem)

        self.q["pool"].append(emit)
        self._commit(tok, reads, writes)
        return tok

    def barrier(self, engines=None):
        toks = list(self.latest.values()) + [t for t in self.dlast if t is not None]
        for eng in (engines or self.ENG):
            waits = self._waits(eng, toks)
            if waits:
                def emit(E, waits=waits):
                    for (s, v) in waits:
                        E.wait_ge(s, v)
                self.q[eng].append(emit)
        self.lastw = {}
        self.readers = {}

    def emit(self):
        nc = self.nc
        with nc.Block() as blk:
            for eng, reg in (("sp", blk.sync), ("pe", blk.tensor), ("act", blk.scalar),
                             ("dve", blk.vector), ("pool", blk.gpsimd)):
                fns = self.q[eng]
                if fns:
                    reg(lambda E, fns=fns: [f(E) for f in fns])
        self.q = {e: [] for e in self.ENG}


class Pool2:
    CNT = [0]

    def __init__(self, nc, stack):
        self.nc = nc
        self.stack = stack

    def sb(self, name, shape, dt):
        Pool2.CNT[0] += 1
        return self.stack.enter_context(self.nc.sbuf_tensor(f"{name}_{Pool2.CNT[0]}", list(shape), dt))

    def ps(self, name, shape, dt=F32):
        Pool2.CNT[0] += 1
        return self.stack.enter_context(self.nc.psum_tensor(f"{name}_{Pool2.CNT[0]}", list(shape), dt))


A_FM_GROUPS = [("ckv", 128), ("idxk", 32), ("idxk_p", 32), ("foxk0", 128), ("foxk1", 128),
               ("sbk0", 128), ("sbk1", 128), ("kcks", 128), ("kcks_p", 128), ("kw", 64), ("kw_p", 64), ("vc", 64)]
A_NFM = sum(n for _, n in A_FM_GROUPS)
A_NTM = 512 + 132
KFM_ROWS = {"dsak": 0, "foxk": 256, "sbk": 512, "kcks": 768, "kw": 896, "vc": 960, "idxk": 1024}
KFM_N = 1152


def build_A(T, ctx=None):
    nc = ctx["nc"] if ctx else bass.Bass("TRN2", target_bir_lowering=False)
    NT = T // 512
    ov = ctx["ov"] if ctx else {}
    dr = lambda name, shape, dt, kind: ov[name] if name in ov else nc.dram_tensor(name, list(shape), dt, kind=kind).ap()
    xT = dr("xT", [1024, T], F32, "ExternalInput")
    pos = dr("pos", [1, T], I32, "ExternalInput")
    gcol = dr("gcol", [128, 8], F32, "ExternalInput")
    wfm = dr("wfm", [1024, A_NFM], F32, "ExternalInput")
    wtm = dr("wtm", [1024, A_NTM], F32, "ExternalInput")
    kvg = dr("kvg", [128, 1], F32, "ExternalInput")
    wuk2 = dr("wuk2", [128, 512], F32, "ExternalInput")
    wuv = dr("wuv", [128, 256], F32, "ExternalInput")
    foxb = dr("foxb", [128, 4], F32, "ExternalInput")
    rc = dr("rc", [128, 8], F32, "ExternalInput")
    hT_o = dr("hT", [1024, T], BF16, "ExternalOutput")
    kfm_o = dr("kfm", [KFM_N, T], BF16, "ExternalOutput")
    v3_o = dr("v3", [3, T, 260], BF16, "ExternalOutput")
    v2_o = dr("v2", [2, T, 65], BF16, "ExternalOutput")
    lf_o = dr("lf", [T, 4], F32, "ExternalOutput")
    tab_o = dr("tab", [4, 128, T], F32, "ExternalOutput")

    with ExitStack() as stack:
        P = ctx["P"] if ctx else Prog(nc, stack)
        M = Pool2(nc, stack)
        ones = M.sb("ones", [128, 128], BF16)
        g_sb = M.sb("g", [128, 8], F32)
        kvg_sb = M.sb("kvg", [128, 1], F32)
        rc_sb = M.sb("rc", [128, 8], F32)
        foxb_sb = M.sb("foxb", [128, 4], F32)
        nfoxb = M.sb("nfoxb", [128, 4], F32)
        wfm_sb = M.sb("wfm", [128, 8, A_NFM], BF16)
        wtm_sb = M.sb("wtm", [128, 8, A_NTM], BF16)
        wuk_sb = M.sb("wuk", [128, 512], BF16)
        wuv_sb = M.sb("wuv", [128, 256], BF16)
        stg = M.sb("stg", [128, 8, 512], F32)
        P.op("pool", lambda E: E.memset(ones[:], 1.0), writes=["ones"])
        P.dma("sp", g_sb[:], gcol, writes=["g"])
        P.dma("sp", kvg_sb[:], kvg, writes=["kvg"])
        P.dma("sp", rc_sb[:], rc, writes=["rc"])
        P.dma("sp", foxb_sb[:], foxb, writes=["foxb"])
        P.op("dve", lambda E: E.tensor_scalar(nfoxb[:], foxb_sb[:], -1.0, None, ALU.mult), reads=["foxb"], writes=["nfoxb"])
        def load_w(dst, src, ncols, key):
            c0 = 0
            while c0 < ncols:
                n = min(512, ncols - c0)
                P.dma("sp", stg[:, :, :n], src[:, c0:c0 + n].rearrange("(c p) n -> p c n", p=128), writes=["stg"])
                P.op("dve", lambda E, c0=c0, n=n: E.tensor_copy(dst[:, :, c0:c0 + n], stg[:, :, :n]), reads=["stg"], writes=[key])
                c0 += n
        load_w(wfm_sb, wfm, A_NFM, "wfm")
        load_w(wtm_sb, wtm, A_NTM, "wtm")
        P.dma("sp", stg[:, 0, :], wuk2, writes=["stg"])
        P.op("dve", lambda E: E.tensor_copy(wuk_sb[:], stg[:, 0, :]), reads=["stg"], writes=["wuk"])
        P.dma("sp", stg[:, 0, :256], wuv, writes=["stg"])
        P.op("dve", lambda E: E.tensor_copy(wuv_sb[:], stg[:, 0, :256]), reads=["stg"], writes=["wuv"])

        x_sb = M.sb("x", [128, 8, 512], F32)
        sq = M.sb("sq", [128, 8, 512], BF16)
        rstd = M.sb("rstd", [128, 512], F32)
        h_sb = M.sb("h", [128, 8, 512], BF16)
        posi = M.sb("posi", [128, 512], I32)
        posf = M.sb("posf", [128, 512], F32)
        ang = M.sb("ang", [128, 512], F32)
        tabs = M.sb("tabs", [128, 4, 512], F32)
        ckv = M.sb("ckv", [128, 512], F32)
        ckvn = M.sb("ckvn", [128, 512], BF16)
        t1 = M.sb("t1", [128, 512], F32)
        t2 = M.sb("t2", [128, 512], F32)
        ofm = [M.sb("ofm", [128, 512], BF16) for _ in range(3)]
        v3t = [M.sb("v3t", [128, 3, 4, 65], BF16) for _ in range(2)]
        v2t = [M.sb("v2t", [128, 2, 65], BF16) for _ in range(2)]
        lft = [M.sb("lft", [128, 4], F32) for _ in range(2)]
        lfe = M.sb("lfe", [128, 4], F32)
        pa = ctx["PS"][:6] if ctx else [M.ps("pa", [128, 512]) for _ in range(6)]
        pk = ["PS%d" % i for i in range(6)]
        for i in range(2):
            P.op("pool", lambda E, i=i: E.memset(v3t[i][:], 1.0), writes=["v3t%d" % i])
            P.op("pool", lambda E, i=i: E.memset(v2t[i][:], 1.0), writes=["v2t%d" % i])

        fm_off = {}
        o = 0
        for nme, n in A_FM_GROUPS:
            fm_off[nme] = (o, n)
            o += n
        ofm_i = [0]
        pa_i = [0]

        def next_pa():
            i = pa_i[0] % 6
            pa_i[0] += 1
            return pa[i], pk[i]

        def next_ofm():
            i = ofm_i[0] % 3
            ofm_i[0] += 1
            return ofm[i], "ofm%d" % i

        def fm_proj(grp):
            off, n = fm_off[grp]
            ps, key = next_pa()
            for c in range(8):
                P.op("pe", lambda E, c=c: E.matmul(ps[:n, :], lhsT=wfm_sb[:, c, off:off + n], rhs=h_sb[:, c, :],
                                                   start=(c == 0), stop=(c == 7)),
                     reads=["wfm", "h"], writes=[key])
            return ps, key

        def rope_out(grp, grp_p, n, tc, ts, row0, tt):
            ps1, k1 = fm_proj(grp)
            ps2, k2 = fm_proj(grp_p)
            rope_combine(ps1, k1, ps2, k2, n, tc, ts, row0, tt)

        def rope_combine(ps1, k1, ps2, k2, n, tc, ts, row0, tt):
            ot, ok = next_ofm()
            P.op("dve", lambda E: E.tensor_tensor(t1[:n, :], ps1[:n, :], tabs[:n, tc, :], ALU.mult), reads=[k1, "tabs"], writes=["t1"])
            P.op("dve", lambda E: E.tensor_tensor(t2[:n, :], ps2[:n, :], tabs[:n, ts, :], ALU.mult), reads=[k2, "tabs"], writes=["t2"])
            P.op("dve", lambda E: E.tensor_tensor(ot[:n, :], t1[:n, :], t2[:n, :], ALU.add), reads=["t1", "t2"], writes=[ok])
            P.dma("sp", kfm_o[row0:row0 + n, tt * 512:(tt + 1) * 512], ot[:n, :], reads=[ok])

        def plain_out(grp, n, row0, tt, eng="act"):
            ps, k = fm_proj(grp)
            ot, ok = next_ofm()
            if eng == "act":
                P.op("act", lambda E: E.copy(ot[:n, :], ps[:n, :]), reads=[k], writes=[ok])
            else:
                P.op("dve", lambda E: E.tensor_copy(ot[:n, :], ps[:n, :]), reads=[k], writes=[ok])
            P.dma("sp", kfm_o[row0:row0 + n, tt * 512:(tt + 1) * 512], ot[:n, :], reads=[ok])

        def do_tile(tt):
            tsl = slice(tt * 512, (tt + 1) * 512)
            P.dma("sp", x_sb[:], xT[:, tsl].rearrange("(c p) t -> p c t", p=128), writes=["x"])
            P.op("act", lambda E: E.activation(sq[:], x_sb[:], AF.Square), reads=["x"], writes=["sq"])
            ps, key = next_pa()
            for c in range(8):
                P.op("pe", lambda E, c=c, ps=ps: E.matmul(ps[:], lhsT=ones[:], rhs=sq[:, c, :], start=(c == 0), stop=(c == 7)),
                     reads=["ones", "sq"], writes=[key])
            P.op("act", lambda E, ps=ps: E.activation(rstd[:], ps[:], AF.Ln, bias=EPS, scale=1.0 / 1024), reads=[key], writes=["rstd"])
            P.op("act", lambda E: E.activation(rstd[:], rstd[:], AF.Exp, scale=-0.5), reads=["rstd"], writes=["rstd"])
            for c in range(8):
                P.op("dve", lambda E, c=c: E.scalar_tensor_tensor(h_sb[:, c, :], x_sb[:, c, :], g_sb[:, c:c + 1], rstd[:],
                                                                  ALU.mult, ALU.mult), reads=["x", "g", "rstd"], writes=["h"])
            P.dma("sp", hT_o[:, tsl].rearrange("(c p) t -> p c t", p=128), h_sb[:], reads=["h"])
            P.dma("sp", posi[:], pos[:, tsl].partition_broadcast(128), writes=["posi"])
            P.op("dve", lambda E: E.tensor_copy(posf[:], posi[:]), reads=["posi"], writes=["posf"])
            MAGIC = 12582912.0
            for (ci, ti) in ((0, 0), (3, 2)):
                for (which, shift) in ((0, 0.5 * PI), (1, 0.0)):
                    P.op("dve", lambda E, ci=ci, shift=shift: E.tensor_scalar(ang[:], posf[:], rc_sb[:, ci:ci + 1], shift, ALU.mult, ALU.add),
                         reads=["posf", "rc"], writes=["ang"])
                    P.op("dve", lambda E: E.tensor_scalar(t1[:], ang[:], 1.0 / (2 * PI), MAGIC, ALU.mult, ALU.add), reads=["ang"], writes=["t1"])
                    P.op("dve", lambda E: E.tensor_scalar(t1[:], t1[:], MAGIC, -2 * PI, ALU.subtract, ALU.mult), reads=["t1"], writes=["t1"])
                    P.op("dve", lambda E: E.tensor_tensor(ang[:], ang[:], t1[:], ALU.add), reads=["ang", "t1"], writes=["ang"])
                    P.op("dve", lambda E: E.tensor_scalar(ang[:], ang[:], PI, -PI, ALU.min, ALU.max), reads=["ang"], writes=["ang"])
                    if which == 0:
                        P.op("act", lambda E, ti=ti: E.activation(tabs[:, ti, :], ang[:], AF.Sin), reads=["ang"], writes=["tabs"])
                    else:
                        P.op("act", lambda E, ci=ci, ti=ti: E.activation(tabs[:, ti + 1, :], ang[:], AF.Sin, scale=rc_sb[:, ci + 1:ci + 2]),
                             reads=["ang", "rc"], writes=["tabs"])
            P.dma("sp", tab_o[:, :, tsl].rearrange("k p t -> p k t"), tabs[:], reads=["tabs"])
            ps, key = fm_proj("ckv")
            P.op("act", lambda E, ps=ps: E.copy(ckv[:], ps[:]), reads=[key], writes=["ckv"])
            P.op("act", lambda E: E.activation(sq[:, 0, :], ckv[:], AF.Square), reads=["ckv"], writes=["sq"])
            ps2, key2 = next_pa()
            P.op("pe", lambda E, ps2=ps2: E.matmul(ps2[:], lhsT=ones[:], rhs=sq[:, 0, :], start=True, stop=True), reads=["ones", "sq"], writes=[key2])
            P.op("act", lambda E, ps2=ps2: E.activation(t1[:], ps2[:], AF.Ln, bias=EPS, scale=1.0 / 128), reads=[key2], writes=["t1"])
            P.op("act", lambda E: E.activation(t1[:], t1[:], AF.Exp, scale=-0.5), reads=["t1"], writes=["t1"])
            P.op("dve", lambda E: E.scalar_tensor_tensor(ckvn[:], ckv[:], kvg_sb[:, 0:1], t1[:], ALU.mult, ALU.mult),
                 reads=["ckv", "kvg", "t1"], writes=["ckvn"])
            for hp in range(2):
                psa, ka = next_pa()
                psb, kb = next_pa()
                P.op("pe", lambda E, hp=hp, psa=psa: E.matmul(psa[:], lhsT=wuk_sb[:, hp * 128:(hp + 1) * 128], rhs=ckvn[:], start=True, stop=True),
                     reads=["wuk", "ckvn"], writes=[ka])
                P.op("pe", lambda E, hp=hp, psb=psb: E.matmul(psb[:], lhsT=wuk_sb[:, 256 + hp * 128:256 + (hp + 1) * 128], rhs=ckvn[:], start=True, stop=True),
                     reads=["wuk", "ckvn"], writes=[kb])
                rope_combine(psa, ka, psb, kb, 128, 0, 1, KFM_ROWS["dsak"] + hp * 128, tt)
            rope_out("idxk", "idxk_p", 32, 2, 3, KFM_ROWS["idxk"], tt)
            plain_out("foxk0", 128, KFM_ROWS["foxk"], tt, "act")
            plain_out("foxk1", 128, KFM_ROWS["foxk"] + 128, tt, "dve")
            plain_out("sbk0", 128, KFM_ROWS["sbk"], tt, "act")
            plain_out("sbk1", 128, KFM_ROWS["sbk"] + 128, tt, "dve")
            rope_out("kcks", "kcks_p", 128, 0, 1, KFM_ROWS["kcks"], tt)
            rope_out("kw", "kw_p", 64, 0, 1, KFM_ROWS["kw"], tt)
            plain_out("vc", 64, KFM_ROWS["vc"], tt, "act")
            for st in range(4):
                tm_part(tt, st)

        def tm_part(tt, st):
            if True:
                tok0 = tt * 512 + st * 128
                bi = (tt * 4 + st) % 2
                v3, v3k = v3t[bi], "v3t%d" % bi
                v2, v2k = v2t[bi], "v2t%d" % bi
                lf, lfk = lft[bi], "lft%d" % bi
                hs = slice(st * 128, (st + 1) * 128)
                ps_v, kv = next_pa()
                P.op("pe", lambda E, ps_v=ps_v: E.matmul(ps_v[:, :256], lhsT=ckvn[:, hs], rhs=wuv_sb[:], start=True, stop=True),
                     reads=["ckvn", "wuv"], writes=[kv])
                P.op("act", lambda E, ps_v=ps_v, v3=v3: E.copy(v3[:, 0, :, 0:64], ps_v[:, :256].rearrange("p (h d) -> p h d", d=64)),
                     reads=[kv], writes=[v3k])
                ps_a, kaa = next_pa()
                for c in range(8):
                    P.op("pe", lambda E, c=c, ps_a=ps_a: E.matmul(ps_a[:], lhsT=h_sb[:, c, hs], rhs=wtm_sb[:, c, 0:512], start=(c == 0), stop=(c == 7)),
                         reads=["h", "wtm"], writes=[kaa])
                P.op("dve", lambda E, ps_a=ps_a, v3=v3: E.tensor_copy(v3[:, 1:3, :, 0:64], ps_a[:].rearrange("p (b h d) -> p b h d", b=2, d=64)),
                     reads=[kaa], writes=[v3k])
                ps_b, kbb = next_pa()
                for c in range(8):
                    P.op("pe", lambda E, c=c, ps_b=ps_b: E.matmul(ps_b[:, :132], lhsT=h_sb[:, c, hs], rhs=wtm_sb[:, c, 512:644], start=(c == 0), stop=(c == 7)),
                         reads=["h", "wtm"], writes=[kbb])
                P.op("act", lambda E, ps_b=ps_b, v2=v2: E.copy(v2[:, :, 0:64], ps_b[:, :128].rearrange("p (b d) -> p b d", d=64)),
                     reads=[kbb], writes=[v2k])
                P.op("dve", lambda E, ps_b=ps_b: E.tensor_tensor(lfe[:], ps_b[:, 128:132], nfoxb[:], ALU.subtract), reads=[kbb, "nfoxb"], writes=["lfe"])
                P.op("act", lambda E: E.activation(lfe[:], lfe[:], AF.Exp, scale=-1.0), reads=["lfe"], writes=["lfe"])
                P.op("act", lambda E, lf=lf: E.activation(lf[:], lfe[:], AF.Ln, bias=1.0), reads=["lfe"], writes=[lfk])
                P.dma("sp", v3_o[:, tok0:tok0 + 128, :].rearrange("b t n -> t b n"), v3[:].rearrange("p b h d -> p b (h d)"), reads=[v3k])
                P.dma("sp", v2_o[:, tok0:tok0 + 128, :].rearrange("b t n -> t b n"), v2[:], reads=[v2k])
                P.dma("sp", lf_o[tok0:tok0 + 128, :], lf[:], reads=[lfk])
        for tt in range(NT):
            do_tile(tt)
        P.barrier()
        P.emit()
    return nc


IN_LAYOUT = (
    ('dsa_q', 256), ('dsa_ckv', 128), ('idx_q', 128), ('idx_k', 32), ('idx_w', 4), ('dsa_z', 256),
    ('fox_q', 256), ('fox_k', 256), ('fox_v', 256), ('fox_f', 4), ('fox_z', 256),
    ('sb_q', 256), ('sb_k', 256), ('sb_v', 256), ('sb_z', 256),
    ('nsa_q', 256), ('nsa_kc', 64), ('nsa_vc', 64), ('nsa_ks', 64), ('nsa_vs', 64),
    ('nsa_kw', 64), ('nsa_vw', 64), ('nsa_g', 12), ('nsa_z', 256),
    ('mem_q', 256), ('mem_z', 256),
    ('merge', 5 * 1024),
)
COL = {}
_o = 0
for _n, _w in IN_LAYOUT:
    COL[_n] = (_o, _w)
    _o += _w
N_IN = _o


def _cols(w_in, name):
    o, n = COL[name]
    return w_in[:, o:o + n]


def _perm_idx(ncols, dh):
    idx = np.arange(ncols)
    base = (idx // dh) * dh
    return base + (idx % dh + dh // 2) % dh


def _rope_consts():
    p = np.arange(128)
    rc = np.zeros((128, 8), np.float32)
    rc[:, 0] = 10000.0 ** (-(2.0 * (p % 32)) / 64.0)
    s64 = np.where((p % 64) < 32, -1.0, 1.0)
    rc[:, 1] = s64
    rc[:, 2] = -PI * s64
    rc[:, 3] = 10000.0 ** (-(2.0 * (p % 16)) / 32.0)
    s32 = np.where((p % 32) < 16, -1.0, 1.0)
    rc[:, 4] = s32
    rc[:, 5] = -PI * s32
    return rc


def host_prep_A(norm_g, w_in, kv_norm, w_uk, w_uv, fox_bias):
    c = lambda n: _cols(w_in, n)
    kcks = np.concatenate([c('nsa_kc'), c('nsa_ks')], 1)
    wfm = np.concatenate([
        c('dsa_ckv'), c('idx_k'), c('idx_k')[:, _perm_idx(32, 32)],
        c('fox_k'), c('sb_k'), kcks, kcks[:, _perm_idx(128, 64)],
        c('nsa_kw'), c('nsa_kw')[:, _perm_idx(64, 64)], c('nsa_vc')], 1)
    wtm = np.concatenate([c('fox_v'), c('sb_v'), c('nsa_vs'), c('nsa_vw'), c('fox_f')], 1)
    assert wfm.shape[1] == A_NFM and wtm.shape[1] == A_NTM
    return {
        "gcol": np.ascontiguousarray(norm_g.reshape(8, 128).T),
        "wfm": np.ascontiguousarray(wfm), "wtm": np.ascontiguousarray(wtm),
        "kvg": np.ascontiguousarray(kv_norm.reshape(128, 1)),
        "wuk2": np.ascontiguousarray(np.concatenate([w_uk, w_uk[:, _perm_idx(256, 64)]], 1)),
        "wuv": np.ascontiguousarray(w_uv),
        "foxb": np.ascontiguousarray(np.broadcast_to(fox_bias.reshape(1, 4), (128, 4))),
        "rc": _rope_consts(),
    }


B_WQ = {"dsa": (0, 512), "idx": (512, 256), "fox": (768, 256), "sb": (1024, 256), "nsa": (1280, 512), "mem": (1792, 256)}
B_NWQ = 2048
NEG_BIG = -1.0e30
BIS_R0 = 512.0
BIS_K = 20


def build_B(T, ctx=None):
    nc = ctx["nc"] if ctx else bass.Bass("TRN2", target_bir_lowering=False)
    ov = ctx["ov"] if ctx else {}
    G = bool(ctx)
    FINAL = ctx["final"] if ctx else True
    S = 4 * T
    NB = S // 128
    NQ = T // 128
    NG = T // 512
    NBS = S // 64
    NCMP = S // 16 - 1
    NCC = (NCMP + 127) // 128
    NCP = NCC * 128
    KSEL = min(256, S // 4)
    SCALE = 0.125
    dr = lambda name, shape, dt, kind="ExternalInput": ov[name] if name in ov else nc.dram_tensor(name, list(shape), dt, kind=kind).ap()
    xT = dr("xT", [1024, T], F32)
    hT = dr("hT", [1024, T], BF16)
    tab = dr("tab", [4, 128, T], F32)
    kfm = dr("kfm", [KFM_N, S], BF16)
    v3 = dr("v3", [3, S, 260], BF16)
    v2 = dr("v2", [2, S, 65], BF16)
    lf = dr("lf", [128, NB * 4], F32)
    wq = dr("wq", [1024, B_NWQ], F32)
    wsm = dr("wsm", [1024, 16], F32)
    wz = dr("wz", [1024, 1280], F32)
    wmg = dr("wmg", [1024, 5120], F32)
    wbr = dr("wbr", [5, 256, 1024], F32)
    wout = dr("wout", [1024, 1024], F32)
    w1 = dr("w1", [128, 32 * 128], F32)
    w2k2 = dr("w2k2", [128, 128], F32)
    w2v = dr("w2v", [128, 64], F32)
    peT = dr("peT", [128, 32], F32)
    memT = dr("memT", [1024, 256], F32)
    mgcol = dr("mgcol", [128, 8], F32)
    wmem = dr("wmem", [1024, 512], F32)
    fgcol = dr("fgcol", [128, 8], F32)
    c_bf = dr("c_bf", [128, 128 * 3 + 512 * 2 + 1024 + 512], BF16)
    c_f = dr("c_f", [128, 128 * 3 + 512 + 4 + 4 + NQ + NCC + NCP], F32)
    trow = dr("trow", [1, T], F32)
    visd = dr("vis", [128, NQ * NBS], F32)
    addd = dr("addc", [128, NQ * NBS], F32)
    xo_o = dr("xo", [1024, T], F32, "ExternalOutput")
    xn_o = dr("xn", [1024, T], F32, "ExternalOutput")
    ydram = dr("ydram", [NQ, 128, 1280], BF16, "ExternalOutput")

    with ExitStack() as stack:
        P = ctx["P"] if ctx else Prog(nc, stack)
        M = Pool2(nc, stack)
        PS = ctx["PS"] if ctx else [M.ps("ps%d" % i, [128, 512]) for i in range(8)]
        PK = ["PS%d" % i for i in range(8)]

        def pe(out, lhsT, rhs, st, sp, r, w):
            P.op("pe", lambda E: E.matmul(out, lhsT=lhsT, rhs=rhs, start=st, stop=sp), reads=r, writes=w)

        def tr(out, in_, r, w, f32=False):
            idn = identF if f32 else ident
            P.op("pe", lambda E: E.matmul(out, lhsT=in_, rhs=idn, start=True, stop=True), reads=list(r) + ["cbf", "cf"], writes=w)

        def act(out, in_, func, r, w, **kw):
            P.op("act", lambda E: E.activation(out, in_, func, **kw), reads=r, writes=w)

        def tt(eng, out, a, b, op, r, w):
            P.op(eng, lambda E: E.tensor_tensor(out, a, b, op), reads=r, writes=w)

        def ts(eng, out, a, s1, s2, op0, op1, r, w, accum=None):
            if accum is None:
                if op1 is None:
                    P.op(eng, lambda E: E.tensor_scalar(out, a, s1, s2, op0), reads=r, writes=w)
                else:
                    P.op(eng, lambda E: E.tensor_scalar(out, a, s1, s2, op0, op1), reads=r, writes=w)
            else:
                P.op(eng, lambda E: E.tensor_scalar(out, a, s1, s2, op0, op1, accum_out=accum), reads=r, writes=w)

        def stt(eng, out, a, s, b, op0, op1, r, w, accum=None):
            if accum is None:
                P.op(eng, lambda E: E.scalar_tensor_tensor(out, a, s, b, op0, op1), reads=r, writes=w)
            else:
                P.op(eng, lambda E: E.scalar_tensor_tensor(out, a, s, b, op0, op1, accum_out=accum), reads=r, writes=w)

        def cp(eng, out, in_, r, w):
            if eng == "act":
                P.op("act", lambda E: E.copy(out, in_), reads=r, writes=w)
            else:
                P.op(eng, lambda E: E.tensor_copy(out, in_), reads=r, writes=w)

        def mset(out, val, w, eng="pool"):
            P.op(eng, lambda E: E.memset(out, val), writes=w)

        cbf = M.sb("cbf", [128, 128 * 3 + 512 * 2 + 1024 + 512], BF16)
        cf = M.sb("cf", [128, 128 * 3 + 512 + 4 + 4 + NQ + NCC + NCP], F32)
        P.dma("sp", cbf[:], c_bf, writes=["cbf"])
        P.dma("sp", cf[:], c_f, writes=["cf"])
        ident = cbf[:, 0:128]
        triS = cbf[:, 128:256]
        ones_b = cbf[:, 256:384]
        dmask = cbf[:, 384:896].rearrange("p (j q) -> p j q", q=128)
        dmaskS = cbf[:, 896:1408].rearrange("p (j q) -> p j q", q=128)
        wmask = cbf[:, 1408:2432].rearrange("p (j q) -> p j q", q=128)
        dmaskN01 = cbf[:, 2432:2944]
        o = 0
        triInc = cf[:, o:o + 128]; o += 128
        ones_f = cf[:, o:o + 128]; o += 128
        identF = cf[:, o:o + 128]; o += 128
        dmaskN = cf[:, o:o + 512]; o += 512
        oh = cf[:, o:o + 4]; o += 4
        sel4 = cf[:, o:o + 4]; o += 4
        tq = cf[:, o:o + NQ]; o += NQ
        ncol = cf[:, o:o + NCC]; o += NCC
        nrow = cf[:, o:o + NCP]; o += NCP
        stg = M.sb("stg", [128, 8, 256], F32)
        cumcol = M.sb("cumcol", [128, 4, NB], F32)
        CPt = M.sb("CPt", [128, 4, NQ], F32)
        kc2T = M.sb("kc2T", [128, NCP], BF16)
        vc1 = M.sb("vc1", [128, NCC, 65], BF16)
        mkT = M.sb("mkT", [128, 2, 256], BF16)
        mv1 = M.sb("mv1", [128, 2, 4, 65], BF16)
        mset(vc1[:], 1.0, ["vc1"])
        mset(mv1[:], 1.0, ["mv1"])

        def ld_kfm(dst, row0, nrows, dkey):
            if not G:
                P.dma("sp", dst, kfm[row0:row0 + nrows, :], writes=[dkey])
            else:
                dv = dst.rearrange("p (i r q) -> p i r q", r=4, q=128)
                ch, off = row0 // 128, row0 % 128
                for r in range(4):
                    P.dma("sp", dv[:, :, r, :], kfm[ch][r * 128 + off:r * 128 + off + nrows, :].rearrange("p (i q) -> p i q", q=128),
                          writes=[dkey])

        def ld_v(dst, src, k, nk, dkey):
            if not G:
                P.dma("sp", dst, src[k].rearrange("(j s) n -> s j n", s=128), writes=[dkey])
            else:
                dv = dst.rearrange("s (i r) n -> s i r n", r=4)
                hq_ = NQ // 2
                for hf in range(2):
                    for r in range(4):
                        P.dma("sp", dv[:, hf * hq_:(hf + 1) * hq_, r, :],
                              src[k][hf][r * (T // 2):(r + 1) * (T // 2), :].rearrange("(i s) n -> s i n", s=128), writes=[dkey])

        def load_w(dst, dkey, src_cols, ncols, kchunks=8):
            c0 = 0
            while c0 < ncols:
                n = min(256, ncols - c0)
                P.dma("sp", stg[:, :kchunks, :n], src_cols(c0, n).rearrange("(c p) n -> p c n", p=128), writes=["stg"])
                P.op("dve", lambda E, c0=c0, n=n: E.tensor_copy(dst[:, :, c0:c0 + n], stg[:, :kchunks, :n]), reads=["stg"], writes=[dkey])
                c0 += n

        def load_small(dst, dkey, src, n):
            P.dma("sp", stg[:, 0, :n], src, writes=["stg"])
            P.op("dve", lambda E: E.tensor_copy(dst, stg[:, 0, :n]), reads=["stg"], writes=[dkey])

        with ExitStack() as st0:
            M0 = Pool2(nc, st0)
            lf_sb = M0.sb("lf", [128, NB * 4], F32)
            tot = M0.sb("tot", [128, NB * 4], F32)
            incl = M0.sb("incl", [128, 4, NB], F32)
            tmpc = M0.sb("tmpc", [128, 4, NB], F32)
            tmp4 = M0.sb("tmp4", [128, NQ, 4], F32)
            if not G:
                P.dma("sp", lf_sb[:], lf, writes=["lf"])
            else:
                lv = lf_sb[:].rearrange("s (i r h) -> s i r h", r=4, h=4)
                for r in range(4):
                    P.dma("sp", lv[:, :, r, :], lf[r * T:(r + 1) * T, :].rearrange("(i s) h -> s i h", s=128), writes=["lf"])
            pe(PS[4][:, :NB * 4], triInc, lf_sb[:], True, True, ["cf", "lf"], [PK[4]])
            pe(PS[6][:, :NB * 4], ones_f, lf_sb[:], True, True, ["cf", "lf"], [PK[6]])
            cp("act", tot[:], PS[6][:, :NB * 4], [PK[6]], ["tot"])
            tot3 = tot[:].rearrange("p (j h) -> p j h", h=4)
            cs3 = PS[4][:, :NB * 4].rearrange("p (j h) -> p j h", h=4)
            for h in range(4):
                P.op("dve", lambda E, h=h: E.tensor_tensor_scan(incl[:, h, :], ones_f[:, :NB], tot3[:, :, h], 0.0, ALU.mult, ALU.add),
                     reads=["cf", "tot"], writes=["incl"])
                tt("dve", tmpc[:, h, :], incl[:, h, :], tot3[:, :, h], ALU.subtract, ["incl", "tot"], ["tmpc"])
                tt("dve", cumcol[:, h, :], cs3[:, :, h], tmpc[:, h, :], ALU.add, [PK[4], "tmpc"], ["cumcol"])
                tt("dve", tmp4[:], incl[:, h, :].rearrange("p (i j) -> p i j", j=4), oh.unsqueeze(1).to_broadcast([128, NQ, 4]),
                   ALU.mult, ["incl", "cf"], ["tmp4"])
                P.op("dve", lambda E, h=h: E.tensor_reduce(CPt[:, h, :], tmp4[:], AX.X, ALU.add), reads=["tmp4"], writes=["CPt"])
            kvtok = M0.sb("kvtok", [128, S], BF16)
            W1 = M0.sb("W1", [128, 32, 128], BF16)
            w2k2_sb = M0.sb("w2k2", [128, 128], BF16)
            w2v_sb = M0.sb("w2v", [128, 64], BF16)
            peT_sb = M0.sb("peT", [128, 32], BF16)
            bH = M0.sb("bH", [128, 2], F32)
            hk = M0.sb("hk", [128, NCP], BF16)
            hv = M0.sb("hv", [128, NCP], BF16)
            ld_kfm(kvtok[0:64, :], KFM_ROWS["kcks"], 64, "kvtok")
            ld_kfm(kvtok[64:128, :], KFM_ROWS["vc"], 64, "kvtok")
            stg2 = stg[:].rearrange("p c n -> p (c n)")
            for half in range(2):
                P.dma("sp", stg2, w1[:, half * 2048:(half + 1) * 2048], writes=["stg"])
                P.op("dve", lambda E, half=half: E.tensor_copy(W1[:, half * 16:(half + 1) * 16, :].rearrange("p l h -> p (l h)"), stg2),
                     reads=["stg"], writes=["W1"])
            load_small(w2k2_sb[:], "w2k2", w2k2, 128)
            load_small(w2v_sb[:], "w2v", w2v, 64)
            load_small(peT_sb[:], "peT", peT, 32)
            mset(hk[:], 0.0, ["hk"])
            mset(hv[:], 0.0, ["hv"])
            for kv_i, (lo, hbuf, hkey) in enumerate(((0, hk, "hk"), (64, hv, "hv"))):
                for l in range(32):
                    pe(PS[4][:, kv_i:kv_i + 1], W1[lo:lo + 64, l, :], peT_sb[lo:lo + 64, l:l + 1], l == 0, l == 31, ["W1", "peT"], [PK[4]])
            cp("act", bH[:], PS[4][:, 0:2], [PK[4]], ["bH"])
            for kv_i, (lo, hbuf, hkey) in enumerate(((0, hk, "hk"), (64, hv, "hv"))):
                for l in range(32):
                    pe(PS[6][:, :NCMP], W1[lo:lo + 64, l, :], kvtok[lo:lo + 64, l:l + 16 * (NCMP - 1) + 1:16], l == 0, l == 31,
                       ["W1", "kvtok"], [PK[6]])
                act(hbuf[:, :NCMP], PS[6][:, :NCMP], AF.Silu, [PK[6], "bH"], [hkey], bias=bH[:, kv_i:kv_i + 1])
            pe(PS[4][:, :NCP], w2k2_sb[:], hk[:], True, True, ["w2k2", "hk"], [PK[4]])
            cp("act", kc2T[:], PS[4][:, :NCP], [PK[4]], ["kc2T"])
            for c in range(NCC):
                pe(PS[6][:, c * 64:(c + 1) * 64], hv[:, c * 128:(c + 1) * 128], w2v_sb[:], True, True, ["hv", "w2v"], [PK[6]])
            cp("act", vc1[:, :, 0:64], PS[6][:, :NCC * 64].rearrange("p (c d) -> p c d", d=64), [PK[6]], ["vc1"])
            xm = M0.sb("xm", [128, 8, 256], F32)
            sqm = M0.sb("sqm", [128, 8, 256], BF16)
            mh = M0.sb("mh", [128, 8, 256], BF16)
            rsm = M0.sb("rsm", [128, 256], F32)
            mg_sb = M0.sb("mg", [128, 8], F32)
            wmem_sb = M0.sb("wmem", [128, 8, 512], BF16)
            P.dma("sp", xm[:], memT.rearrange("(c p) t -> p c t", p=128), writes=["xm"])
            P.dma("sp", mg_sb[:], mgcol, writes=["mg"])
            load_w(wmem_sb, "wmem", lambda c0, n: wmem[:, c0:c0 + n], 512)
            act(sqm[:], xm[:], AF.Square, ["xm"], ["sqm"])
            for c in range(8):
                pe(PS[4][:, :256], ones_b, sqm[:, c, :], c == 0, c == 7, ["cbf", "sqm"], [PK[4]])
            act(rsm[:], PS[4][:, :256], AF.Ln, [PK[4]], ["rsm"], bias=EPS, scale=1.0 / 1024)
            act(rsm[:], rsm[:], AF.Exp, ["rsm"], ["rsm"], scale=-0.5)
            for c in range(8):
                stt("dve", mh[:, c, :], xm[:, c, :], mg_sb[:, c:c + 1], rsm[:], ALU.mult, ALU.mult, ["xm", "mg", "rsm"], ["mh"])
            for hp in range(2):
                for c in range(8):
                    pe(PS[6][:, :256], wmem_sb[:, c, hp * 128:(hp + 1) * 128], mh[:, c, :], c == 0, c == 7, ["wmem", "mh"], [PK[6]])
                cp("act", mkT[:, hp, :], PS[6][:, :256], [PK[6]], ["mkT"])
            for mc in range(2):
                for c in range(8):
                    pe(PS[4][:, :256], mh[:, c, mc * 128:(mc + 1) * 128], wmem_sb[:, c, 256:512], c == 0, c == 7, ["mh", "wmem"], [PK[4]])
                cp("act", mv1[:, mc, :, 0:64], PS[4][:, :256].rearrange("p (h d) -> p h d", d=64), [PK[4]], ["mv1"])
            P.barrier()
            P.emit()

        with ExitStack() as st1:
            M1 = Pool2(nc, st1)
            KT = M1.sb("KT", [128, 2, S], BF16)
            V1 = M1.sb("V1", [128, NB, 260], BF16)
            kiT4 = M1.sb("kiT4", [128, S], BF16)
            sc = M1.sb("sc", [128, S], F32)
            maskT = M1.sb("maskT", [128, NB, 128], BF16)
            junk = maskT[:].rearrange("p j q -> p (j q)")
            wqb = M1.sb("wqb", [128, 8, 512], BF16)
            wsm_sb = M1.sb("wsm", [128, 8, 16], BF16)
            wqb2 = M1.sb("wqb2", [128, 8, 256], BF16)
            hq = M1.sb("hq", [128, 8, 512], BF16)
            tabg = M1.sb("tabg", [128, 2, 512], F32)
            QT = M1.sb("QT", [128, 2, 512], BF16)
            qiT = M1.sb("qiT", [128, 512], BF16)
            qm = M1.sb("qm", [128, 4, 512], BF16)
            t1 = M1.sb("t1", [128, 512], F32)
            t2 = M1.sb("t2", [128, 512], F32)
            PT = [M1.sb("PT", [128, 4, 128], BF16) for _ in range(2)]
            ebuf = M1.sb("ebuf", [128, 512], F32)
            ubuf = M1.sb("ubuf", [128, 4, 128], BF16)
            ebufB = M1.sb("ebufB", [128, 512], F32)
            ubufB = M1.sb("ubufB", [128, 4, 128], BF16)
            Rt = M1.sb("Rt", [128, 128], F32)
            Bih = [M1.sb("Bih", [128, NB], F32) for _ in range(2)]
            rd = M1.sb("rd", [128, 4], F32)
            ybuf = [M1.sb("ybuf", [128, 4, 64], BF16) for _ in range(2)]
            rbuf = [M1.sb("rbuf", [128, 512], F32) for _ in range(2)]
            wsmall = M1.sb("wsmall", [128, 16], F32)
            g12 = M1.sb("g12", [128, 12], F32)
            mid = M1.sb("mid", [128, 1], F32)
            cnt = M1.sb("cnt", [128, 1], F32)
            tmpb = M1.sb("tmpb", [128, 1], F32)
            ec = t1[:, :NCP]
            impacc = t2[:, :NCP]
            cmaskN = ebuf[:, :NCP]
            cmaskT = M1.sb("cmaskT", [128, NCC, 128], BF16)
            eT = M1.sb("eT", [128, NCC, 128], BF16)
            rs4 = M1.sb("rs4", [128, 4], F32)
            imp4 = M1.sb("imp4", [128, NBS], F32)
            imp2 = M1.sb("imp2", [128, NBS], F32)
            m8a = M1.sb("m8a", [128, 8], F32)
            m8b = M1.sb("m8b", [128, 8], F32)
            bm = M1.sb("bm", [128, NBS], F32)
            vis_t = M1.sb("vis_t", [128, NBS], F32)
            add_t = M1.sb("add_t", [128, NBS], F32)
            trow_t = Rt
            yacc = rbuf[0][:, 0:256].rearrange("p (h d) -> p h d", d=64)
            ytmp = rbuf[1][:, 0:256].rearrange("p (h d) -> p h d", d=64)
            coef = M1.sb("coef", [128, 4], F32)
            ctr = {"ps": 0, "pt": 0, "po": 0, "y": 0, "b": 0, "r": 0, "sbk": 0}

            def nxt(name, n):
                v = ctr[name] % n
                ctr[name] += 1
                return v

            def load_hq(g):
                P.dma("sp", hq[:], hT[:, g * 512:(g + 1) * 512].rearrange("(c p) t -> p c t", p=128), writes=["hq"])

            def fm_q(ps_i, col0, wt=None, wkey="wqb"):
                wt = wqb if wt is None else wt
                for c in range(8):
                    pe(PS[ps_i][:], wt[:, c, col0:col0 + 128], hq[:, c, :], c == 0, c == 7, [wkey, "hq"], [PK[ps_i]])

            def load_tabs(g, k0):
                P.dma("sp", tabg[:], tab[k0:k0 + 2, :, g * 512:(g + 1) * 512].rearrange("k p t -> p k t"), writes=["tabg"])

            def make_QT(g, rope):
                if rope:
                    load_tabs(g, 0)
                for hp in range(2):
                    fm_q(4, hp * 128)
                    if rope:
                        fm_q(5, 256 + hp * 128)
                        tt("dve", t1[:], PS[4][:], tabg[:, 0, :], ALU.mult, [PK[4], "tabg"], ["t1"])
                        tt("dve", t2[:], PS[5][:], tabg[:, 1, :], ALU.mult, [PK[5], "tabg"], ["t2"])
                        tt("dve", QT[:, hp, :], t1[:], t2[:], ALU.add, ["t1", "t2"], ["QT"])
                    else:
                        cp("act", QT[:, hp, :], PS[4][:], [PK[4]], ["QT"])

            def qslice(h, qi):
                lo = (h % 2) * 64
                return QT[lo:lo + 64, h // 2, qi * 128:(qi + 1) * 128]

            def finalize(po_i, i, bidx):
                po3 = PS[po_i][:, :260].rearrange("p (h d) -> p h d", d=65)
                yb = nxt("y", 2)
                ts("dve", rd[:], po3[:, :, 64], 1e-30, None, ALU.max, None, [PK[po_i]], ["rd"])
                P.op("dve", lambda E: E.reciprocal(rd[:], rd[:]), reads=["rd"], writes=["rd"])
                tt("dve", ybuf[yb][:], po3[:, :, 0:64], rd[:].unsqueeze(2).to_broadcast([128, 4, 64]), ALU.mult,
                   [PK[po_i], "rd"], ["ybuf%d" % yb])
                P.dma("sp", ydram[i, :, bidx * 256:(bidx + 1) * 256], ybuf[yb][:].rearrange("p h d -> p (h d)"), reads=["ybuf%d" % yb], writes=["ydram"])

            def load_branch_kv(krow0, vidx):
                for hp in range(2):
                    ld_kfm(KT[:, hp, :], krow0 + hp * 128, 128, "KT")
                ld_v(V1[:], v3, vidx, 3, "V1")

            def kslice(h, j):
                lo = (h % 2) * 64
                return KT[lo:lo + 64, h // 2, j * 128:(j + 1) * 128]

            def fox_tile(g, qi):
                i = 4 * g + qi
                po_i = 2 + nxt("po", 2)
                for h in range(4):
                    b = nxt("b", 2)
                    bk = "Bih%d" % b
                    nblk = 4 * (i + 1)
                    ts("dve", Bih[b][:, :nblk], cumcol[:, h, :nblk], CPt[:, h, i:i + 1], 0.0, ALU.subtract, ALU.min,
                       ["cumcol", "CPt"], [bk])
                    for gg in range(i + 1):
                        ps_i = nxt("ps", 2)
                        p_i = nxt("pt", 2)
                        psv = PS[ps_i][:].rearrange("p (j q) -> p j q", q=128)
                        for jj in range(4):
                            pe(psv[:, jj, :], kslice(h, 4 * gg + jj), qslice(h, qi), True, True, ["KT", "QT"], [PK[ps_i]])
                        for jj in range(4):
                            act(PT[p_i][:, jj, :], psv[:, jj, :], AF.Exp, [PK[ps_i], bk], ["PT%d" % p_i],
                                bias=Bih[b][:, 4 * gg + jj:4 * gg + jj + 1], scale=SCALE)
                        if gg == i:
                            tt("dve", PT[p_i][:], PT[p_i][:], dmask, ALU.mult, ["PT%d" % p_i, "cbf"], ["PT%d" % p_i])
                        for jj in range(4):
                            pe(PS[po_i][:, h * 65:(h + 1) * 65], PT[p_i][:, jj, :], V1[:, 4 * gg + jj, h * 65:(h + 1) * 65],
                               gg == 0 and jj == 0, gg == i and jj == 3, ["PT%d" % p_i, "V1"], [PK[po_i]])
                finalize(po_i, i, 1)

            def sb_tile(g, qi):
                i = 4 * g + qi
                po_i = 2 + nxt("po", 2)
                for h in range(4):
                    mset(Rt[:], 0.0, ["Rt"])
                    for gg in range(i, -1, -1):
                        ps_i = nxt("ps", 2)
                        p_i = nxt("pt", 2)
                        psv = PS[ps_i][:].rearrange("p (j q) -> p j q", q=128)
                        kk = nxt("sbk", 2)
                        eb, ebk = (ebuf, "ebuf") if kk == 0 else (ebufB, "ebufB")
                        ub, ubk = (ubuf, "ubuf") if kk == 0 else (ubufB, "ubufB")
                        tb, tbk = (t1, "t1") if kk == 0 else (t2, "t2")
                        pl, pr = (6, 7) if kk == 0 else (4, 5)
                        ps6 = PS[pl][:].rearrange("p (j q) -> p j q", q=128)
                        for jj in range(4):
                            pe(psv[:, jj, :], kslice(h, 4 * gg + jj), qslice(h, qi), True, True, ["KT", "QT"], [PK[ps_i]])
                        act(eb[:], PS[ps_i][:], AF.Exp, [PK[ps_i]], [ebk], scale=SCALE)
                        act(ub[:].rearrange("p j q -> p (j q)"), eb[:], AF.Ln, [ebk], [ubk], bias=1.0)
                        if gg == i:
                            tt("dve", ub[:], ub[:], dmaskS, ALU.mult, [ubk, "cbf"], [ubk])
                        for jj in range(4):
                            pe(ps6[:, jj, :], triS, ub[:, jj, :], True, jj == 3, ["cbf", ubk], [PK[pl]])
                            for j2 in range(jj + 1, 4):
                                pe(ps6[:, jj, :], ones_b, ub[:, j2, :], False, j2 == 3, ["cbf", ubk], [PK[pl]])
                        if gg > 0:
                            for jj in range(4):
                                pe(PS[pr][:, :128], ones_b, ub[:, jj, :], jj == 0, jj == 3, ["cbf", ubk], [PK[pr]])
                        tt("dve", tb[:].rearrange("p (j q) -> p j q", q=128), ps6, Rt[:].unsqueeze(1).to_broadcast([128, 4, 128]),
                           ALU.add, [PK[pl], "Rt"], [tbk])
                        stt("dve", eb[:], PS[ps_i][:], SCALE, tb[:], ALU.mult, ALU.subtract, [PK[ps_i], tbk], [ebk])
                        act(PT[p_i][:].rearrange("p j q -> p (j q)"), eb[:], AF.Exp, [ebk], ["PT%d" % p_i])
                        if gg == i:
                            tt("dve", PT[p_i][:], PT[p_i][:], dmaskS, ALU.mult, ["PT%d" % p_i, "cbf"], ["PT%d" % p_i])
                        for jj in range(4):
                            pe(PS[po_i][:, h * 65:(h + 1) * 65], PT[p_i][:, jj, :], V1[:, 4 * gg + jj, h * 65:(h + 1) * 65],
                               gg == i and jj == 0, gg == 0 and jj == 3, ["PT%d" % p_i, "V1"], [PK[po_i]])
                        if gg > 0:
                            tt("dve", Rt[:], Rt[:], PS[pr][:, :128], ALU.add, ["Rt", PK[pr]], ["Rt"])
                yb = nxt("y", 2)
                po3 = PS[po_i][:, :260].rearrange("p (h d) -> p h d", d=65)
                cp("act", ybuf[yb][:], po3[:, :, 0:64], [PK[po_i]], ["ybuf%d" % yb])
                P.dma("sp", ydram[i, :, 2 * 256:3 * 256], ybuf[yb][:].rearrange("p h d -> p (h d)"), reads=["ybuf%d" % yb], writes=["ydram"])

            def masked_attn(i, qi, po_i, kfn, vfn, kkeys, vkeys):
                for h in range(4):
                    for gg in range(i + 1):
                        ps_i = nxt("ps", 2)
                        p_i = nxt("pt", 2)
                        psv = PS[ps_i][:].rearrange("p (j q) -> p j q", q=128)
                        for jj in range(4):
                            pe(psv[:, jj, :], kfn(h, 4 * gg + jj), qslice(h, qi), True, True, kkeys + ["QT"], [PK[ps_i]])
                        act(PT[p_i][:].rearrange("p j q -> p (j q)"), PS[ps_i][:], AF.Exp, [PK[ps_i]], ["PT%d" % p_i], scale=SCALE)
                        tt("dve", PT[p_i][:], PT[p_i][:], maskT[:, 4 * gg:4 * gg + 4, :], ALU.mult, ["PT%d" % p_i, "maskT"], ["PT%d" % p_i])
                        for jj in range(4):
                            pe(PS[po_i][:, h * 65:(h + 1) * 65], PT[p_i][:, jj, :], vfn(h, 4 * gg + jj),
                               gg == 0 and jj == 0, gg == i and jj == 3, ["PT%d" % p_i] + vkeys, [PK[po_i]])

            def build_maskT(i):
                nblk = 4 * (i + 1)
                for j0 in range(0, nblk, 4):
                    ps_i = 6 + nxt("r", 2)
                    psv = PS[ps_i][:].rearrange("p (j q) -> p j q", q=128)
                    for jj in range(4):
                        tr(psv[:, jj, :], sc[:, (j0 + jj) * 128:(j0 + jj + 1) * 128], ["sc"], [PK[ps_i]], f32=True)
                    cp("act", maskT[:, j0:j0 + 4, :], psv, [PK[ps_i]], ["maskT"])

            def small_proj(qi):
                for c in range(8):
                    pe(PS[5][:, :16], hq[:, c, qi * 128:(qi + 1) * 128], wsm_sb[:, c, :], c == 0, c == 7, ["hq", "wsm"], [PK[5]])
                cp("act", wsmall[:], PS[5][:, :16], [PK[5]], ["wsmall"])

            def dsa_tile(g, qi):
                i = 4 * g + qi
                L = 512 * (i + 1)
                small_proj(qi)
                for gg in range(i + 1):
                    for h in range(4):
                        ps_i = 6 + nxt("r", 2)
                        rb = nxt("b", 2)
                        pe(PS[ps_i][:], qm[:, h, qi * 128:(qi + 1) * 128], kiT4[:, gg * 512:(gg + 1) * 512], True, True, ["qm", "kiT4"], [PK[ps_i]])
                        act(rbuf[rb][:], PS[ps_i][:], AF.Relu, [PK[ps_i]], ["rbuf%d" % rb])
                        if h == 0:
                            ts("dve", sc[:, gg * 512:(gg + 1) * 512], rbuf[rb][:], wsmall[:, 0:1], None, ALU.mult, None,
                               ["rbuf%d" % rb, "wsmall"], ["sc"])
                        else:
                            stt("dve", sc[:, gg * 512:(gg + 1) * 512], rbuf[rb][:], wsmall[:, h:h + 1], sc[:, gg * 512:(gg + 1) * 512],
                                ALU.mult, ALU.add, ["rbuf%d" % rb, "wsmall", "sc"], ["sc"])
                tt("dve", sc[:, i * 512:(i + 1) * 512], sc[:, i * 512:(i + 1) * 512], dmaskN, ALU.add, ["sc", "cf"], ["sc"])
                mset(mid[:], 0.0, ["mid"], eng="dve")
                w = BIS_R0
                for _ in range(BIS_K):
                    ts("dve", junk[:, :L], sc[:, :L], mid[:, 0:1], None, ALU.is_ge, ALU.add, ["sc", "mid"], ["maskT", "cnt"], accum=cnt[:, 0:1])
                    ts("dve", tmpb[:], cnt[:], KSEL - 0.5, w, ALU.is_ge, ALU.mult, ["cnt"], ["tmpb"])
                    stt("dve", mid[:], tmpb[:], -0.5 * w, mid[:], ALU.add, ALU.add, ["tmpb", "mid"], ["mid"])
                    w *= 0.5
                ts("dve", mid[:], mid[:], -w, None, ALU.add, None, ["mid"], ["mid"])
                ts("dve", sc[:, :L], sc[:, :L], mid[:, 0:1], None, ALU.is_ge, None, ["sc", "mid"], ["sc"])
                build_maskT(i)
                po_i = 2 + nxt("po", 2)
                masked_attn(i, qi, po_i, kslice, lambda h, j: V1[:, j, h * 65:(h + 1) * 65], ["KT"], ["V1"])
                finalize(po_i, i, 0)

            def mem_tile(g, qi):
                i = 4 * g + qi
                po_i = 2 + nxt("po", 2)
                for h in range(4):
                    ps_i = nxt("ps", 2)
                    p_i = nxt("pt", 2)
                    psv = PS[ps_i][:].rearrange("p (j q) -> p j q", q=128)
                    lo = (h % 2) * 64
                    for c in range(2):
                        pe(psv[:, c, :], mkT[lo:lo + 64, h // 2, c * 128:(c + 1) * 128], qslice(h, qi), True, True, ["mkT", "QT"], [PK[ps_i]])
                    act(PT[p_i][:, 0:2, :], psv[:, 0:2, :], AF.Exp, [PK[ps_i]], ["PT%d" % p_i], scale=SCALE)
                    for c in range(2):
                        pe(PS[po_i][:, h * 65:(h + 1) * 65], PT[p_i][:, c, :], mv1[:, c, h, :], c == 0, c == 1, ["PT%d" % p_i, "mv1"], [PK[po_i]])
                finalize(po_i, i, 4)

            def nsa_tile(g, qi):
                i = 4 * g + qi
                L = 512 * (i + 1)
                po_c, po_s, po_w = 2, 3, 5
                small_proj(qi)
                act(g12[:], wsmall[:, 4:16], AF.Sigmoid, ["wsmall"], ["g12"])
                P.dma("sp", vis_t[:], visd[:, i * NBS:(i + 1) * NBS], writes=["vis_t"])
                P.dma("sp", add_t[:], addd[:, i * NBS:(i + 1) * NBS], writes=["add_t"])
                P.dma("sp", trow_t[:], trow[:, i * 128:(i + 1) * 128].partition_broadcast(128), writes=["Rt"])
                ts("dve", cmaskN, nrow, tq[:, i:i + 1], None, ALU.is_le, None, ["cf"], ["ebuf"])
                for c in range(NCC):
                    ts("dve", cmaskT[:, c, :], trow_t[:], ncol[:, c:c + 1], None, ALU.is_ge, None, ["Rt", "cf"], ["cmaskT"])
                for h in range(4):
                    lo = (h % 2) * 64
                    pe(PS[4][:, :NCP], qslice(h, qi), kc2T[lo:lo + 64, :], True, True, ["QT", "kc2T"], [PK[4]])
                    act(ec, PS[4][:, :NCP], AF.Exp, [PK[4]], ["t1"], scale=SCALE)
                    stt("dve", ec, ec, 1.0, cmaskN, ALU.mult, ALU.mult, ["t1", "ebuf"], ["t1", "rs4"], accum=rs4[:, h:h + 1])
                    ts("dve", rs4[:, h:h + 1], rs4[:, h:h + 1], 1e-30, None, ALU.max, None, ["rs4"], ["rs4"])
                    P.op("dve", lambda E, h=h: E.reciprocal(rs4[:, h:h + 1], rs4[:, h:h + 1]), reads=["rs4"], writes=["rs4"])
                    if h == 0:
                        ts("dve", impacc, ec, rs4[:, 0:1], None, ALU.mult, None, ["t1", "rs4"], ["t2"])
                    else:
                        stt("dve", impacc, ec, rs4[:, h:h + 1], impacc, ALU.mult, ALU.add, ["t1", "rs4", "t2"], ["t2"])
                P.op("dve", lambda E: E.tensor_reduce(imp4[:], impacc.rearrange("p (b u) -> p b u", u=4), AX.X, ALU.add),
                     reads=["t2"], writes=["imp4"])
                tt("dve", imp4[:], imp4[:], vis_t[:], ALU.mult, ["imp4", "vis_t"], ["imp4"])
                tt("dve", imp4[:], imp4[:], add_t[:], ALU.add, ["imp4", "add_t"], ["imp4"])
                P.op("dve", lambda E: E.max(out=m8a[:], in_=imp4[:]), reads=["imp4"], writes=["m8a"])
                P.op("dve", lambda E: E.match_replace(out=imp2[:], in_to_replace=m8a[:], in_values=imp4[:], imm_value=-1.0e30),
                     reads=["imp4", "m8a"], writes=["imp2"])
                P.op("dve", lambda E: E.max(out=m8b[:], in_=imp2[:]), reads=["imp2"], writes=["m8b"])
                ts("dve", bm[:], imp4[:], m8b[:, 7:8], None, ALU.is_ge, None, ["imp4", "m8b"], ["bm"])
                nsb = 8 * (i + 1)
                cp("dve", sc[:, :L].rearrange("p (b u) -> p b u", u=64), bm[:, :nsb].unsqueeze(2).to_broadcast([128, nsb, 64]), ["bm"], ["sc"])
                tt("dve", sc[:, i * 512:(i + 1) * 512], sc[:, i * 512:(i + 1) * 512], dmaskN01, ALU.mult, ["sc", "cbf"], ["sc"])
                build_maskT(i)
                masked_attn(i, qi, po_s, lambda h, j: KT[(h % 2) * 64:(h % 2) * 64 + 64, 0, j * 128:(j + 1) * 128],
                            lambda h, j: V1[:, j, 0:65], ["KT"], ["V1"])
                for h in range(4):
                    lo = (h % 2) * 64
                    ps_i = nxt("ps", 2)
                    psv = PS[ps_i][:].rearrange("p (j q) -> p j q", q=128)
                    for c in range(NCC):
                        pe(psv[:, c, :], kc2T[lo:lo + 64, c * 128:(c + 1) * 128], qslice(h, qi), True, True, ["kc2T", "QT"], [PK[ps_i]])
                    act(eT[:], psv[:, 0:NCC, :], AF.Exp, [PK[ps_i]], ["eT"], scale=SCALE)
                    tt("dve", eT[:], eT[:], cmaskT[:], ALU.mult, ["eT", "cmaskT"], ["eT"])
                    for c in range(NCC):
                        pe(PS[po_c][:, h * 65:(h + 1) * 65], eT[:, c, :], vc1[:, c, :], c == 0, c == NCC - 1, ["eT", "vc1"], [PK[po_c]])
                for h in range(4):
                    lo = (h % 2) * 64
                    grps = [gr for gr in range(2) if 4 * i - 4 + 4 * gr >= 0]
                    for gr in grps:
                        kb0 = 4 * i - 4 + 4 * gr
                        ps_i = nxt("ps", 2)
                        p_i = nxt("pt", 2)
                        psv = PS[ps_i][:].rearrange("p (j q) -> p j q", q=128)
                        for jj in range(4):
                            pe(psv[:, jj, :], KT[lo:lo + 64, 1, (kb0 + jj) * 128:(kb0 + jj + 1) * 128], qslice(h, qi), True, True, ["KT", "QT"], [PK[ps_i]])
                        act(PT[p_i][:].rearrange("p j q -> p (j q)"), PS[ps_i][:], AF.Exp, [PK[ps_i]], ["PT%d" % p_i], scale=SCALE)
                        tt("dve", PT[p_i][:], PT[p_i][:], wmask[:, 4 * gr:4 * gr + 4, :], ALU.mult, ["PT%d" % p_i, "cbf"], ["PT%d" % p_i])
                        for jj in range(4):
                            pe(PS[po_w][:, h * 65:(h + 1) * 65], PT[p_i][:, jj, :], V1[:, kb0 + jj, 65:130],
                               gr == grps[0] and jj == 0, gr == grps[-1] and jj == 3, ["PT%d" % p_i, "V1"], [PK[po_w]])
                for b, po_i in enumerate((po_c, po_s, po_w)):
                    po3 = PS[po_i][:, :260].rearrange("p (h d) -> p h d", d=65)
                    ts("dve", rd[:], po3[:, :, 64], 1e-30, None, ALU.max, None, [PK[po_i]], ["rd"])
                    P.op("dve", lambda E: E.reciprocal(rd[:], rd[:]), reads=["rd"], writes=["rd"])
                    tt("dve", coef[:], rd[:], g12[:, b * 4:(b + 1) * 4], ALU.mult, ["rd", "g12"], ["coef"])
                    if b == 0:
                        tt("dve", yacc, po3[:, :, 0:64], coef[:].unsqueeze(2).to_broadcast([128, 4, 64]), ALU.mult, [PK[po_i], "coef"], ["rbuf0"])
                    else:
                        tt("dve", ytmp, po3[:, :, 0:64], coef[:].unsqueeze(2).to_broadcast([128, 4, 64]), ALU.mult, [PK[po_i], "coef"], ["rbuf1"])
                        tt("dve", yacc, yacc, ytmp, ALU.add, ["rbuf0", "rbuf1"], ["rbuf0"])
                yb = nxt("y", 2)
                cp("act", ybuf[yb][:], yacc, ["rbuf0"], ["ybuf%d" % yb])
                P.dma("sp", ydram[i, :, 3 * 256:4 * 256], ybuf[yb][:].rearrange("p h d -> p (h d)"), reads=["ybuf%d" % yb], writes=["ydram"])

            def load_wq(name, wt=None, wkey="wqb"):
                off, n = B_WQ[name]
                load_w(wqb if wt is None else wt, wkey, lambda c0, nn: wq[:, off + c0:off + c0 + nn], n)

            def run_branch(qgroup_fn, tile_fn):
                for g in range(NG):
                    load_hq(g)
                    qgroup_fn(g)
                    for qi in range(4):
                        tile_fn(g, qi)

            load_w(wsm_sb, "wsm", lambda c0, nn: wsm[:, c0:c0 + nn], 16)
            load_branch_kv(KFM_ROWS["foxk"], 1)
            load_wq("fox")
            run_branch(lambda g: make_QT(g, False), fox_tile)
            load_branch_kv(KFM_ROWS["sbk"], 2)
            load_wq("sb")
            run_branch(lambda g: make_QT(g, False), sb_tile)
            load_wq("mem")
            run_branch(lambda g: make_QT(g, False), mem_tile)
            load_branch_kv(KFM_ROWS["dsak"], 0)
            for h in range(4):
                ld_kfm(kiT4[32 * h:32 * h + 32, :], KFM_ROWS["idxk"], 32, "kiT4")
            load_wq("dsa")
            load_wq("idx", wqb2, "wqb2")

            def dsa_group(g):
                make_QT(g, True)
                load_tabs(g, 2)
                fm_q(4, 0, wqb2, "wqb2")
                fm_q(5, 128, wqb2, "wqb2")
                tt("dve", t1[:], PS[4][:], tabg[:, 0, :], ALU.mult, [PK[4], "tabg"], ["t1"])
                tt("dve", t2[:], PS[5][:], tabg[:, 1, :], ALU.mult, [PK[5], "tabg"], ["t2"])
                tt("dve", qiT[:], t1[:], t2[:], ALU.add, ["t1", "t2"], ["qiT"])
                for h in range(4):
                    ts("dve", qm[:, h, :], qiT[:], sel4[:, h:h + 1], None, ALU.mult, None, ["qiT", "cf"], ["qm"])

            run_branch(dsa_group, dsa_tile)
            ks0 = KFM_ROWS["kcks"] + 64
            for half in range(2):
                ld_kfm(KT[64 * half:64 * half + 64, 0, :], ks0, 64, "KT")
                ld_kfm(KT[64 * half:64 * half + 64, 1, :], KFM_ROWS["kw"], 64, "KT")
            ld_v(V1[:, :, 0:65], v2, 0, 2, "V1")
            ld_v(V1[:, :, 65:130], v2, 1, 2, "V1")
            load_wq("nsa")
            run_branch(lambda g: make_QT(g, True), nsa_tile)
            P.barrier()
            P.emit()

        with ExitStack() as st2:
            M2 = Pool2(nc, st2)
            NT3 = T // 256
            wz_sb = M2.sb("wz", [128, 8, 1280], BF16)
            wmg_sb = M2.sb("wmg", [128, 8, 5120], BF16)
            wbr_sb = M2.sb("wbr", [128, 5, 2, 1024], BF16)
            wout_sb = M2.sb("wout", [128, 8, 1024], BF16)
            fg_sb = M2.sb("fg", [128, 8], F32)
            h3 = M2.sb("h3", [128, 8, 256], BF16)
            x3 = M2.sb("x3", [128, 8, 256], F32)
            yl = M2.sb("yl", [128, 1280], BF16)
            zs = M2.sb("zs", [128, 1280], BF16)
            ysT = M2.sb("ysT", [128, 10, 256], BF16)
            sg = [M2.sb("sg", [128, 256], F32) for _ in range(2)]
            tmpm = M2.sb("tmpm", [128, 256], F32)
            macc = M2.sb("macc", [128, 256], F32)
            mergedT = M2.sb("mergedT", [128, 8, 256], BF16)
            xo = M2.sb("xo", [128, 8, 256], F32)
            rstd3 = M2.sb("rstd3", [128, 256], F32)
            c3 = {"ps": 0, "r": 0, "sg": 0}

            def nx3(name, n):
                v = c3[name] % n
                c3[name] += 1
                return v

            P.dma("sp", fg_sb[:], fgcol, writes=["fg"])
            load_w(wz_sb, "wz", lambda c0, nn: wz[:, c0:c0 + nn], 1280)
            load_w(wmg_sb, "wmg", lambda c0, nn: wmg[:, c0:c0 + nn], 5120)
            load_w(wout_sb, "wout", lambda c0, nn: wout[:, c0:c0 + nn], 1024)
            stgf = stg[:].rearrange("p c n -> p (c n)")
            for n in range(5):
                for kk in range(2):
                    P.dma("sp", stgf[:, :1024], wbr[n, kk * 128:(kk + 1) * 128, :], writes=["stg"])
                    P.op("dve", lambda E, n=n, kk=kk: E.tensor_copy(wbr_sb[:, n, kk, :], stgf[:, :1024]), reads=["stg"], writes=["wbr"])

            def p3_group(tg):
                tsl = slice(tg * 256, (tg + 1) * 256)
                P.dma("sp", h3[:], hT[:, tsl].rearrange("(c p) t -> p c t", p=128), writes=["h3"])
                P.dma("sp", x3[:], xT[:, tsl].rearrange("(c p) t -> p c t", p=128), writes=["x3"])
                for sub in range(2):
                    i = 2 * tg + sub
                    qs = slice(sub * 128, (sub + 1) * 128)
                    P.dma("sp", yl[:], ydram[i], reads=["ydram"], writes=["yl"])
                    for (pi, c0, n) in ((4, 0, 512), (5, 512, 512), (6, 1024, 256)):
                        for c in range(8):
                            pe(PS[pi][:, :n], h3[:, c, qs], wz_sb[:, c, c0:c0 + n], c == 0, c == 7, ["h3", "wz"], [PK[pi]])
                        act(zs[:, c0:c0 + n], PS[pi][:, :n], AF.Silu, [PK[pi]], ["zs"])
                    tt("dve", zs[:], zs[:], yl[:], ALU.mult, ["zs", "yl"], ["zs"])
                    for k0 in (0, 4, 8):
                        nb = min(4, 10 - k0)
                        ps_i = nx3("ps", 2)
                        psv = PS[ps_i][:].rearrange("p (j q) -> p j q", q=128)
                        for kk in range(nb):
                            tr(psv[:, kk, :], zs[:, (k0 + kk) * 128:(k0 + kk + 1) * 128], ["zs"], [PK[ps_i]])
                        cp("act", ysT[:, k0:k0 + nb, qs], psv[:, 0:nb, :], [PK[ps_i]], ["ysT"])
                for dch in range(8):
                    ds_ = slice(dch * 128, (dch + 1) * 128)
                    for n in range(5):
                        pb = nx3("ps", 2)
                        pg = 6 + nx3("r", 2)
                        si = nx3("sg", 2)
                        for kk in range(2):
                            pe(PS[pb][:, :256], wbr_sb[:, n, kk, ds_], ysT[:, 2 * n + kk, :], kk == 0, kk == 1, ["wbr", "ysT"], [PK[pb]])
                        for c in range(8):
                            pe(PS[pg][:, :256], wmg_sb[:, c, n * 1024 + dch * 128:n * 1024 + (dch + 1) * 128], h3[:, c, :], c == 0, c == 7,
                               ["wmg", "h3"], [PK[pg]])
                        act(sg[si][:], PS[pg][:, :256], AF.Sigmoid, [PK[pg]], ["sg%d" % si])
                        if n == 0:
                            tt("dve", macc[:], PS[pb][:, :256], sg[si][:], ALU.mult, [PK[pb], "sg%d" % si], ["macc"])
                        else:
                            tt("dve", tmpm[:], PS[pb][:, :256], sg[si][:], ALU.mult, [PK[pb], "sg%d" % si], ["tmpm"])
                            tt("dve", macc[:], macc[:], tmpm[:], ALU.add, ["macc", "tmpm"], ["macc"])
                    cp("act", mergedT[:, dch, :], macc[:], ["macc"], ["mergedT"])
                for dch in range(8):
                    ds_ = slice(dch * 128, (dch + 1) * 128)
                    for c in range(8):
                        pe(PS[4][:, :256], wout_sb[:, c, ds_], mergedT[:, c, :], c == 0, c == 7, ["wout", "mergedT"], [PK[4]])
                    tt("dve", xo[:, dch, :], PS[4][:, :256], x3[:, dch, :], ALU.add, [PK[4], "x3"], ["xo"])
                P.dma("sp", xo_o[:, tsl].rearrange("(c p) t -> p c t", p=128), xo[:], reads=["xo"], writes=["xo_o"])
                if not FINAL:
                    return
                act(h3[:], xo[:], AF.Square, ["xo"], ["h3"])
                for c in range(8):
                    pe(PS[5][:, :256], ones_b, h3[:, c, :], c == 0, c == 7, ["cbf", "h3"], [PK[5]])
                act(rstd3[:], PS[5][:, :256], AF.Ln, [PK[5]], ["rstd3"], bias=EPS, scale=1.0 / 1024)
                act(rstd3[:], rstd3[:], AF.Exp, ["rstd3"], ["rstd3"], scale=-0.5)
                for c in range(8):
                    stt("dve", x3[:, c, :], xo[:, c, :], fg_sb[:, c:c + 1], rstd3[:], ALU.mult, ALU.mult, ["xo", "fg", "rstd3"], ["x3"])
                P.dma("sp", xn_o[:, tsl].rearrange("(c p) t -> p c t", p=128), x3[:], reads=["x3"], writes=["xn_o"])

            for tg in range(NT3):
                p3_group(tg)
            P.barrier()
            P.emit()
    return nc


def host_prep_B(w_in, pe_k, pe_v, wc1_k, wc2_k, wc1_v, wc2_v, mem_norm, w_mem_kv, w_branch, w_out, final_norm):
    c = lambda n: _cols(w_in, n)
    p64 = _perm_idx(256, 64)
    wq = np.concatenate([
        c('dsa_q'), c('dsa_q')[:, p64],
        c('idx_q'), c('idx_q')[:, _perm_idx(128, 32)],
        c('fox_q'), c('sb_q'),
        c('nsa_q'), c('nsa_q')[:, p64],
        c('mem_q')], 1)
    assert wq.shape[1] == B_NWQ
    w1 = np.concatenate([wc1_k.reshape(32, 64, 128).transpose(1, 0, 2), wc1_v.reshape(32, 64, 128).transpose(1, 0, 2)], 0)
    return {
        "wq": np.ascontiguousarray(wq),
        "wsm": np.ascontiguousarray(np.concatenate([c('idx_w'), c('nsa_g')], 1)),
        "wz": np.ascontiguousarray(np.concatenate([c('dsa_z'), c('fox_z'), c('sb_z'), c('nsa_z'), c('mem_z')], 1)),
        "wmg": np.ascontiguousarray(c('merge')),
        "wbr": np.ascontiguousarray(w_branch),
        "wout": np.ascontiguousarray(w_out),
        "w1": np.ascontiguousarray(w1.reshape(128, 32 * 128)),
        "w2k2": np.ascontiguousarray(np.concatenate([wc2_k, wc2_k], 1)),
        "w2v": np.ascontiguousarray(wc2_v),
        "peT": np.ascontiguousarray(np.concatenate([pe_k.T, pe_v.T], 0)),
        "mgcol": np.ascontiguousarray(mem_norm.reshape(8, 128).T),
        "wmem": np.ascontiguousarray(w_mem_kv),
        "fgcol": np.ascontiguousarray(final_norm.reshape(8, 128).T),
    }


def host_consts_B(T, r):
    S = 4 * T
    NQ = T // 128
    NBS = S // 64
    NCMP = S // 16 - 1
    NCC = (NCMP + 127) // 128
    NCP = NCC * 128
    s = np.arange(128)[:, None]
    q = np.arange(128)[None, :]
    ident = (s == q).astype(np.float32)
    triS = (s >= q).astype(np.float32)
    ones = np.ones((128, 128), np.float32)
    dmask = np.zeros((128, 4, 128), np.float32)
    dmaskS = np.zeros((128, 4, 128), np.float32)
    for jj in range(4):
        if jj < r:
            dmask[:, jj, :] = 1.0
            dmaskS[:, jj, :] = 1.0
        elif jj == r:
            dmask[:, jj, :] = (s <= q)
            dmaskS[:, jj, :] = (s < q)
    wmask = np.zeros((128, 8, 128), np.float32)
    for jj in range(8):
        diff = (r + 4 - jj) * 128 + q - s
        wmask[:, jj, :] = ((diff >= 0) & (diff < 512))
    dmaskN01 = dmask.transpose(2, 1, 0).reshape(128, 512)
    c_bf = np.concatenate([ident, triS, ones, dmask.reshape(128, 512), dmaskS.reshape(128, 512),
                           wmask.reshape(128, 1024), dmaskN01], 1).astype(NPBF)
    triInc = (s <= q).astype(np.float32)
    dmaskN = np.where(dmaskN01 > 0, 0.0, NEG_BIG).astype(np.float32)
    oh = np.zeros((128, 4), np.float32)
    oh[:, r] = 1.0
    sel4 = (np.arange(128)[:, None] // 32 == np.arange(4)[None, :]).astype(np.float32)
    tq = ((4 * np.arange(NQ)[None, :] + r) * 128 + np.arange(128)[:, None]).astype(np.float32)
    ncol = (16.0 * (128 * np.arange(NCC)[None, :] + np.arange(128)[:, None]) + 31.0).astype(np.float32)
    nrow = np.broadcast_to((16.0 * np.arange(NCP) + 31.0)[None, :], (128, NCP)).astype(np.float32)
    c_f = np.concatenate([triInc, ones, ident, dmaskN, oh, sel4, tq, ncol, nrow], 1).astype(np.float32)
    trow = np.ascontiguousarray(tq.T.reshape(1, T))
    cur = (tq // 64)[:, :, None]
    blk = np.arange(NBS)[None, None, :]
    forced = (blk == 0) | (blk == cur) | (blk == cur - 1)
    visible = blk <= cur
    vis = (visible & ~forced).astype(np.float32)
    addc = np.where(forced, 1.0e4 + blk, np.where(visible, 0.0, -1.0)).astype(np.float32)
    return {"c_bf": np.ascontiguousarray(c_bf), "c_f": np.ascontiguousarray(c_f), "trow": trow.astype(np.float32),
            "vis": np.ascontiguousarray(vis.reshape(128, NQ * NBS)), "addc": np.ascontiguousarray(addc.reshape(128, NQ * NBS))}


def shard_tokens(a, T):
    NQ = T // 128
    v = a.reshape((NQ, 4, 128) + a.shape[1:])
    return [np.ascontiguousarray(v[:, r].reshape((T,) + a.shape[1:])) for r in range(4)]


def unshard_tokens(parts, T):
    NQ = T // 128
    tail = parts[0].shape[1:]
    v = np.stack([p.reshape((NQ, 128) + tail) for p in parts], 1)
    return v.reshape((4 * T,) + tail)


A_W = {"gcol": [128, 8], "wfm": [1024, A_NFM], "wtm": [1024, A_NTM], "kvg": [128, 1], "wuk2": [128, 512],
       "wuv": [128, 256], "foxb": [128, 4]}
B_W = {"wq": [1024, B_NWQ], "wsm": [1024, 16], "wz": [1024, 1280], "wmg": [1024, 5120], "wbr": [5, 256, 1024],
       "wout": [1024, 1024], "w1": [128, 32 * 128], "w2k2": [128, 128], "w2v": [128, 64], "peT": [128, 32],
       "mgcol": [128, 8], "wmem": [1024, 512]}
GROUPS = [[0, 1, 2, 3], [4, 5, 6, 7]]


def build_F(T, depth):
    nc = bass.Bass("TRN2", target_bir_lowering=False)
    S = 4 * T
    NB = S // 128
    NQ = T // 128
    NBS = S // 64
    NCMP = S // 16 - 1
    NCC = (NCMP + 127) // 128
    NCP = NCC * 128
    EI = lambda name, shape, dt=F32: nc.dram_tensor(name, list(shape), dt, kind="ExternalInput").ap()
    IN = lambda name, shape, dt: nc.dram_tensor(name, list(shape), dt).ap()
    xT = EI("xT", [1024, T])
    pos = EI("pos", [1, T], I32)
    memT = EI("memT", [1024, 256])
    rc = EI("rc", [128, 8])
    fgcol = EI("fgcol", [128, 8])
    cst = {"c_bf": EI("c_bf", [128, 128 * 3 + 512 * 2 + 1024 + 512], BF16),
           "c_f": EI("c_f", [128, 128 * 3 + 512 + 4 + 4 + NQ + NCC + NCP]),
           "trow": EI("trow", [1, T]), "vis": EI("vis", [128, NQ * NBS]), "addc": EI("addc", [128, NQ * NBS])}
    WA = {k: EI(k, [depth] + v) for k, v in A_W.items()}
    WB = {k: EI(k, [depth] + v) for k, v in B_W.items()}
    xn_o = nc.dram_tensor("xn", [1024, T], F32, kind="ExternalOutput").ap()
    ydram = IN("ydram", [NQ, 128, 1280], BF16)
    with ExitStack() as stack:
        P = Prog(nc, stack)
        M = Pool2(nc, stack)
        PS = [M.ps("ps%d" % i, [128, 512]) for i in range(8)]
        x_cur = xT
        for l in range(depth):
            hT_l = IN("hT%d" % l, [1024, T], BF16)
            tab_l = IN("tab%d" % l, [4, 128, T], F32)
            kfmL = IN("kfmL%d" % l, [KFM_N, T], BF16)
            v3L = IN("v3L%d" % l, [3, T, 260], BF16)
            v2L = IN("v2L%d" % l, [2, T, 65], BF16)
            lfL = IN("lfL%d" % l, [T, 4], F32)
            kfmG = [IN("kfmG%d_%d" % (l, c), [512, T], BF16) for c in range(KFM_N // 128)]
            v3G = [[IN("v3G%d_%d_%d" % (l, k, hf), [4 * (T // 2), 260], BF16) for hf in range(2)] for k in range(3)]
            v2G = [[IN("v2G%d_%d_%d" % (l, k, hf), [4 * (T // 2), 65], BF16) for hf in range(2)] for k in range(2)]
            lfG = IN("lfG%d" % l, [4 * T, 4], F32)
            x_nxt = IN("x%d" % (l + 1), [1024, T], F32)
            ovA = {k: v[l] for k, v in WA.items()}
            ovA.update({"xT": x_cur, "pos": pos, "rc": rc, "hT": hT_l, "kfm": kfmL, "v3": v3L, "v2": v2L, "lf": lfL, "tab": tab_l})
            build_A(T, {"nc": nc, "P": P, "PS": PS, "ov": ovA})
            for c in range(KFM_N // 128):
                P.coll([kfmL[c * 128:(c + 1) * 128, :]], [kfmG[c]], GROUPS)
            for k in range(3):
                for hf in range(2):
                    P.coll([v3L[k, hf * (T // 2):(hf + 1) * (T // 2), :]], [v3G[k][hf]], GROUPS)
            for k in range(2):
                for hf in range(2):
                    P.coll([v2L[k, hf * (T // 2):(hf + 1) * (T // 2), :]], [v2G[k][hf]], GROUPS)
            P.coll([lfL], [lfG], GROUPS)
            P.barrier()
            ovB = {k: v[l] for k, v in WB.items()}
            ovB.update(cst)
            ovB.update({"xT": x_cur, "hT": hT_l, "tab": tab_l, "kfm": kfmG, "v3": v3G, "v2": v2G, "lf": lfG, "memT": memT,
                        "fgcol": fgcol, "xo": x_nxt, "xn": xn_o, "ydram": ydram})
            build_B(T, {"nc": nc, "P": P, "PS": PS, "ov": ovB, "final": l == depth - 1})
            x_cur = x_nxt
        P.barrier()
        P.emit()
    return nc


_NC_CACHE = {}


def _get_nc(kind, T):
    key = (kind, T)
    if key not in _NC_CACHE:
        _NC_CACHE[key] = build_A(T) if kind == "A" else build_B(T)
    return _NC_CACHE[key]


FUSED = True


def kernel_fused(x, mem, positions, norm_g, w_in, kv_norm, w_uk, w_uv, fox_bias, nsa_pe_k, nsa_pe_v,
                 nsa_wc1_k, nsa_wc2_k, nsa_wc1_v, nsa_wc2_v, mem_norm, w_mem_kv, w_branch, w_out, final_norm):
    f = lambda a: np.asarray(a, dtype=np.float32)
    x = f(x)
    mem = f(mem)
    positions = np.asarray(positions).astype(np.int32)
    Bn, S, D = x.shape
    T = S // 4
    depth = np.asarray(norm_g).shape[0]
    key = ("F", T, depth)
    if key not in _NC_CACHE:
        _NC_CACHE[key] = build_F(T, depth)
    nc = _NC_CACHE[key]
    cores = list(range(8))
    wA = [host_prep_A(f(norm_g[l]), f(w_in[l]), f(kv_norm[l]), f(w_uk[l]), f(w_uv[l]), f(fox_bias[l])) for l in range(depth)]
    wB = [host_prep_B(f(w_in[l]), f(nsa_pe_k[l]), f(nsa_pe_v[l]), f(nsa_wc1_k[l]), f(nsa_wc2_k[l]), f(nsa_wc1_v[l]),
                      f(nsa_wc2_v[l]), f(mem_norm[l]), f(w_mem_kv[l]), f(w_branch[l]), f(w_out[l]), f(final_norm)) for l in range(depth)]
    shared = {k: np.ascontiguousarray(np.stack([wA[l][k] for l in range(depth)], 0)) for k in A_W}
    shared.update({k: np.ascontiguousarray(np.stack([wB[l][k] for l in range(depth)], 0)) for k in B_W})
    shared["rc"] = wA[0]["rc"]
    shared["fgcol"] = wB[0]["fgcol"]
    del wA, wB
    consts = [host_consts_B(T, r) for r in range(4)]
    xs = [shard_tokens(x[b], T) for b in range(Bn)]
    ps = [shard_tokens(positions[b], T) for b in range(Bn)]
    in_maps = []
    for c in cores:
        b, r = c // 4, c % 4
        m = dict(shared)
        m.update(consts[r])
        m["xT"] = np.ascontiguousarray(xs[b][r].T)
        m["pos"] = ps[b][r].reshape(1, T)
        m["memT"] = np.ascontiguousarray(mem[b].T)
        in_maps.append(m)
    R = run_bass_kernel_spmd(nc, in_maps, core_ids=cores).results
    out = np.stack([unshard_tokens([np.ascontiguousarray(np.asarray(R[4 * b + r]["xn"]).T) for r in range(4)], T) for b in range(Bn)], 0)
    return out.astype(np.float32)


def kernel(x, mem, positions, norm_g, w_in, kv_norm, w_uk, w_uv, fox_bias, nsa_pe_k, nsa_pe_v,
           nsa_wc1_k, nsa_wc2_k, nsa_wc1_v, nsa_wc2_v, mem_norm, w_mem_kv, w_branch, w_out, final_norm):
    if FUSED:
        return kernel_fused(x, mem, positions, norm_g, w_in, kv_norm, w_uk, w_uv, fox_bias, nsa_pe_k, nsa_pe_v,
                            nsa_wc1_k, nsa_wc2_k, nsa_wc1_v, nsa_wc2_v, mem_norm, w_mem_kv, w_branch, w_out, final_norm)
    f = lambda a: np.asarray(a, dtype=np.float32)
    x = f(x)
    mem = f(mem)
    positions = np.asarray(positions).astype(np.int32)
    Bn, S, D = x.shape
    T = S // 4
    depth = np.asarray(norm_g).shape[0]
    ncA = _get_nc("A", T)
    ncB = _get_nc("B", T)
    cores = list(range(8))
    consts = [host_consts_B(T, r) for r in range(4)]
    xs = [shard_tokens(x[b], T) for b in range(Bn)]
    ps = [shard_tokens(positions[b], T) for b in range(Bn)]
    xT = [np.ascontiguousarray(xs[c // 4][c % 4].T) for c in cores]
    posr = [ps[c // 4][c % 4].reshape(1, T) for c in cores]
    memT = [np.ascontiguousarray(mem[b].T) for b in range(Bn)]
    xn = None
    for l in range(depth):
        wA = host_prep_A(f(norm_g[l]), f(w_in[l]), f(kv_norm[l]), f(w_uk[l]), f(w_uv[l]), f(fox_bias[l]))
        wB = host_prep_B(f(w_in[l]), f(nsa_pe_k[l]), f(nsa_pe_v[l]), f(nsa_wc1_k[l]), f(nsa_wc2_k[l]), f(nsa_wc1_v[l]),
                         f(nsa_wc2_v[l]), f(mem_norm[l]), f(w_mem_kv[l]), f(w_branch[l]), f(w_out[l]), f(final_norm))
        in_maps = []
        for c in cores:
            m = dict(wA)
            m["xT"] = xT[c]
            m["pos"] = posr[c]
            in_maps.append(m)
        RA = run_bass_kernel_spmd(ncA, in_maps, core_ids=cores).results
        del in_maps
        full = []
        for b in range(Bn):
            kfm = unshard_tokens([np.ascontiguousarray(np.asarray(RA[4 * b + r]["kfm"]).T) for r in range(4)], T)
            v3 = np.stack([unshard_tokens([np.asarray(RA[4 * b + r]["v3"])[k] for r in range(4)], T) for k in range(3)], 0)
            v2 = np.stack([unshard_tokens([np.asarray(RA[4 * b + r]["v2"])[k] for r in range(4)], T) for k in range(2)], 0)
            lf = unshard_tokens([np.asarray(RA[4 * b + r]["lf"]) for r in range(4)], T)
            full.append({
                "kfm": np.ascontiguousarray(kfm.T), "v3": np.ascontiguousarray(v3), "v2": np.ascontiguousarray(v2),
                "lf": np.ascontiguousarray(lf.reshape(S // 128, 128, 4).transpose(1, 0, 2).reshape(128, -1)),
            })
        in_maps = []
        for c in cores:
            b, r = c // 4, c % 4
            m = dict(wB)
            m.update(consts[r])
            m.update(full[b])
            m["xT"] = xT[c]
            m["hT"] = np.asarray(RA[c]["hT"])
            m["tab"] = np.asarray(RA[c]["tab"])
            m["memT"] = memT[b]
            in_maps.append(m)
        del RA
        RB = run_bass_kernel_spmd(ncB, in_maps, core_ids=cores).results
        del in_maps, full
        xT = [np.ascontiguousarray(np.asarray(RB[c]["xo"])) for c in cores]
        if l == depth - 1:
            xn = [np.asarray(RB[c]["xn"]) for c in cores]
        del RB
    out = np.stack([unshard_tokens([np.ascontiguousarray(xn[4 * b + r].T) for r in range(4)], T) for b in range(Bn)], 0)
    return out.astype(np.float32)
```
